# Optimizing a Trainium2 kernel written in Bass

```python
import jax, jax.numpy as jnp
from jax import lax
import numpy as np

D_MODEL = 1024
BATCH = 8
SEQ = 8192
DEPTH = 2

NORM_EPS = 1e-6
HEAD_DIM = 64
MIX_WIDTH = D_MODEL
GROUP_WIDTH = MIX_WIDTH // 4
A_HEADS = GROUP_WIDTH // HEAD_DIM
A_WIDTH = A_HEADS * HEAD_DIM
MOBA_BLOCK = 256
MOBA_TOPK = 3
MOBA_QCHUNK = 64
POOL_WINDOWS = (2, 4, 8, 16)
POOL_GROUPS = 4
B_WIDTH = GROUP_WIDTH
POOL_GROUP_DIM = B_WIDTH // POOL_GROUPS
C_HEADS = GROUP_WIDTH // HEAD_DIM
C_KV_HEADS = C_HEADS // 2
C_WIDTH = C_HEADS * HEAD_DIM
C_KV_WIDTH = C_KV_HEADS * HEAD_DIM
SWA_WINDOW = 128
SWA_BLOCK = 128
D_WIDTH = GROUP_WIDTH
CONV_WIDTH = 3
PROJ_SIZES = (A_WIDTH, A_WIDTH, A_WIDTH, A_WIDTH,
              B_WIDTH, B_WIDTH,
              C_WIDTH, C_KV_WIDTH, C_KV_WIDTH, C_WIDTH,
              D_WIDTH, D_WIDTH, D_WIDTH, D_WIDTH)
IN_PROJ_WIDTH = 4 * A_WIDTH + 2 * B_WIDTH + 2 * C_WIDTH + 2 * C_KV_WIDTH + 4 * D_WIDTH

kernel_name = "hymba_style_moba_pool_swa_conv_hybrid"


def rms_norm(x, gain):
    xf = x.astype(jnp.float32)
    y = xf * lax.rsqrt(jnp.mean(xf * xf, axis=-1, keepdims=True) + NORM_EPS)
    return (y * gain.astype(jnp.float32)).astype(x.dtype)


def alibi_slopes(n):
    return jnp.exp2(-(8.0 / n) * jnp.arange(1, n + 1, dtype=jnp.float32))


def moba_attention(q, k, v, slopes):
    B, H, S, Dh = q.shape
    nb = max(-(-S // MOBA_BLOCK), MOBA_TOPK)
    pad = nb * MOBA_BLOCK - S
    kb = jnp.pad(k, ((0, 0), (0, 0), (0, pad), (0, 0))).reshape(B, H, nb, MOBA_BLOCK, Dh)
    vb = jnp.pad(v, ((0, 0), (0, 0), (0, pad), (0, 0))).reshape(B, H, nb, MOBA_BLOCK, Dh)
    kmean = jnp.mean(kb.astype(jnp.float32), axis=3)
    nq = S // MOBA_QCHUNK
    qc = q.reshape(B, H, nq, MOBA_QCHUNK, Dh).transpose(2, 0, 1, 3, 4)
    scale = Dh ** -0.5
    bidx = jnp.arange(B)[:, None, None, None]
    hidx = jnp.arange(H)[None, :, None, None]
    sl = slopes.astype(jnp.float32)

    def one_chunk(args):
        qi, c = args
        t0 = c * MOBA_QCHUNK
        own = t0 // MOBA_BLOCK
        tq = t0 + jnp.arange(MOBA_QCHUNK)
        qf = qi.astype(jnp.float32)
        bscore = jnp.einsum('bhqd,bhnd->bhqn', qf, kmean)
        bscore = jnp.where(jnp.arange(nb) < own, bscore, -jnp.inf)
        _, idx = lax.top_k(bscore, MOBA_TOPK)
        sel_valid = jnp.arange(MOBA_TOPK) < own
        ksel = kb[bidx, hidx, idx].astype(jnp.float32)
        vsel = vb[bidx, hidx, idx].astype(jnp.float32)
        kpos = idx[..., None] * MOBA_BLOCK + jnp.arange(MOBA_BLOCK)
        dsel = (tq[None, None, :, None, None] - kpos).astype(jnp.float32)
        s_sel = jnp.einsum('bhqd,bhqjkd->bhqjk', qf, ksel) * scale - sl[None, :, None, None, None] * dsel
        s_sel = jnp.where(sel_valid[:, None], s_sel, -jnp.inf)
        s_sel = s_sel.reshape(B, H, MOBA_QCHUNK, MOBA_TOPK * MOBA_BLOCK)
        kown = lax.dynamic_index_in_dim(kb, own, axis=2, keepdims=False).astype(jnp.float32)
        vown = lax.dynamic_index_in_dim(vb, own, axis=2, keepdims=False).astype(jnp.float32)
        dist = tq[:, None] - (own * MOBA_BLOCK + jnp.arange(MOBA_BLOCK))[None, :]
        s_own = jnp.einsum('bhqd,bhkd->bhqk', qf, kown) * scale - sl[None, :, None, None] * dist.astype(jnp.float32)
        s_own = jnp.where(dist >= 0, s_own, -jnp.inf)
        p = jax.nn.softmax(jnp.concatenate([s_sel, s_own], axis=-1), axis=-1)
        p_sel = p[..., :MOBA_TOPK * MOBA_BLOCK].reshape(B, H, MOBA_QCHUNK, MOBA_TOPK, MOBA_BLOCK)
        p_own = p[..., MOBA_TOPK * MOBA_BLOCK:]
        out = (jnp.einsum('bhqjk,bhqjkd->bhqd', p_sel, vsel)
               + jnp.einsum('bhqk,bhkd->bhqd', p_own, vown))
        return out.astype(q.dtype)

    out = lax.map(one_chunk, (qc, jnp.arange(nq)))
    return out.transpose(1, 2, 0, 3, 4).reshape(B, H, S, Dh)


def swa_attention(q, k, v, sinks, slopes):
    B, Hq, S, Dh = q.shape
    Hkv = k.shape[1]
    G = Hq // Hkv
    W = SWA_BLOCK
    nblk = S // W
    qb = q.reshape(B, Hkv, G, nblk, W, Dh).astype(jnp.float32)
    kb = k.reshape(B, Hkv, nblk, W, Dh).astype(jnp.float32)
    vb = v.reshape(B, Hkv, nblk, W, Dh).astype(jnp.float32)
    kk = jnp.concatenate([jnp.pad(kb, ((0, 0), (0, 0), (1, 0), (0, 0), (0, 0)))[:, :, :-1], kb], axis=3)
    vv = jnp.concatenate([jnp.pad(vb, ((0, 0), (0, 0), (1, 0), (0, 0), (0, 0)))[:, :, :-1], vb], axis=3)
    s = jnp.einsum('bkgnqd,bkncd->bkgnqc', qb, kk) * (Dh ** -0.5)
    kpos = jnp.arange(2 * W) - W
    dist = jnp.arange(W)[:, None] - kpos[None, :]
    mask = ((dist >= 0) & (dist < SWA_WINDOW))[None] & ((jnp.arange(nblk)[:, None, None] > 0) | (kpos[None, None, :] >= 0))
    sl = slopes.astype(jnp.float32).reshape(Hkv, G)
    s = s - sl[None, :, :, None, None, None] * dist.astype(jnp.float32)
    s = jnp.where(mask, s, -jnp.inf)
    sink = jnp.broadcast_to(sinks.astype(jnp.float32).reshape(Hkv, G)[None, :, :, None, None, None], s.shape[:-1] + (1,))
    p = jax.nn.softmax(jnp.concatenate([s, sink], axis=-1), axis=-1)[..., :2 * W]
    o = jnp.einsum('bkgnqc,bkncd->bkgnqd', p, vv)
    return o.reshape(B, Hq, S, Dh).astype(q.dtype)


def multiscale_pool(u):
    S = u.shape[1]
    uf = u.astype(jnp.float32)
    cs = jnp.cumsum(uf, axis=1)
    pos = jnp.arange(S)
    outs = []
    for gi, w in enumerate(POOL_WINDOWS):
        c = cs[..., gi * POOL_GROUP_DIM:(gi + 1) * POOL_GROUP_DIM]
        shifted = jnp.pad(c, ((0, 0), (w, 0), (0, 0)))[:, :S]
        cnt = jnp.minimum(pos + 1, w).astype(jnp.float32)[None, :, None]
        outs.append((c - shifted) / cnt)
    return (jnp.concatenate(outs, axis=-1) - uf).astype(u.dtype)


def short_conv(u, w):
    C = u.shape[-1]
    return lax.conv_general_dilated(u, w[:, None, :].astype(u.dtype), window_strides=(1,),
                                    padding=((CONV_WIDTH - 1, 0),),
                                    dimension_numbers=('NWC', 'WIO', 'NWC'),
                                    feature_group_count=C)


def hybrid_layer(x, norm_g, w_in, w_out, a_qn, a_kn, pool_w, pool_scale, c_qn, c_kn, c_sinks, conv_w, slopes_a, slopes_c):
    B, S, _ = x.shape
    h = rms_norm(x, norm_g)
    proj = h @ w_in.astype(h.dtype)
    split_points = [int(p) for p in np.cumsum(PROJ_SIZES)[:-1]]
    aq, ak, av, ag, bu, bg, cq, ck, cv, cg, dh, db, dc, dg = jnp.split(proj, split_points, axis=-1)

    def to_heads(t):
        return t.reshape(B, S, -1, HEAD_DIM).transpose(0, 2, 1, 3)

    def from_heads(t):
        return t.transpose(0, 2, 1, 3).reshape(B, S, -1)

    ya = moba_attention(rms_norm(to_heads(aq), a_qn), rms_norm(to_heads(ak), a_kn), to_heads(av), slopes_a)
    ya = from_heads(ya) * jax.nn.silu(ag)
    pooled = multiscale_pool(bu).reshape(B, S, POOL_GROUPS, POOL_GROUP_DIM)
    yb = jnp.einsum('bsgc,gcd->bsgd', pooled, pool_w.astype(pooled.dtype)).reshape(B, S, B_WIDTH)
    yb = yb * pool_scale * jax.nn.silu(bg)
    yc = swa_attention(rms_norm(to_heads(cq), c_qn), rms_norm(to_heads(ck), c_kn), to_heads(cv), c_sinks, slopes_c)
    yc = from_heads(yc) * jax.nn.silu(cg)
    yd = db * short_conv(dc * dh, conv_w) * jax.nn.silu(dg)

    y = jnp.concatenate([ya, yb, yc, yd], axis=-1) @ w_out.astype(x.dtype)
    return x + y


def setup_inputs(seed: int = 0) -> dict:
    key = jax.random.key(seed)
    ks = jax.random.split(key, 14)
    f32 = jnp.float32
    nrm = lambda k, s: jax.random.normal(k, s, dtype=f32)
    return {
        "x": nrm(ks[0], (BATCH, SEQ, D_MODEL)),
        "norm_g": 1.0 + 0.02 * nrm(ks[1], (DEPTH, D_MODEL)),
        "w_in": nrm(ks[2], (DEPTH, D_MODEL, IN_PROJ_WIDTH)) * D_MODEL ** -0.5,
        "w_out": nrm(ks[3], (DEPTH, MIX_WIDTH, D_MODEL)) * MIX_WIDTH ** -0.5,
        "a_q_norm": 1.0 + 0.02 * nrm(ks[4], (DEPTH, HEAD_DIM)),
        "a_k_norm": 1.0 + 0.02 * nrm(ks[5], (DEPTH, HEAD_DIM)),
        "pool_w": nrm(ks[6], (DEPTH, POOL_GROUPS, POOL_GROUP_DIM, POOL_GROUP_DIM)) * POOL_GROUP_DIM ** -0.5,
        "pool_scale": 1.0 + 0.1 * nrm(ks[7], (DEPTH, B_WIDTH)),
        "c_q_norm": 1.0 + 0.02 * nrm(ks[8], (DEPTH, HEAD_DIM)),
        "c_k_norm": 1.0 + 0.02 * nrm(ks[9], (DEPTH, HEAD_DIM)),
        "c_sinks": nrm(ks[10], (DEPTH, C_HEADS)),
        "conv_w": nrm(ks[11], (DEPTH, CONV_WIDTH, D_WIDTH)) * CONV_WIDTH ** -0.5,
    }


def reference(x, norm_g, w_in, w_out, a_q_norm, a_k_norm, pool_w, pool_scale, c_q_norm, c_k_norm, c_sinks, conv_w):
    slopes = alibi_slopes(A_HEADS + C_HEADS)
    slopes_c = slopes[:C_HEADS]
    slopes_a = slopes[C_HEADS:]
    for l in range(DEPTH):
        x = hybrid_layer(x, norm_g[l], w_in[l], w_out[l], a_q_norm[l], a_k_norm[l],
                         pool_w[l], pool_scale[l], c_q_norm[l], c_k_norm[l], c_sinks[l],
                         conv_w[l], slopes_a, slopes_c)
    return x
```

```python
import contextlib
import types
import numpy as np
import ml_dtypes
import concourse.bass as bass
import concourse.mybir as mybir
from concourse.bass_utils import run_bass_kernel_spmd

F32 = mybir.dt.float32
BF16 = mybir.dt.bfloat16
AF = mybir.ActivationFunctionType
ALU = mybir.AluOpType

D = 1024
NIN = 3328
TT = 512
BIG = 30000.0
EPS = 1e-6
NSPR = 24
ENGS = ("pe", "act", "dve", "pool", "sp")
BLK = {"pe": "tensor", "act": "scalar", "dve": "vector", "pool": "gpsimd", "sp": "sync"}

CB_ID, CB_OB, CB_CM, CB_CMS, CB_N = 0, 128, 256, 256 + 2048, 256 + 2048 + 1024
CF_AB, CF_SB, CF_IW, CF_TB, CF_N = 0, 256, 260, 262, 262 + 32


def slopes_all():
    return np.exp2(-(8.0 / 8) * np.arange(1, 9, dtype=np.float32)).astype(np.float32)


def make_consts(S):
    sl = slopes_all()
    sl_c, sl_a = sl[:4], sl[4:]
    cb = np.zeros((128, CB_N), np.float32)
    cb[:, CB_ID:CB_ID + 128] = np.eye(128)
    cb[0:64, CB_OB:CB_OB + 64] = 1.0
    cb[64:128, CB_OB + 64:CB_OB + 128] = 1.0
    kl = np.arange(128)[:, None]
    ql = np.arange(512)[None, :]
    for kt in range(4):
        m = ((ql // 256) == (kt // 2)) & (ql < 128 * kt + kl)
        cb[:, CB_CM + kt * 512:CB_CM + (kt + 1) * 512] = np.where(m, -BIG, 0.0)
    q1 = np.arange(128)[None, :]
    for h in range(4):
        prev = np.where(kl > q1, -8.0 * sl_c[h] * 128.0, -BIG)
        own = np.where(kl <= q1, 0.0, -BIG)
        cb[:, CB_CMS + h * 256:CB_CMS + h * 256 + 128] = prev
        cb[:, CB_CMS + h * 256 + 128:CB_CMS + (h + 1) * 256] = own
    cf = np.zeros((128, CF_N), np.float32)
    for h in range(4):
        for i in range(64):
            d = 3 - i
            cf[:, CF_AB + h * 64 + i] = sl_a[h] * (np.arange(128) - 511 + 128 * d)
        cf[:, CF_SB + h] = sl_c[h] * np.arange(128)
    wins = (2, 4, 8, 16)
    for c in range(2):
        for half in range(2):
            w = wins[2 * c + half]
            rows = slice(half * 64, (half + 1) * 64)
            cf[rows, CF_IW + c] = 1.0 / w
            cf[rows, CF_TB + c * 16:CF_TB + (c + 1) * 16] = 1.0 / np.minimum(np.arange(16) + 1, w)
    khot = np.zeros((32, S), np.float32)
    for n in range(S // 256):
        khot[n, n * 256:(n + 1) * 256] = 1.0
    cq = np.zeros((4, 512), np.float32)
    for h in range(4):
        cq[h] = -8.0 * sl_c[h] * (np.arange(512) % 128)
    bf = ml_dtypes.bfloat16
    return {"cb": cb.astype(bf), "cf": cf, "khot": khot.astype(bf), "cqrow": cq.astype(bf)}


def _freeze(fn):
    if fn is None or fn.__closure__ is None:
        return fn
    cells = []
    for c in fn.__closure__:
        try:
            cells.append(types.CellType(c.cell_contents))
        except ValueError:
            cells.append(c)
    g = types.FunctionType(fn.__code__, fn.__globals__, fn.__name__, fn.__defaults__, tuple(cells))
    g.__kwdefaults__ = fn.__kwdefaults__
    return g


class Prog:
    def __init__(self, nc, es):
        self.nc = nc
        self.es = es
        self.ops = {e: [] for e in ENGS}
        self.cnt = {e: 0 for e in ENGS}
        self.semh = {}
        self.dcnt = {}
        self.lastw = {}
        self.readers = {}
        self.waited = {e: {} for e in ENGS}
        for e in ENGS:
            self.semh["E:" + e] = es.enter_context(nc.semaphore("sem_" + e))

    def _collect(self, eng, r, w):
        need = {}

        def add(ev):
            semid, val, src = ev
            if src == eng and eng == "pe":
                return
            if self.waited[eng].get(semid, 0) >= val:
                return
            if need.get(semid, 0) < val:
                need[semid] = val

        for k in r:
            if k in self.lastw:
                add(self.lastw[k])
        for k in w:
            if k in self.lastw:
                add(self.lastw[k])
            for ev in self.readers.get(k, {}).values():
                add(ev)
        for semid, val in need.items():
            self.waited[eng][semid] = val
        return list(need.items())

    def _commit(self, ev, r, w):
        for k in r:
            d = self.readers.setdefault(k, {})
            if ev[0] not in d or d[ev[0]][1] < ev[1]:
                d[ev[0]] = ev
        for k in w:
            self.lastw[k] = ev
            self.readers[k] = {}

    stopped = False

    def cut(self, k):
        import os
        if int(os.environ.get("STAGE", "99")) == k and not self.stopped:
            self.barrier()
            self.stopped = True

    def op(self, eng, fn, r=(), w=(), sig=True):
        if self.stopped:
            return
        fn = _freeze(fn)
        waits = self._collect(eng, r, w)
        semid = "E:" + eng
        if sig:
            self.cnt[eng] += 1
            ev = (semid, self.cnt[eng], eng)
            inc = (semid, 1)
        else:
            ev = (semid, self.cnt[eng] + 1, eng)
            inc = None
        self.ops[eng].append((waits, fn, inc))
        self._commit(ev, r, w)

    def dma(self, fn, r=(), w=(), key=None, q="sp"):
        if self.stopped:
            return
        fn = _freeze(fn)
        waits = self._collect(q, r, w)
        semid = "D:" + key
        if semid not in self.semh:
            self.semh[semid] = self.es.enter_context(self.nc.semaphore("dsem_" + key))
            self.dcnt[semid] = 0
        self.dcnt[semid] += 16
        ev = (semid, self.dcnt[semid], "dma")
        self.ops[q].append((waits, fn, (semid, 16)))
        self._commit(ev, r, w)

    def dma_group(self, key, items, q="sp"):
        if self.stopped:
            return
        semid = "D:" + key
        if semid not in self.semh:
            self.semh[semid] = self.es.enter_context(self.nc.semaphore("dsem_" + key))
            self.dcnt[semid] = 0
        total = self.dcnt[semid] + 16 * len(items)
        ev = (semid, total, "dma")
        for fn, r, w in items:
            waits = self._collect(q, r, w)
            self.ops[q].append((waits, _freeze(fn), (semid, 16)))
        for fn, r, w in items:
            self._commit(ev, r, w)
        self.dcnt[semid] = total

    def barrier(self):
        if self.stopped:
            return
        for e in ENGS:
            waits = []
            for o in ENGS:
                if o == e or self.cnt[o] == 0:
                    continue
                sid = "E:" + o
                if self.waited[e].get(sid, 0) < self.cnt[o]:
                    waits.append((sid, self.cnt[o]))
                    self.waited[e][sid] = self.cnt[o]
            for sid, c in self.dcnt.items():
                if self.waited[e].get(sid, 0) < c:
                    waits.append((sid, c))
                    self.waited[e][sid] = c
            if waits:
                self.ops[e].append((waits, None, None))
        self.lastw = {}
        self.readers = {}

    def emit(self, block):
        for eng in ENGS:
            def body(e, eng=eng):
                for waits, fn, inc in self.ops[eng]:
                    for semid, val in waits:
                        e.wait_ge(self.semh[semid], val)
                    if fn is None:
                        continue
                    ins = fn(e)
                    if inc is not None:
                        ins.then_inc(self.semh[inc[0]], inc[1])
            getattr(block, BLK[eng])(body)


class Arena:
    def __init__(self, nc, base, top):
        self.nc, self.base, self.top, self.cur, self.n = nc, base, top, base, 0

    def alloc(self, name, shape, dtype):
        nbytes = int(np.prod(shape[1:])) * (2 if dtype == BF16 else 4)
        nbytes = (nbytes + 63) // 64 * 64
        off = self.cur
        assert off + nbytes <= self.top, f"SBUF overflow at {name}: {off + nbytes} > {self.top}"
        self.cur += nbytes
        self.n += 1
        return self.nc.alloc_sbuf_tensor_at(f"{name}_{self.n}", list(shape), dtype, offset=off)


def build_program(S, NL):
    NT = S // TT
    NKT = S // 128
    nc = bass.Bass("TRN2", target_bir_lowering=False)
    dt = nc.dram_tensor
    x_in = dt("x", [S, D], F32, kind="ExternalInput").ap()
    w_in = dt("w_in", [NL, D, NIN], F32, kind="ExternalInput").ap()
    w_out = dt("w_out", [NL, D, D], F32, kind="ExternalInput").ap()
    spr_d = dt("spr", [NL, 128, NSPR], F32, kind="ExternalInput").ap()
    pw_d = dt("pw", [NL, 128, 2, 128], F32, kind="ExternalInput").ap()
    cb_d = dt("cb", [128, CB_N], BF16, kind="ExternalInput").ap()
    cf_d = dt("cf", [128, CF_N], F32, kind="ExternalInput").ap()
    khot_d = dt("khot", [32, S], BF16, kind="ExternalInput").ap()
    cq_d = dt("cqrow", [4, 512], BF16, kind="ExternalInput").ap()
    out_d = dt("out", [S, D], F32, kind="ExternalOutput").ap()
    hT_d = dt("hT_scr", [D, S], BF16, kind="Internal").ap()
    ya_d = dt("ya_scr", [256, S], BF16, kind="Internal").ap()
    x1_d = dt("x1_scr", [S, D], F32, kind="Internal").ap() if NL > 1 else None

    with contextlib.ExitStack() as es:
        P = Prog(nc, es)
        a_base = (int(nc.sbuf_base) + 63) // 64 * 64
        a_size = int(nc.sbuf_top) - a_base - 2048
        es.enter_context(nc.sbuf_tensor("arena_slab", [128, a_size], mybir.dt.uint8))
        A = Arena(nc, a_base, a_base + a_size)
        pb = [es.enter_context(nc.psum_tensor(f"pb{i}", [128, 512], F32)) for i in range(6)]
        tpb = es.enter_context(nc.psum_tensor("tpb", [128, 512], F32))
        msb = es.enter_context(nc.psum_tensor("msb", [128, 512], F32))
        CB = A.alloc("CB", [128, CB_N], BF16)
        CF = A.alloc("CF", [128, CF_N], F32)
        SPR = A.alloc("SPR", [128, NSPR], F32)
        ESK = A.alloc("ESK", [128, 4], F32)
        SQ = [A.alloc(f"SQ{i}", [128, 512], BF16) for i in range(2)]
        RS = [A.alloc(f"RS{i}", [128, 512], F32) for i in range(2)]
        RC = [A.alloc(f"RC{i}", [128, 512], F32) for i in range(2)]
        ident = CB[:, CB_ID:CB_ID + 128]
        onesblk = CB[:, CB_OB:CB_OB + 128]
        import os
        DBG = os.environ.get("DBG") == "1"
        dbg_off = [0]
        dbg_names = []
        if DBG:
            dbg_d = dt("dbg", [128, 12288], BF16, kind="ExternalOutput").ap()
            DBGT = A.alloc("DBGT", [128, 12288], BF16)

        def dump(name, ap, r, npart=128):
            if not DBG or P.stopped:
                return
            n = ap.shape[-1] if len(ap.shape) == 2 else int(np.prod(ap.shape[1:]))
            o = dbg_off[0]
            dbg_names.append((name, o, n, npart))
            dbg_off[0] += n
            assert dbg_off[0] <= 12288
            P.op("dve", lambda e: e.tensor_copy(out=DBGT[0:npart, o:o + n], in_=ap), r=r, w=["DBG"])
        build_program.dbg_names = dbg_names
        mark = A.cur

        first_items = [(lambda e: e.dma_start(out=CB[:], in_=cb_d[:, :]), [], ["CB"]),
                       (lambda e: e.dma_start(out=CF[:], in_=cf_d[:, :]), [], ["CF"])]

        rr = {"sq": 0, "pj": 0, "rc": 0}

        def nxt(name, n):
            rr[name] = (rr[name] + 1) % n
            return rr[name]

        def qk_norm_prep(pj, pjk):
            i = nxt("sq", 2)
            P.op("act", lambda e: e.activation(out=SQ[i][:], in_=pj[:], func=AF.Square), r=[pjk], w=[("SQ", i)])
            P.op("pe", lambda e: e.matmul(msb[:], onesblk, SQ[i][:], start=True, stop=True),
                 r=[("SQ", i), "CB"], w=["MS"])
            P.op("act", lambda e: e.activation(out=RS[i][:], in_=msb[:], func=AF.Ln, bias=EPS, scale=1.0 / 64),
                 r=["MS"], w=[("RS", i)])
            P.op("act", lambda e: e.activation(out=RS[i][:], in_=RS[i][:], func=AF.Exp, scale=-0.5),
                 r=[("RS", i)], w=[("RS", i)])
            return i

        for l in range(NL):
            xsrc = x_in if l == 0 else x1_d
            xdst = out_d if l == NL - 1 else x1_d
            items = first_items if l == 0 else []
            items.append((lambda e, l=l: e.dma_start(out=SPR[:], in_=spr_d[l, :, :]), [], ["SPR"]))

            A.cur = mark
            WA = A.alloc("WA", [128, 8, 1024], BF16)
            KA = [A.alloc(f"KA{h}", [96, S], BF16) for h in range(4)]
            VA = A.alloc("VA", [128, NKT, 384], BF16)
            KM = A.alloc("KM", [128, 2, 64], F32)
            wbase = A.cur
            XT = A.alloc("XT", [128, 4, 1024], F32)
            XS = A.alloc("XS", [128, 4, 1024], BF16)
            WSTG = [nc.alloc_sbuf_tensor_at(f"WSTGa{l}_{i}", [128, 2304], F32, offset=wbase + i * 9216) for i in range(2)]
            HT = [A.alloc(f"HT{i}", [128, 8, 512], BF16) for i in range(2)]
            QF = A.alloc("QF", [128, 2, 512], F32)
            QA = [[A.alloc(f"QA{h}_{s}", [96, 512], BF16) for s in range(1)] * 2 for h in range(4)]
            GA = [A.alloc("GA", [128, 2, 512], BF16)] * 2
            PT = [A.alloc(f"PT{i}", [128, 512], BF16) for i in range(4)]
            YA = [A.alloc("YA", [128, 2, 512], BF16)] * 2
            SSX = A.alloc("SSX", [128, 4], F32)
            RSX = A.alloc("RSX", [128, 4], F32)
            BSM = A.alloc("BSM", [128, 4, 32], F32)
            M8 = A.alloc("M8", [128, 4, 8], F32)
            THR = A.alloc("THR", [128, 4], F32)
            MB = A.alloc("MB", [128, 4, 32], BF16)
            PJ = pb[0:2]
            ST = pb[2:4]
            OT = pb[4:6]

            for h in range(4):
                items.append((lambda e, h=h: e.dma_start(out=KA[h][64:96, :], in_=khot_d[:, :]), [], [("KAaug", h)]))
            P.dma_group("const", items)
            P.op("act", lambda e: e.activation(out=ESK[:], in_=SPR[:, 20:24], func=AF.Exp), r=["SPR"], w=["ESK"])
            P.op("pool", lambda e: e.memset(VA[:, :, 64:128], 1.0), w=["VAones"])
            P.op("pool", lambda e: e.memset(VA[:, :, 256:320], 1.0), w=["VAones"])
            P.op("pool", lambda e: e.memset(KM[:], 0.0), w=["KM"])
            for kc in range(8):
                i = kc % 2
                P.dma(lambda e, kc=kc, i=i, l=l: e.dma_start(out=WSTG[i][:, 0:1024], in_=w_in[l, kc * 128:(kc + 1) * 128, 0:1024]),
                      w=[("WSTG", i)], key=f"w{i}")
                P.op("dve", lambda e, kc=kc, i=i: e.tensor_scalar(out=WA[:, kc, :], in0=WSTG[i][:, 0:1024], scalar1=SPR[:, kc:kc + 1],
                                                                  scalar2=None, op0=ALU.mult),
                     r=[("WSTG", i), "SPR"], w=["WA", "XTa", "XS"])

            def load_x(t):
                P.dma(lambda e, t=t: e.dma_start(out=XT[:], in_=xsrc[t * TT:(t + 1) * TT, :].rearrange("(s p) d -> p s d", p=128)),
                      r=["XTa"], w=["XT"], key="xt")

            P.cut(1)
            load_x(0)
            for t in range(NT):
                s = t % 2
                for sub in range(4):
                    P.op("act", lambda e, sub=sub: e.activation(out=XS[:, sub, :], in_=XT[:, sub, :], func=AF.Square,
                                                                accum_out=SSX[:, sub:sub + 1]),
                         r=["XT"], w=["XS", "SSX"])
                P.op("act", lambda e: e.activation(out=RSX[:], in_=SSX[:], func=AF.Ln, bias=EPS, scale=1.0 / D), r=["SSX"], w=["RSX"])
                P.op("act", lambda e: e.activation(out=RSX[:], in_=RSX[:], func=AF.Exp, scale=-0.5), r=["RSX"], w=["RSX"])
                for sub in range(4):
                    P.op("dve", lambda e, sub=sub: e.tensor_scalar(out=XS[:, sub, :], in0=XT[:, sub, :], scalar1=RSX[:, sub:sub + 1],
                                                                   scalar2=None, op0=ALU.mult),
                         r=["XT", "RSX"], w=["XS"])
                if t + 1 < NT:
                    load_x(t + 1)
                P.cut(12)
                for kc in range(8):
                    hf = kc % 2
                    bank, bkey = (tpb, ("TP", 0)) if hf == 0 else (msb, "MS")
                    for sub in range(4):
                        P.op("pe", lambda e, kc=kc, sub=sub, bank=bank: e.matmul(bank[:, sub * 128:(sub + 1) * 128], XS[:, sub, kc * 128:(kc + 1) * 128], ident,
                                                                                 start=True, stop=True),
                             r=["XS", "CB"], w=[bkey], sig=(sub == 3))
                    P.op("dve", lambda e, kc=kc, bank=bank: e.tensor_copy(out=HT[s][:, kc, :], in_=bank[:, 0:512]),
                         r=[bkey], w=[("HT", s)])
                P.cut(13)
                if t == 0 and l == 0:
                    dump("XS0", XS[:, 0, 0:512], ["XS"])
                    dump("HT0", HT[s][:, 0, :], [("HT", s)])
                    dump("WA0", WA[:, 0, 0:512], ["WA"])
                P.dma(lambda e, t=t, s=s: e.dma_start(out=hT_d[:, t * TT:(t + 1) * TT].rearrange("(kc p) n -> p kc n", p=128), in_=HT[s][:]),
                      r=[("HT", s)], w=["hT_d"], key=f"hts{s}")

                P.cut(2)

                def proj(c0):
                    i = nxt("pj", 2)
                    for kc in range(8):
                        P.op("pe", lambda e, kc=kc, i=i: e.matmul(PJ[i][:], WA[:, kc, c0:c0 + 128], HT[s][:, kc, :], start=(kc == 0), stop=(kc == 7)),
                             r=["WA", ("HT", s)], w=[("PJ", i)], sig=(kc == 7))
                    return i

                for c in range(2):
                    i = proj(256 + c * 128)
                    ri = qk_norm_prep(PJ[i], ("PJ", i))
                    for j in range(2):
                        h = 2 * c + j
                        rows = slice(j * 64, (j + 1) * 64)
                        for b in range(2):
                            n = 2 * t + b
                            P.op("dve", lambda e, i=i, ri=ri, h=h, rows=rows, b=b, n=n: e.scalar_tensor_tensor(
                                out=KA[h][0:64, t * TT + b * 256:t * TT + (b + 1) * 256], in0=PJ[i][rows, b * 256:(b + 1) * 256],
                                scalar=SPR[rows, 9:10], in1=RS[ri][rows, b * 256:(b + 1) * 256], op0=ALU.mult, op1=ALU.mult,
                                accum_out=KM[rows, c, j * 32 + n:j * 32 + n + 1]),
                                 r=[("PJ", i), ("RS", ri), "SPR"], w=[("KA", h, t), "KM"])
                for sub in range(4):
                    i = nxt("pj", 2)
                    for kc in range(8):
                        P.op("pe", lambda e, kc=kc, i=i, sub=sub: e.matmul(PJ[i][:, 0:256], HT[s][:, kc, sub * 128:(sub + 1) * 128], WA[:, kc, 512:768],
                                                                           start=(kc == 0), stop=(kc == 7)),
                             r=["WA", ("HT", s)], w=[("PJ", i)], sig=(kc == 7))
                    kt = 4 * t + sub
                    for (d0, s0, wd) in ((0, 0, 64), (128, 64, 128), (320, 192, 64)):
                        P.op("dve", lambda e, i=i, kt=kt, d0=d0, s0=s0, wd=wd: e.tensor_copy(out=VA[:, kt, d0:d0 + wd], in_=PJ[i][:, s0:s0 + wd]),
                             r=[("PJ", i)], w=[("VA", t)])
                for c in range(2):
                    i = proj(c * 128)
                    ri = qk_norm_prep(PJ[i], ("PJ", i))
                    P.op("dve", lambda e, i=i, ri=ri, c=c: e.scalar_tensor_tensor(
                        out=QF[:, c, :], in0=PJ[i][:], scalar=SPR[:, 8:9], in1=RS[ri][:], op0=ALU.mult, op1=ALU.mult),
                         r=[("PJ", i), ("RS", ri), "SPR"], w=[("QF", c)])
                    P.op("pool", lambda e, c=c: e.tensor_copy(out=QA[2 * c][0][0:64, :], in_=QF[0:64, c, :]),
                         r=[("QF", c)], w=[("QA", 2 * c, 0)])
                    P.op("dve", lambda e, c=c: e.tensor_copy(out=QA[2 * c + 1][0][0:64, :], in_=QF[64:128, c, :]),
                         r=[("QF", c)], w=[("QA", 2 * c + 1, 0)])
                for c in range(2):
                    i = proj(768 + c * 128)
                    P.op("act", lambda e, i=i, c=c: e.activation(out=GA[0][:, c, :], in_=PJ[i][:], func=AF.Silu),
                         r=[("PJ", i)], w=[("GA", 0, c)])
                if t == 0 and l == 0:
                    dump("KA0", KA[0][0:64, 0:512], [("KA", 0, 0)], 64)
                    dump("QF0", QF[:, 0, :], [("QF", 0)])
                    dump("GA0", GA[0][:, 0, :], [("GA", 0, 0)])
                    dump("VA0", VA[:, 0, :], [("VA", 0)])
                    dump("KM", KM[:].rearrange("p c n -> p (c n)"), ["KM"])
                P.cut(3)
                for sub in range(4):
                    own = 2 * t + sub // 2
                    for c in range(2):
                        P.op("pe", lambda e, c=c, sub=sub: e.matmul(msb[:, c * 64:(c + 1) * 64], QF[:, c, sub * 128:(sub + 1) * 128], KM[:, c, :],
                                                                    start=True, stop=True),
                             r=[("QF", c), "KM"], w=["MS"], sig=(c == 1))
                    P.op("pool", lambda e: e.memset(BSM[:], -1e30), w=["BSM"])
                    if own > 0:
                        P.op("dve", lambda e, own=own: e.tensor_copy(out=BSM[:, :, 0:own],
                                                                     in_=msb[:, 0:128].rearrange("p (h n) -> p h n", h=4)[:, :, 0:own]),
                             r=["MS"], w=["BSM"])
                    for h in range(4):
                        P.op("dve", lambda e, h=h: e.max(out=M8[:, h, :], in_=BSM[:, h, :]), r=["BSM"], w=["M8"])
                    P.op("dve", lambda e: e.tensor_scalar(out=THR[:], in0=M8[:, :, 2], scalar1=-1e29, scalar2=None, op0=ALU.max),
                         r=["M8"], w=["THR"])
                    for h in range(4):
                        P.op("dve", lambda e, h=h: e.tensor_scalar(out=MB[:, h, :], in0=BSM[:, h, :], scalar1=THR[:, h:h + 1], scalar2=-BIG,
                                                                   op0=ALU.is_lt, op1=ALU.mult),
                             r=["BSM", "THR"], w=["MB"])
                    P.op("dve", lambda e, own=own: e.memset(MB[:, :, own:own + 1], 0.0), w=["MB"])
                    P.op("pe", lambda e: e.matmul(tpb[:, 0:128], MB[:].rearrange("p h n -> p (h n)"), ident, start=True, stop=True), r=["MB", "CB"], w=[("TP", 0)])
                    for h in range(4):
                        P.op("dve", lambda e, h=h, sub=sub: e.tensor_copy(out=QA[h][0][64:96, sub * 128:(sub + 1) * 128], in_=tpb[h * 32:(h + 1) * 32, 0:128]),
                             r=[("TP", 0)], w=[("QAm", h, 0)])

                if t == 0 and l == 0:
                    for h in range(4):
                        dump(f"QA{h}", QA[h][0][0:96, :], [("QA", h, 0), ("QAm", h, 0)], 96)
                P.cut(4)
                for h in range(4):
                    oslot = h % 2
                    nkt = 4 * t + 4
                    vc0 = (0, 64, 192, 256)[h]
                    pend = None
                    for kt in range(nkt):
                        si = kt % 2
                        diag = kt >= 4 * t
                        P.op("pe", lambda e, h=h, kt=kt, si=si, diag=diag: e.matmul(ST[si][:], KA[h][0:96, kt * 128:(kt + 1) * 128], QA[h][0][0:96, :],
                                                                                    start=True, stop=not diag),
                             r=[("KA", h, kt // 4), ("KAaug", h), ("QA", h, 0), ("QAm", h, 0)], w=[("ST", si)], sig=not diag)
                        if diag:
                            dk = kt - 4 * t
                            P.op("pe", lambda e, si=si, dk=dk: e.matmul(ST[si][:], ident, CB[:, CB_CM + dk * 512:CB_CM + (dk + 1) * 512], start=False, stop=True),
                                 r=["CB"], w=[("ST", si)])
                        if pend is not None:
                            pend()
                        pi = kt % 4
                        col = CF_AB + h * 64 + (3 - (kt - 4 * t))
                        P.op("act", lambda e, si=si, pi=pi, col=col: e.activation(out=PT[pi][:], in_=ST[si][:], func=AF.Exp, bias=CF[:, col:col + 1], scale=0.125),
                             r=[("ST", si), "CF"], w=[("PT", pi)])

                        def pv(h=h, kt=kt, pi=pi, oslot=oslot, vc0=vc0, nkt=nkt):
                            P.op("pe", lambda e: e.matmul(OT[oslot][:], VA[:, kt, vc0:vc0 + 128], PT[pi][:], start=(kt == 0), stop=(kt == nkt - 1)),
                                 r=[("VA", kt // 4), "VAones", ("PT", pi)], w=[("OT", oslot)], sig=True)
                        pend = pv
                    pend()
                    nr = slice(0, 64) if h % 2 == 0 else slice(64, 128)
                    dr = slice(64, 128) if h % 2 == 0 else slice(0, 64)
                    hr = slice((h % 2) * 64, (h % 2) * 64 + 64)
                    ci = nxt("rc", 2)
                    P.op("dve", lambda e, oslot=oslot, dr=dr, ci=ci, hr=hr: e.reciprocal(out=RC[ci][hr, :], in_=OT[oslot][dr, :]), r=[("OT", oslot)], w=[("RC", ci)])
                    P.op("dve", lambda e, ci=ci, hr=hr, h=h: e.tensor_tensor(out=RC[ci][hr, :], in0=RC[ci][hr, :], in1=GA[0][hr, h // 2, :], op=ALU.mult),
                         r=[("RC", ci), ("GA", 0, h // 2)], w=[("RC", ci)])
                    P.op("dve", lambda e, oslot=oslot, nr=nr, ci=ci, hr=hr, h=h: e.tensor_tensor(out=YA[0][hr, h // 2, :], in0=OT[oslot][nr, :], in1=RC[ci][hr, :], op=ALU.mult),
                         r=[("OT", oslot), ("RC", ci)], w=[("YA", 0)])
                P.dma(lambda e, t=t, s=s: e.dma_start(out=ya_d[:, t * TT:(t + 1) * TT].rearrange("(c p) n -> p c n", p=128), in_=YA[0][:]),
                      r=[("YA", 0)], w=["ya_d"], key="yas")
                if t == 0 and l == 0:
                    dump("YA0", YA[0][:, 0, :], [("YA", 0)])
                    dump("YA1", YA[0][:, 1, :], [("YA", 0)])
                P.cut(5)
            P.barrier()
            P.cut(6)

            A.cur = mark
            W2 = A.alloc("W2", [128, 8, 2304], BF16)
            WO = A.alloc("WO", [128, 8, 1024], BF16)
            PWB = A.alloc("PWB", [128, 2, 128], BF16)
            H2 = [A.alloc(f"H2{i}", [128, 8, 512], BF16) for i in range(2)]
            wbase = A.cur
            X2 = [A.alloc(f"X2{i}", [128, 4, 1024], F32) for i in range(2)]
            WSTG = [nc.alloc_sbuf_tensor_at(f"WSTGb{l}_{i}", [128, 2304], F32, offset=wbase + i * 16384) for i in range(2)]
            YT = A.alloc("YT", [128, 8, 512], BF16)
            GS = A.alloc("GS", [128, 6, 512], BF16)
            U = A.alloc("U", [128, 2, 528], F32)
            T1 = A.alloc("T1", [128, 528], F32)
            T2 = A.alloc("T2", [128, 528], F32)
            TF = A.alloc("TF", [128, 16], F32)
            PL = A.alloc("PL", [128, 2, 512], BF16)
            DH = A.alloc("DH", [128, 512], F32)
            Z = A.alloc("Z", [128, 2, 514], F32)
            ACC = A.alloc("ACC", [128, 512], F32)
            QC = [A.alloc(f"QC{h}", [65, 512], BF16) for h in range(4)]
            KC = [A.alloc(f"KC{k}", [65, 640], BF16) for k in range(2)]
            VC = A.alloc("VC", [128, 5, 2, 128], BF16)
            PS = [A.alloc(f"PS{i}", [128, 256], BF16) for i in range(4)]
            PJ = pb[0:2]
            SW = pb[2]
            OC = pb[3]
            PO = pb[4:6]

            P.op("pool", lambda e: e.memset(U[:], 0.0), w=[("U", 0), ("U", 1)])
            P.op("pool", lambda e: e.memset(Z[:], 0.0), w=[("Z", 0), ("Z", 1)])
            P.op("pool", lambda e: e.memset(VC[:, :, :, 64:128], 1.0), w=["VCones"])
            P.op("pool", lambda e: e.memset(VC[:, 0, :, 0:64], 0.0), w=["VC"])
            for k in range(2):
                P.op("pool", lambda e, k=k: e.memset(KC[k][64:65, :], 1.0), w=[("KCaug", k)])
                P.op("pool", lambda e, k=k: e.memset(KC[k][0:64, 0:128], 0.0), w=[("KC", k)])
            P.dma_group("const2", [(lambda e, h=h: e.dma_start(out=QC[h][64:65, :], in_=cq_d[h:h + 1, :]), [], [("QCaug", h)]) for h in range(4)])
            for kc in range(8):
                i = kc % 2
                P.dma(lambda e, kc=kc, i=i, l=l: e.dma_start(out=WSTG[i][:], in_=w_in[l, kc * 128:(kc + 1) * 128, 1024:3328]),
                      w=[("X2", i)], key=f"w{i}")
                P.op("dve", lambda e, kc=kc, i=i: e.tensor_scalar(out=W2[:, kc, :], in0=WSTG[i][:], scalar1=SPR[:, kc:kc + 1], scalar2=None, op0=ALU.mult),
                     r=[("X2", i), "SPR"], w=["W2"])
            for kc in range(8):
                i = kc % 2
                P.dma(lambda e, kc=kc, i=i, l=l: e.dma_start(out=WSTG[i][:, 0:1024], in_=w_out[l, kc * 128:(kc + 1) * 128, :]),
                      w=[("X2", i)], key=f"w{i}")
                P.op("pool", lambda e, kc=kc, i=i: e.tensor_copy(out=WO[:, kc, :], in_=WSTG[i][:, 0:1024]), r=[("X2", i)], w=["WO"])
            P.dma(lambda e, l=l: e.dma_start(out=WSTG[0][:, 0:256], in_=pw_d[l].rearrange("p c n -> p (c n)")), w=[("X2", 0)], key="w0")
            P.op("pool", lambda e: e.tensor_copy(out=PWB[:].rearrange("p c n -> p (c n)"), in_=WSTG[0][:, 0:256]), r=[("X2", 0)], w=["PWB"])

            def load2(t):
                s = t % 2
                P.dma(lambda e, t=t, s=s: e.dma_start(out=H2[s][:], in_=hT_d[:, t * TT:(t + 1) * TT].rearrange("(kc p) n -> p kc n", p=128)),
                      r=["hT_d"], w=[("H2", s)], key=f"h2{s}")
                P.dma(lambda e, t=t, s=s: e.dma_start(out=X2[s][:], in_=xsrc[t * TT:(t + 1) * TT, :].rearrange("(s p) d -> p s d", p=128)),
                      w=[("X2", s)], key=f"x2{s}")

            P.cut(7)
            load2(0)
            for t in range(NT):
                s = t % 2
                if t + 1 < NT:
                    load2(t + 1)
                P.dma(lambda e, t=t: e.dma_start(out=YT[:, 0:2, :], in_=ya_d[:, t * TT:(t + 1) * TT].rearrange("(c p) n -> p c n", p=128)),
                      r=["ya_d"], w=[("YT", 0), ("YT", 1)], key="yal")

                def proj2(c0):
                    i = nxt("pj", 2)
                    for kc in range(8):
                        P.op("pe", lambda e, kc=kc, i=i: e.matmul(PJ[i][:], W2[:, kc, c0:c0 + 128], H2[s][:, kc, :], start=(kc == 0), stop=(kc == 7)),
                             r=["W2", ("H2", s)], w=[("PJ", i)], sig=(kc == 7))
                    return i

                for gi, c0 in enumerate((256, 384, 1024, 1152, 2048, 2176)):
                    i = proj2(c0)
                    P.op("act", lambda e, i=i, gi=gi: e.activation(out=GS[:, gi, :], in_=PJ[i][:], func=AF.Silu), r=[("PJ", i)], w=[("GS", gi)])

                P.cut(8)
                for c in range(2):
                    i = proj2(c * 128)
                    P.op("act", lambda e, i=i, c=c: e.activation(out=U[:, c, 16:528], in_=PJ[i][:], func=AF.Copy), r=[("PJ", i)], w=[("U", c)])
                    Uc = U[:, c, :]
                    P.op("pool", lambda e, Uc=Uc: e.tensor_tensor(out=T1[:, 1:528], in0=Uc[:, 1:528], in1=Uc[:, 0:527], op=ALU.add), r=[("U", c)], w=["T1"])
                    lo, hi = slice(0, 64), slice(64, 128)
                    if c == 0:
                        P.op("pool", lambda e: e.tensor_tensor(out=T2[hi, 3:528], in0=T1[hi, 3:528], in1=T1[hi, 1:526], op=ALU.add), r=["T1"], w=["T2"])
                    else:
                        P.op("pool", lambda e: e.tensor_tensor(out=T2[:, 3:528], in0=T1[:, 3:528], in1=T1[:, 1:526], op=ALU.add), r=["T1"], w=["T2"])
                        P.op("pool", lambda e: e.tensor_tensor(out=T1[:, 7:528], in0=T2[:, 7:528], in1=T2[:, 3:524], op=ALU.add), r=["T2"], w=["T1"])
                        P.op("pool", lambda e: e.tensor_tensor(out=T2[hi, 15:528], in0=T1[hi, 15:528], in1=T1[hi, 7:520], op=ALU.add), r=["T1"], w=["T2"])
                    for rows, src, sk in ((lo, T1, "T1"), (hi, T2, "T2")):
                        P.op("dve", lambda e, rows=rows, src=src, c=c, Uc=Uc: e.scalar_tensor_tensor(
                            out=PL[rows, c, :], in0=src[rows, 16:528], scalar=CF[rows, CF_IW + c:CF_IW + c + 1], in1=Uc[rows, 16:528],
                            op0=ALU.mult, op1=ALU.subtract), r=[sk, ("U", c), "CF"], w=[("PL", c)])
                        if t == 0:
                            P.op("dve", lambda e, rows=rows, src=src, c=c: e.tensor_tensor(out=TF[rows, :], in0=src[rows, 16:32],
                                                                                          in1=CF[rows, CF_TB + c * 16:CF_TB + (c + 1) * 16], op=ALU.mult),
                                 r=[sk, "CF"], w=["TF"])
                            P.op("dve", lambda e, rows=rows, c=c, Uc=Uc: e.tensor_tensor(out=PL[rows, c, 0:16], in0=TF[rows, :], in1=Uc[rows, 16:32], op=ALU.subtract),
                                 r=["TF", ("U", c)], w=[("PL", c)])
                    P.op("pool", lambda e, Uc=Uc: e.tensor_copy(out=Uc[:, 0:16], in_=Uc[:, 512:528]), r=["T1", "T2", ("PL", c)], w=[("U", c)])
                    P.op("pe", lambda e, c=c: e.matmul(msb[:], PWB[:, c, :], PL[:, c, :], start=True, stop=True), r=["PWB", ("PL", c)], w=["MS"])
                    P.op("dve", lambda e, c=c: e.scalar_tensor_tensor(out=YT[:, 2 + c, :], in0=msb[:], scalar=SPR[:, 12 + c:13 + c], in1=GS[:, c, :],
                                                                      op0=ALU.mult, op1=ALU.mult),
                         r=["MS", "SPR", ("GS", c)], w=[("YT", 2 + c)])

                P.cut(9)
                for c in range(2):
                    i = proj2(1280 + c * 128)
                    P.op("act", lambda e, i=i: e.activation(out=DH[:], in_=PJ[i][:], func=AF.Copy), r=[("PJ", i)], w=["DH"])
                    i = proj2(1792 + c * 128)
                    Zc = Z[:, c, :]
                    P.op("dve", lambda e, i=i, Zc=Zc: e.tensor_tensor(out=Zc[:, 2:514], in0=PJ[i][:], in1=DH[:], op=ALU.mult), r=[("PJ", i), "DH"], w=[("Z", c)])
                    wc = 14 + c * 3
                    P.op("dve", lambda e, Zc=Zc, wc=wc: e.tensor_scalar(out=ACC[:], in0=Zc[:, 2:514], scalar1=SPR[:, wc + 2:wc + 3], scalar2=None, op0=ALU.mult),
                         r=[("Z", c), "SPR"], w=["ACC"])
                    P.op("dve", lambda e, Zc=Zc, wc=wc: e.scalar_tensor_tensor(out=ACC[:], in0=Zc[:, 1:513], scalar=SPR[:, wc + 1:wc + 2], in1=ACC[:], op0=ALU.mult, op1=ALU.add),
                         r=[("Z", c), "SPR", "ACC"], w=["ACC"])
                    P.op("dve", lambda e, Zc=Zc, wc=wc: e.scalar_tensor_tensor(out=ACC[:], in0=Zc[:, 0:512], scalar=SPR[:, wc:wc + 1], in1=ACC[:], op0=ALU.mult, op1=ALU.add),
                         r=[("Z", c), "SPR", "ACC"], w=["ACC"])
                    P.op("pool", lambda e, Zc=Zc: e.tensor_copy(out=Zc[:, 0:2], in_=Zc[:, 512:514]), r=[("Z", c)], w=[("Z", c)])
                    i = proj2(1536 + c * 128)
                    P.op("dve", lambda e, i=i: e.tensor_tensor(out=ACC[:], in0=PJ[i][:], in1=ACC[:], op=ALU.mult), r=[("PJ", i), "ACC"], w=["ACC"])
                    P.op("pool", lambda e, c=c: e.tensor_tensor(out=YT[:, 6 + c, :], in0=ACC[:], in1=GS[:, 4 + c, :], op=ALU.mult),
                         r=["ACC", ("GS", 4 + c)], w=[("YT", 6 + c)])

                P.cut(10)
                i = proj2(768)
                ri = qk_norm_prep(PJ[i], ("PJ", i))
                for k in range(2):
                    rows = slice(k * 64, (k + 1) * 64)
                    P.op("dve", lambda e, i=i, ri=ri, k=k, rows=rows: e.scalar_tensor_tensor(
                        out=KC[k][0:64, 128:640], in0=PJ[i][rows, :], scalar=SPR[rows, 11:12], in1=RS[ri][rows, :], op0=ALU.mult, op1=ALU.mult),
                         r=[("PJ", i), ("RS", ri), "SPR"], w=[("KC", k)])
                for c in range(2):
                    i = proj2(512 + c * 128)
                    ri = qk_norm_prep(PJ[i], ("PJ", i))
                    for j in range(2):
                        h = 2 * c + j
                        rows = slice(j * 64, (j + 1) * 64)
                        P.op("dve", lambda e, i=i, ri=ri, h=h, rows=rows: e.scalar_tensor_tensor(
                            out=QC[h][0:64, :], in0=PJ[i][rows, :], scalar=SPR[rows, 10:11], in1=RS[ri][rows, :], op0=ALU.mult, op1=ALU.mult),
                             r=[("PJ", i), ("RS", ri), "SPR"], w=[("QC", h)])
                for sub in range(4):
                    i = nxt("pj", 2)
                    for kc in range(8):
                        P.op("pe", lambda e, kc=kc, i=i, sub=sub: e.matmul(PJ[i][:, 0:128], H2[s][:, kc, sub * 128:(sub + 1) * 128], W2[:, kc, 896:1024],
                                                                           start=(kc == 0), stop=(kc == 7)),
                             r=["W2", ("H2", s)], w=[("PJ", i)], sig=(kc == 7))
                    P.op("dve", lambda e, i=i, sub=sub: e.tensor_copy(out=VC[:, 1 + sub, :, 0:64], in_=PJ[i][:, 0:128].rearrange("p (k d) -> p k d", k=2)),
                         r=[("PJ", i)], w=["VC"])
                for h in range(4):
                    k = h // 2
                    for b in range(4):
                        g = 4 * t + b
                        lo = 0 if g > 0 else 128
                        pi = (h * 4 + b) % 4
                        qs = QC[h][0:65, b * 128:(b + 1) * 128]
                        P.op("pe", lambda e, h=h, lo=lo: e.matmul(SW[:, lo:256], ident, CB[:, CB_CMS + h * 256 + lo:CB_CMS + (h + 1) * 256], start=True, stop=False),
                             r=["CB"], w=["SW"], sig=False)
                        if g > 0:
                            P.op("pe", lambda e, k=k, b=b, qs=qs: e.matmul(SW[:, 0:128], KC[k][0:65, b * 128:(b + 1) * 128], qs, start=False, stop=False),
                                 r=[("KC", k), ("KCaug", k), ("QC", h), ("QCaug", h)], w=["SW"], sig=False)
                        P.op("pe", lambda e, k=k, b=b, qs=qs: e.matmul(SW[:, 128:256], KC[k][0:65, 128 + b * 128:128 + (b + 1) * 128], qs, start=False, stop=True),
                             r=[("KC", k), ("KCaug", k), ("QC", h), ("QCaug", h)], w=["SW"])
                        P.op("act", lambda e, pi=pi, lo=lo, h=h: e.activation(out=PS[pi][:, lo:256], in_=SW[:, lo:256], func=AF.Exp,
                                                                              bias=CF[:, CF_SB + h:CF_SB + h + 1], scale=0.125),
                             r=["SW", "CF"], w=[("PS", pi)])
                        oc = OC[:, b * 128:(b + 1) * 128]
                        if g > 0:
                            P.op("pe", lambda e, oc=oc, b=b, k=k, pi=pi: e.matmul(oc, VC[:, b, k, :], PS[pi][:, 0:128], start=True, stop=False),
                                 r=["VC", "VCones", ("PS", pi)], w=["OC"], sig=False)
                        P.op("pe", lambda e, oc=oc, b=b, k=k, pi=pi, g=g: e.matmul(oc, VC[:, 1 + b, k, :], PS[pi][:, 128:256], start=(g == 0), stop=True),
                             r=["VC", "VCones", ("PS", pi)], w=["OC"])
                    hr = slice((h % 2) * 64, (h % 2) * 64 + 64)
                    ci = nxt("rc", 2)
                    P.op("dve", lambda e, ci=ci, h=h, hr=hr: e.tensor_scalar(out=RC[ci][hr, :], in0=OC[64:128, :], scalar1=ESK[64:128, h:h + 1], scalar2=None, op0=ALU.add),
                         r=["OC", "ESK"], w=[("RC", ci)])
                    P.op("dve", lambda e, ci=ci, hr=hr: e.reciprocal(out=RC[ci][hr, :], in_=RC[ci][hr, :]), r=[("RC", ci)], w=[("RC", ci)])
                    P.op("dve", lambda e, ci=ci, hr=hr, h=h: e.tensor_tensor(out=RC[ci][hr, :], in0=RC[ci][hr, :], in1=GS[hr, 2 + h // 2, :], op=ALU.mult),
                         r=[("RC", ci), ("GS", 2 + h // 2)], w=[("RC", ci)])
                    P.op("dve", lambda e, ci=ci, hr=hr, h=h: e.tensor_tensor(out=YT[hr, 4 + h // 2, :], in0=OC[0:64, :], in1=RC[ci][hr, :], op=ALU.mult),
                         r=["OC", ("RC", ci)], w=[("YT", 4 + h // 2)])
                for k in range(2):
                    P.op("pool", lambda e, k=k: e.tensor_copy(out=KC[k][0:64, 0:128], in_=KC[k][0:64, 512:640]), r=[("KC", k)], w=[("KC", k)])
                P.op("pool", lambda e: e.tensor_copy(out=VC[:, 0, :, 0:64], in_=VC[:, 4, :, 0:64]), r=["VC"], w=["VC"])

                if t == 0 and l == 0:
                    dump("H20", H2[s][:, 0, :], [("H2", s)])
                    for c in range(8):
                        dump(f"YT{c}", YT[:, c, :], [("YT", c)])
                P.cut(11)
                for sub in range(4):
                    for hf in range(2):
                        oi = (sub * 2 + hf) % 2
                        for kc in range(8):
                            P.op("pe", lambda e, kc=kc, oi=oi, sub=sub, hf=hf: e.matmul(PO[oi][:], YT[:, kc, sub * 128:(sub + 1) * 128], WO[:, kc, hf * 512:(hf + 1) * 512],
                                                                                       start=(kc == 0), stop=(kc == 7)),
                                 r=["WO"] + [("YT", q) for q in range(8)], w=[("PO", oi)], sig=(kc == 7))
                        P.op("dve", lambda e, oi=oi, sub=sub, hf=hf: e.tensor_tensor(out=X2[s][:, sub, hf * 512:(hf + 1) * 512], in0=PO[oi][:],
                                                                                    in1=X2[s][:, sub, hf * 512:(hf + 1) * 512], op=ALU.add),
                             r=[("PO", oi), ("X2", s)], w=[("X2", s)])
                P.dma(lambda e, t=t, s=s: e.dma_start(out=xdst[t * TT:(t + 1) * TT, :].rearrange("(s p) d -> p s d", p=128), in_=X2[s][:]),
                      r=[("X2", s)], w=["xdst"], key=f"x2{s}")
            P.barrier()

        if DBG:
            P.stopped = False
            P.op("pool", lambda e: e.memset(DBGT[:, dbg_off[0]:12288], 0.0), w=["DBG"]) if dbg_off[0] < 12288 else None
            P.dma(lambda e: e.dma_start(out=dbg_d[:, :], in_=DBGT[:]), r=["DBG"], w=["dbg_d"], key="dbg")
            P.barrier()
        block = es.enter_context(nc.Block())
        P.emit(block)
    return nc


_CACHE = {}


def _host_layout(inputs, NL):
    f = lambda a: np.ascontiguousarray(np.asarray(a, dtype=np.float32))
    spr = np.zeros((NL, 128, NSPR), np.float32)
    pw = np.zeros((NL, 128, 2, 128), np.float32)
    for l in range(NL):
        spr[l, :, 0:8] = f(inputs["norm_g"])[l].reshape(8, 128).T
        spr[l, :, 8] = np.tile(f(inputs["a_q_norm"])[l], 2)
        spr[l, :, 9] = np.tile(f(inputs["a_k_norm"])[l], 2)
        spr[l, :, 10] = np.tile(f(inputs["c_q_norm"])[l], 2)
        spr[l, :, 11] = np.tile(f(inputs["c_k_norm"])[l], 2)
        spr[l, :, 12:14] = f(inputs["pool_scale"])[l].reshape(2, 128).T
        cw = f(inputs["conv_w"])[l]
        for c in range(2):
            for j in range(3):
                spr[l, :, 14 + c * 3 + j] = cw[j, c * 128:(c + 1) * 128]
        spr[l, :, 20:24] = f(inputs["c_sinks"])[l][None, :]
        pwl = f(inputs["pool_w"])[l]
        for c in range(2):
            for half in range(2):
                pw[l, half * 64:(half + 1) * 64, c, half * 64:(half + 1) * 64] = pwl[2 * c + half]
    return spr, pw


def run(inputs, S, NL, n_cores):
    key = (S, NL)
    if key not in _CACHE:
        _CACHE[key] = (build_program(S, NL), make_consts(S))
    nc, consts = _CACHE[key]
    spr, pw = _host_layout(inputs, NL)
    x = np.asarray(inputs["x"], dtype=np.float32)
    w_in = np.ascontiguousarray(np.asarray(inputs["w_in"], dtype=np.float32))
    w_out = np.ascontiguousarray(np.asarray(inputs["w_out"], dtype=np.float32))
    in_maps = []
    for c in range(n_cores):
        m = {"x": np.ascontiguousarray(x[c]), "w_in": w_in, "w_out": w_out, "spr": spr, "pw": pw}
        m.update(consts)
        in_maps.append(m)
    res = run_bass_kernel_spmd(nc, in_maps, core_ids=list(range(n_cores)))
    if "dbg" in res.results[0]:
        run.dbg = np.asarray(res.results[0]["dbg"]).astype(np.float32)
    return np.stack([np.asarray(r["out"], dtype=np.float32) for r in res.results], axis=0)


def kernel(x, norm_g, w_in, w_out, a_q_norm, a_k_norm, pool_w, pool_scale, c_q_norm, c_k_norm, c_sinks, conv_w):
    inputs = dict(x=x, norm_g=norm_g, w_in=w_in, w_out=w_out, a_q_norm=a_q_norm, a_k_norm=a_k_norm, pool_w=pool_w,
                  pool_scale=pool_scale, c_q_norm=c_q_norm, c_k_norm=c_k_norm, c_sinks=c_sinks, conv_w=conv_w)
    x = np.asarray(x)
    return run(inputs, x.shape[1], np.asarray(w_in).shape[0], x.shape[0])
```

```python
import contextlib
import types
import numpy as np
import ml_dtypes
import concourse.bass as bass
import concourse.mybir as mybir
from concourse.bass_utils import run_bass_kernel_spmd

F32 = mybir.dt.float32
BF16 = mybir.dt.bfloat16
AF = mybir.ActivationFunctionType
ALU = mybir.AluOpType

D = 1024
NIN = 3328
TT = 512
BIG = 30000.0
EPS = 1e-6
NSPR = 24
ENGS = ("pe", "act", "dve", "pool", "sp")
BLK = {"pe": "tensor", "act": "scalar", "dve": "vector", "pool": "gpsimd", "sp": "sync"}

CB_ID, CB_OB, CB_CM, CB_CMS, CB_N = 0, 128, 256, 256 + 2048, 256 + 2048 + 1024
CF_AB, CF_SB, CF_IW, CF_TB, CF_N = 0, 256, 260, 262, 262 + 32


def slopes_all():
    return np.exp2(-(8.0 / 8) * np.arange(1, 9, dtype=np.float32)).astype(np.float32)


def make_consts(S):
    sl = slopes_all()
    sl_c, sl_a = sl[:4], sl[4:]
    cb = np.zeros((128, CB_N), np.float32)
    cb[:, CB_ID:CB_ID + 128] = np.eye(128)
    cb[0:64, CB_OB:CB_OB + 64] = 1.0
    cb[64:128, CB_OB + 64:CB_OB + 128] = 1.0
    kl = np.arange(128)[:, None]
    ql = np.arange(512)[None, :]
    for kt in range(4):
        m = ((ql // 256) == (kt // 2)) & (ql < 128 * kt + kl)
        cb[:, CB_CM + kt * 512:CB_CM + (kt + 1) * 512] = np.where(m, -BIG, 0.0)
    q1 = np.arange(128)[None, :]
    for h in range(4):
        prev = np.where(kl > q1, -8.0 * sl_c[h] * 128.0, -BIG)
        own = np.where(kl <= q1, 0.0, -BIG)
        cb[:, CB_CMS + h * 256:CB_CMS + h * 256 + 128] = prev
        cb[:, CB_CMS + h * 256 + 128:CB_CMS + (h + 1) * 256] = own
    cf = np.zeros((128, CF_N), np.float32)
    for h in range(4):
        for i in range(64):
            d = 3 - i
            cf[:, CF_AB + h * 64 + i] = sl_a[h] * (np.arange(128) - 511 + 128 * d)
        cf[:, CF_SB + h] = sl_c[h] * np.arange(128)
    wins = (2, 4, 8, 16)
    for c in range(2):
        for half in range(2):
            w = wins[2 * c + half]
            rows = slice(half * 64, (half + 1) * 64)
            cf[rows, CF_IW + c] = 1.0 / w
            cf[rows, CF_TB + c * 16:CF_TB + (c + 1) * 16] = 1.0 / np.minimum(np.arange(16) + 1, w)
    khot = np.zeros((32, S), np.float32)
    for n in range(S // 256):
        khot[n, n * 256:(n + 1) * 256] = 1.0
    cq = np.zeros((4, 512), np.float32)
    for h in range(4):
        cq[h] = -8.0 * sl_c[h] * (np.arange(512) % 128)
    bf = ml_dtypes.bfloat16
    return {"cb": cb.astype(bf), "cf": cf, "khot": khot.astype(bf), "cqrow": cq.astype(bf)}


def _freeze(fn):
    if fn is None or fn.__closure__ is None:
        return fn
    cells = []
    for c in fn.__closure__:
        try:
            cells.append(types.CellType(c.cell_contents))
        except ValueError:
            cells.append(c)
    g = types.FunctionType(fn.__code__, fn.__globals__, fn.__name__, fn.__defaults__, tuple(cells))
    g.__kwdefaults__ = fn.__kwdefaults__
    return g


class Prog:
    def __init__(self, nc, es):
        self.nc = nc
        self.es = es
        self.ops = {e: [] for e in ENGS}
        self.cnt = {e: 0 for e in ENGS}
        self.semh = {}
        self.dcnt = {}
        self.lastw = {}
        self.readers = {}
        self.waited = {e: {} for e in ENGS}
        for e in ENGS:
            self.semh["E:" + e] = es.enter_context(nc.semaphore("sem_" + e))

    def _collect(self, eng, r, w):
        need = {}

        def add(ev):
            semid, val, src = ev
            if src == eng and eng == "pe":
                return
            if self.waited[eng].get(semid, 0) >= val:
                return
            if need.get(semid, 0) < val:
                need[semid] = val

        for k in r:
            if k in self.lastw:
                add(self.lastw[k])
        for k in w:
            if k in self.lastw:
                add(self.lastw[k])
            for ev in self.readers.get(k, {}).values():
                add(ev)
        for semid, val in need.items():
            self.waited[eng][semid] = val
        return list(need.items())

    def _commit(self, ev, r, w):
        for k in r:
            d = self.readers.setdefault(k, {})
            if ev[0] not in d or d[ev[0]][1] < ev[1]:
                d[ev[0]] = ev
        for k in w:
            self.lastw[k] = ev
            self.readers[k] = {}

    stopped = False

    def cut(self, k):
        import os
        if int(os.environ.get("STAGE", "99")) == k and not self.stopped:
            self.barrier()
            self.stopped = True

    def op(self, eng, fn, r=(), w=(), sig=True):
        if self.stopped:
            return
        fn = _freeze(fn)
        waits = self._collect(eng, r, w)
        semid = "E:" + eng
        if sig:
            self.cnt[eng] += 1
            ev = (semid, self.cnt[eng], eng)
            inc = (semid, 1)
        else:
            ev = (semid, self.cnt[eng] + 1, eng)
            inc = None
        self.ops[eng].append((waits, fn, inc))
        self._commit(ev, r, w)

    def dma(self, fn, r=(), w=(), key=None, q="sp"):
        if self.stopped:
            return
        fn = _freeze(fn)
        waits = self._collect(q, r, w)
        semid = "D:" + key
        if semid not in self.semh:
            self.semh[semid] = self.es.enter_context(self.nc.semaphore("dsem_" + key))
            self.dcnt[semid] = 0
        self.dcnt[semid] += 16
        ev = (semid, self.dcnt[semid], "dma")
        self.ops[q].append((waits, fn, (semid, 16)))
        self._commit(ev, r, w)

    def dma_group(self, key, items, q="sp"):
        if self.stopped:
            return
        semid = "D:" + key
        if semid not in self.semh:
            self.semh[semid] = self.es.enter_context(self.nc.semaphore("dsem_" + key))
            self.dcnt[semid] = 0
        total = self.dcnt[semid] + 16 * len(items)
        ev = (semid, total, "dma")
        for fn, r, w in items:
            waits = self._collect(q, r, w)
            self.ops[q].append((waits, _freeze(fn), (semid, 16)))
        for fn, r, w in items:
            self._commit(ev, r, w)
        self.dcnt[semid] = total

    def barrier(self):
        if self.stopped:
            return
        for e in ENGS:
            waits = []
            for o in ENGS:
                if o == e or self.cnt[o] == 0:
                    continue
                sid = "E:" + o
                if self.waited[e].get(sid, 0) < self.cnt[o]:
                    waits.append((sid, self.cnt[o]))
                    self.waited[e][sid] = self.cnt[o]
            for sid, c in self.dcnt.items():
                if self.waited[e].get(sid, 0) < c:
                    waits.append((sid, c))
                    self.waited[e][sid] = c
            if waits:
                self.ops[e].append((waits, None, None))
        self.lastw = {}
        self.readers = {}

    def emit(self, block):
        for eng in ENGS:
            def body(e, eng=eng):
                for waits, fn, inc in self.ops[eng]:
                    for semid, val in waits:
                        e.wait_ge(self.semh[semid], val)
                    if fn is None:
                        continue
                    ins = fn(e)
                    if inc is not None:
                        ins.then_inc(self.semh[inc[0]], inc[1])
            getattr(block, BLK[eng])(body)


class Arena:
    def __init__(self, nc, base, top):
        self.nc, self.base, self.top, self.cur, self.n = nc, base, top, base, 0

    def alloc(self, name, shape, dtype):
        nbytes = int(np.prod(shape[1:])) * (2 if dtype == BF16 else 4)
        nbytes = (nbytes + 63) // 64 * 64
        off = self.cur
        assert off + nbytes <= self.top, f"SBUF overflow at {name}: {off + nbytes} > {self.top}"
        self.cur += nbytes
        self.n += 1
        return self.nc.alloc_sbuf_tensor_at(f"{name}_{self.n}", list(shape), dtype, offset=off)


def build_program(S, NL):
    NT = S // TT
    NKT = S // 128
    nc = bass.Bass("TRN2", target_bir_lowering=False)
    dt = nc.dram_tensor
    x_in = dt("x", [S, D], F32, kind="ExternalInput").ap()
    w_in = dt("w_in", [NL, D, NIN], F32, kind="ExternalInput").ap()
    w_out = dt("w_out", [NL, D, D], F32, kind="ExternalInput").ap()
    spr_d = dt("spr", [NL, 128, NSPR], F32, kind="ExternalInput").ap()
    pw_d = dt("pw", [NL, 128, 2, 128], F32, kind="ExternalInput").ap()
    cb_d = dt("cb", [128, CB_N], BF16, kind="ExternalInput").ap()
    cf_d = dt("cf", [128, CF_N], F32, kind="ExternalInput").ap()
    khot_d = dt("khot", [32, S], BF16, kind="ExternalInput").ap()
    cq_d = dt("cqrow", [4, 512], BF16, kind="ExternalInput").ap()
    out_d = dt("out", [S, D], F32, kind="ExternalOutput").ap()
    hT_d = dt("hT_scr", [D, S], BF16, kind="Internal").ap()
    ya_d = dt("ya_scr", [256, S], BF16, kind="Internal").ap()
    x1_d = dt("x1_scr", [S, D], F32, kind="Internal").ap() if NL > 1 else None

    with contextlib.ExitStack() as es:
        P = Prog(nc, es)
        a_base = (int(nc.sbuf_base) + 63) // 64 * 64
        a_size = int(nc.sbuf_top) - a_base - 2048
        es.enter_context(nc.sbuf_tensor("arena_slab", [128, a_size], mybir.dt.uint8))
        A = Arena(nc, a_base, a_base + a_size)
        pb = [es.enter_context(nc.psum_tensor(f"pb{i}", [128, 512], F32)) for i in range(6)]
        tpb = es.enter_context(nc.psum_tensor("tpb", [128, 512], F32))
        msb = es.enter_context(nc.psum_tensor("msb", [128, 512], F32))
        CB = A.alloc("CB", [128, CB_N], BF16)
        CF = A.alloc("CF", [128, CF_N], F32)
        SPR = A.alloc("SPR", [128, NSPR], F32)
        ESK = A.alloc("ESK", [128, 4], F32)
        SQ = [A.alloc(f"SQ{i}", [128, 512], BF16) for i in range(2)]
        RS = [A.alloc(f"RS{i}", [128, 512], F32) for i in range(2)]
        RC = [A.alloc(f"RC{i}", [128, 512], F32) for i in range(2)]
        ident = CB[:, CB_ID:CB_ID + 128]
        onesblk = CB[:, CB_OB:CB_OB + 128]
        import os
        DBG = os.environ.get("DBG") == "1"
        dbg_off = [0]
        dbg_names = []
        if DBG:
            dbg_d = dt("dbg", [128, 12288], BF16, kind="ExternalOutput").ap()
            DBGT = A.alloc("DBGT", [128, 12288], BF16)

        def dump(name, ap, r, npart=128):
            if not DBG or P.stopped:
                return
            n = ap.shape[-1] if len(ap.shape) == 2 else int(np.prod(ap.shape[1:]))
            o = dbg_off[0]
            dbg_names.append((name, o, n, npart))
            dbg_off[0] += n
            assert dbg_off[0] <= 12288
            P.op("dve", lambda e: e.tensor_copy(out=DBGT[0:npart, o:o + n], in_=ap), r=r, w=["DBG"])
        build_program.dbg_names = dbg_names
        mark = A.cur

        first_items = [(lambda e: e.dma_start(out=CB[:], in_=cb_d[:, :]), [], ["CB"]),
                       (lambda e: e.dma_start(out=CF[:], in_=cf_d[:, :]), [], ["CF"])]

        rr = {"sq": 0, "pj": 0, "rc": 0}

        def nxt(name, n):
            rr[name] = (rr[name] + 1) % n
            return rr[name]

        def qk_norm_prep(pj, pjk):
            i = nxt("sq", 2)
            P.op("act", lambda e: e.activation(out=SQ[i][:], in_=pj[:], func=AF.Square), r=[pjk], w=[("SQ", i)])
            P.op("pe", lambda e: e.matmul(msb[:], onesblk, SQ[i][:], start=True, stop=True),
                 r=[("SQ", i), "CB"], w=["MS"])
            P.op("act", lambda e: e.activation(out=RS[i][:], in_=msb[:], func=AF.Ln, bias=EPS, scale=1.0 / 64),
                 r=["MS"], w=[("RS", i)])
            P.op("act", lambda e: e.activation(out=RS[i][:], in_=RS[i][:], func=AF.Exp, scale=-0.5),
                 r=[("RS", i)], w=[("RS", i)])
            return i

        for l in range(NL):
            xsrc = x_in if l == 0 else x1_d
            xdst = out_d if l == NL - 1 else x1_d
            items = first_items if l == 0 else []
            items.append((lambda e, l=l: e.dma_start(out=SPR[:], in_=spr_d[l, :, :]), [], ["SPR"]))

            A.cur = mark
            WA = A.alloc("WA", [128, 8, 1024], BF16)
            KA = [A.alloc(f"KA{h}", [96, S], BF16) for h in range(4)]
            VA = A.alloc("VA", [128, NKT, 384], BF16)
            KM = A.alloc("KM", [128, 2, 64], F32)
            wbase = A.cur
            XT = A.alloc("XT", [128, 4, 1024], F32)
            XS = A.alloc("XS", [128, 4, 1024], BF16)
            WSTG = [nc.alloc_sbuf_tensor_at(f"WSTGa{l}_{i}", [128, 2304], F32, offset=wbase + i * 9216) for i in range(2)]
            HT = [A.alloc(f"HT{i}", [128, 8, 512], BF16) for i in range(2)]
            QF = A.alloc("QF", [128, 2, 512], F32)
            QA = [[A.alloc(f"QA{h}_{s}", [96, 512], BF16) for s in range(1)] * 2 for h in range(4)]
            GA = [A.alloc("GA", [128, 2, 512], BF16)] * 2
            PT = [A.alloc(f"PT{i}", [128, 512], BF16) for i in range(4)]
            YA = [A.alloc("YA", [128, 2, 512], BF16)] * 2
            SSX = A.alloc("SSX", [128, 4], F32)
            RSX = A.alloc("RSX", [128, 4], F32)
            BSM = A.alloc("BSM", [128, 4, 32], F32)
            M8 = A.alloc("M8", [128, 4, 8], F32)
            THR = A.alloc("THR", [128, 4], F32)
            MB = A.alloc("MB", [128, 4, 32], BF16)
            PJ = pb[0:2]
            ST = pb[2:4]
            OT = pb[4:6]

            for h in range(4):
                items.append((lambda e, h=h: e.dma_start(out=KA[h][64:96, :], in_=khot_d[:, :]), [], [("KAaug", h)]))
            P.dma_group("const", items)
            P.op("act", lambda e: e.activation(out=ESK[:], in_=SPR[:, 20:24], func=AF.Exp), r=["SPR"], w=["ESK"])
            P.op("pool", lambda e: e.memset(VA[:, :, 64:128], 1.0), w=["VAones"])
            P.op("pool", lambda e: e.memset(VA[:, :, 256:320], 1.0), w=["VAones"])
            P.op("pool", lambda e: e.memset(KM[:], 0.0), w=["KM"])
            for kc in range(8):
                i = kc % 2
                P.dma(lambda e, kc=kc, i=i, l=l: e.dma_start(out=WSTG[i][:, 0:1024], in_=w_in[l, kc * 128:(kc + 1) * 128, 0:1024]),
                      w=[("WSTG", i)], key=f"w{i}")
                P.op("dve", lambda e, kc=kc, i=i: e.tensor_scalar(out=WA[:, kc, :], in0=WSTG[i][:, 0:1024], scalar1=SPR[:, kc:kc + 1],
                                                                  scalar2=None, op0=ALU.mult),
                     r=[("WSTG", i), "SPR"], w=["WA", "XTa", "XS"])

            def load_x(t):
                P.dma(lambda e, t=t: e.dma_start(out=XT[:], in_=xsrc[t * TT:(t + 1) * TT, :].rearrange("(s p) d -> p s d", p=128)),
                      r=["XTa"], w=["XT"], key="xt")

            P.cut(1)
            load_x(0)
            def proj(c0, s):
                i = nxt("pj", 2)
                for kc in range(8):
                    P.op("pe", lambda e, kc=kc, i=i: e.matmul(PJ[i][:], WA[:, kc, c0:c0 + 128], HT[s][:, kc, :], start=(kc == 0), stop=(kc == 7)),
                         r=["WA", ("HT", s)], w=[("PJ", i)], sig=(kc == 7))
                return i

            def stage1(t):
                s = t % 2
                for sub in range(4):
                    P.op("act", lambda e, sub=sub: e.activation(out=XS[:, sub, :], in_=XT[:, sub, :], func=AF.Square,
                                                                accum_out=SSX[:, sub:sub + 1]),
                         r=["XT"], w=["XS", "SSX"])
                P.op("act", lambda e: e.activation(out=RSX[:], in_=SSX[:], func=AF.Ln, bias=EPS, scale=1.0 / D), r=["SSX"], w=["RSX"])
                P.op("act", lambda e: e.activation(out=RSX[:], in_=RSX[:], func=AF.Exp, scale=-0.5), r=["RSX"], w=["RSX"])
                for sub in range(4):
                    P.op("dve", lambda e, sub=sub: e.tensor_scalar(out=XS[:, sub, :], in0=XT[:, sub, :], scalar1=RSX[:, sub:sub + 1],
                                                                   scalar2=None, op0=ALU.mult),
                         r=["XT", "RSX"], w=["XS"])
                if t + 1 < NT:
                    load_x(t + 1)
                for kc in range(8):
                    hf = kc % 2
                    bank, bkey = (tpb, ("TP", 0)) if hf == 0 else (msb, "MS")
                    for sub in range(4):
                        P.op("pe", lambda e, kc=kc, sub=sub, bank=bank: e.matmul(bank[:, sub * 128:(sub + 1) * 128], XS[:, sub, kc * 128:(kc + 1) * 128], ident,
                                                                                 start=True, stop=True),
                             r=["XS", "CB"], w=[bkey], sig=(sub == 3))
                    P.op("dve", lambda e, kc=kc, bank=bank: e.tensor_copy(out=HT[s][:, kc, :], in_=bank[:, 0:512]),
                         r=[bkey], w=[("HT", s)])
                P.dma(lambda e, t=t, s=s: e.dma_start(out=hT_d[:, t * TT:(t + 1) * TT].rearrange("(kc p) n -> p kc n", p=128), in_=HT[s][:]),
                      r=[("HT", s)], w=["hT_d"], key=f"hts{s}")


                for c in range(2):
                    i = proj(256 + c * 128, s)
                    ri = qk_norm_prep(PJ[i], ("PJ", i))
                    for j in range(2):
                        h = 2 * c + j
                        rows = slice(j * 64, (j + 1) * 64)
                        for b in range(2):
                            n = 2 * t + b
                            P.op("dve", lambda e, i=i, ri=ri, h=h, rows=rows, b=b, n=n: e.scalar_tensor_tensor(
                                out=KA[h][0:64, t * TT + b * 256:t * TT + (b + 1) * 256], in0=PJ[i][rows, b * 256:(b + 1) * 256],
                                scalar=SPR[rows, 9:10], in1=RS[ri][rows, b * 256:(b + 1) * 256], op0=ALU.mult, op1=ALU.mult,
                                accum_out=KM[rows, c, j * 32 + n:j * 32 + n + 1]),
                                 r=[("PJ", i), ("RS", ri), "SPR"], w=[("KA", h, t), "KM"])
                for sub in range(4):
                    i = nxt("pj", 2)
                    for kc in range(8):
                        P.op("pe", lambda e, kc=kc, i=i, sub=sub: e.matmul(PJ[i][:, 0:256], HT[s][:, kc, sub * 128:(sub + 1) * 128], WA[:, kc, 512:768],
                                                                           start=(kc == 0), stop=(kc == 7)),
                             r=["WA", ("HT", s)], w=[("PJ", i)], sig=(kc == 7))
                    kt = 4 * t + sub
                    for (d0, s0, wd) in ((0, 0, 64), (128, 64, 128), (320, 192, 64)):
                        P.op("dve", lambda e, i=i, kt=kt, d0=d0, s0=s0, wd=wd: e.tensor_copy(out=VA[:, kt, d0:d0 + wd], in_=PJ[i][:, s0:s0 + wd]),
                             r=[("PJ", i)], w=[("VA", t)])

            def stage2(t):
                s = t % 2
                for c in range(2):
                    i = proj(c * 128, s)
                    ri = qk_norm_prep(PJ[i], ("PJ", i))
                    P.op("dve", lambda e, i=i, ri=ri, c=c: e.scalar_tensor_tensor(
                        out=QF[:, c, :], in0=PJ[i][:], scalar=SPR[:, 8:9], in1=RS[ri][:], op0=ALU.mult, op1=ALU.mult),
                         r=[("PJ", i), ("RS", ri), "SPR"], w=[("QF", c)])
                    P.op("pool", lambda e, c=c: e.tensor_copy(out=QA[2 * c][0][0:64, :], in_=QF[0:64, c, :]),
                         r=[("QF", c)], w=[("QA", 2 * c, 0)])
                    P.op("dve", lambda e, c=c: e.tensor_copy(out=QA[2 * c + 1][0][0:64, :], in_=QF[64:128, c, :]),
                         r=[("QF", c)], w=[("QA", 2 * c + 1, 0)])
                for c in range(2):
                    i = proj(768 + c * 128, s)
                    P.op("act", lambda e, i=i, c=c: e.activation(out=GA[0][:, c, :], in_=PJ[i][:], func=AF.Silu),
                         r=[("PJ", i)], w=[("GA", 0, c)])
                for sub in range(4):
                    own = 2 * t + sub // 2
                    for c in range(2):
                        P.op("pe", lambda e, c=c, sub=sub: e.matmul(msb[:, c * 64:(c + 1) * 64], QF[:, c, sub * 128:(sub + 1) * 128], KM[:, c, :],
                                                                    start=True, stop=True),
                             r=[("QF", c), "KM"], w=["MS"], sig=(c == 1))
                    P.op("pool", lambda e: e.memset(BSM[:], -1e30), w=["BSM"])
                    if own > 0:
                        P.op("dve", lambda e, own=own: e.tensor_copy(out=BSM[:, :, 0:own],
                                                                     in_=msb[:, 0:128].rearrange("p (h n) -> p h n", h=4)[:, :, 0:own]),
                             r=["MS"], w=["BSM"])
                    for h in range(4):
                        P.op("dve", lambda e, h=h: e.max(out=M8[:, h, :], in_=BSM[:, h, :]), r=["BSM"], w=["M8"])
                    P.op("dve", lambda e: e.tensor_scalar(out=THR[:], in0=M8[:, :, 2], scalar1=-1e29, scalar2=None, op0=ALU.max),
                         r=["M8"], w=["THR"])
                    for h in range(4):
                        P.op("dve", lambda e, h=h: e.tensor_scalar(out=MB[:, h, :], in0=BSM[:, h, :], scalar1=THR[:, h:h + 1], scalar2=-BIG,
                                                                   op0=ALU.is_lt, op1=ALU.mult),
                             r=["BSM", "THR"], w=["MB"])
                    P.op("dve", lambda e, own=own: e.memset(MB[:, :, own:own + 1], 0.0), w=["MB"])
                    P.op("pe", lambda e: e.matmul(tpb[:, 0:128], MB[:].rearrange("p h n -> p (h n)"), ident, start=True, stop=True), r=["MB", "CB"], w=[("TP", 0)])
                    for h in range(4):
                        P.op("dve", lambda e, h=h, sub=sub: e.tensor_copy(out=QA[h][0][64:96, sub * 128:(sub + 1) * 128], in_=tpb[h * 32:(h + 1) * 32, 0:128]),
                             r=[("TP", 0)], w=[("QAm", h, 0)])


            def moba(t, hook):
                s = t % 2
                for h in range(4):
                    oslot = h % 2
                    nkt = 4 * t + 4
                    vc0 = (0, 64, 192, 256)[h]
                    pend = None
                    for kt in range(nkt):
                        si = kt % 2
                        diag = kt >= 4 * t
                        P.op("pe", lambda e, h=h, kt=kt, si=si, diag=diag: e.matmul(ST[si][:], KA[h][0:96, kt * 128:(kt + 1) * 128], QA[h][0][0:96, :],
                                                                                    start=True, stop=not diag),
                             r=[("KA", h, kt // 4), ("KAaug", h), ("QA", h, 0), ("QAm", h, 0)], w=[("ST", si)], sig=not diag)
                        if diag:
                            dk = kt - 4 * t
                            P.op("pe", lambda e, si=si, dk=dk: e.matmul(ST[si][:], ident, CB[:, CB_CM + dk * 512:CB_CM + (dk + 1) * 512], start=False, stop=True),
                                 r=["CB"], w=[("ST", si)])
                        if pend is not None:
                            pend()
                        pi = kt % 4
                        col = CF_AB + h * 64 + (3 - (kt - 4 * t))
                        P.op("act", lambda e, si=si, pi=pi, col=col: e.activation(out=PT[pi][:], in_=ST[si][:], func=AF.Exp, bias=CF[:, col:col + 1], scale=0.125),
                             r=[("ST", si), "CF"], w=[("PT", pi)])

                        def pv(h=h, kt=kt, pi=pi, oslot=oslot, vc0=vc0, nkt=nkt):
                            P.op("pe", lambda e: e.matmul(OT[oslot][:], VA[:, kt, vc0:vc0 + 128], PT[pi][:], start=(kt == 0), stop=(kt == nkt - 1)),
                                 r=[("VA", kt // 4), "VAones", ("PT", pi)], w=[("OT", oslot)], sig=True)
                        pend = pv
                    pend()
                    nr = slice(0, 64) if h % 2 == 0 else slice(64, 128)
                    dr = slice(64, 128) if h % 2 == 0 else slice(0, 64)
                    hr = slice((h % 2) * 64, (h % 2) * 64 + 64)
                    ci = nxt("rc", 2)
                    P.op("dve", lambda e, oslot=oslot, dr=dr, ci=ci, hr=hr: e.reciprocal(out=RC[ci][hr, :], in_=OT[oslot][dr, :]), r=[("OT", oslot)], w=[("RC", ci)])
                    P.op("dve", lambda e, ci=ci, hr=hr, h=h: e.tensor_tensor(out=RC[ci][hr, :], in0=RC[ci][hr, :], in1=GA[0][hr, h // 2, :], op=ALU.mult),
                         r=[("RC", ci), ("GA", 0, h // 2)], w=[("RC", ci)])
                    P.op("dve", lambda e, oslot=oslot, nr=nr, ci=ci, hr=hr, h=h: e.tensor_tensor(out=YA[0][hr, h // 2, :], in0=OT[oslot][nr, :], in1=RC[ci][hr, :], op=ALU.mult),
                         r=[("OT", oslot), ("RC", ci)], w=[("YA", 0)])
                    hook(h)
                P.dma(lambda e, t=t, s=s: e.dma_start(out=ya_d[:, t * TT:(t + 1) * TT].rearrange("(c p) n -> p c n", p=128), in_=YA[0][:]),
                      r=[("YA", 0)], w=["ya_d"], key="yas")

            stage1(0)
            stage2(0)
            for t in range(NT):
                def hook(h, t=t):
                    if h == 0 and t + 1 < NT:
                        stage1(t + 1)
                moba(t, hook)
                if t + 1 < NT:
                    stage2(t + 1)
            P.barrier()
            P.cut(6)

            A.cur = mark
            W2 = A.alloc("W2", [128, 8, 2304], BF16)
            WO = A.alloc("WO", [128, 8, 1024], BF16)
            PWB = A.alloc("PWB", [128, 2, 128], BF16)
            H2 = [A.alloc(f"H2{i}", [128, 8, 512], BF16) for i in range(2)]
            wbase = A.cur
            X2 = [A.alloc(f"X2{i}", [128, 4, 1024], F32) for i in range(2)]
            WSTG = [nc.alloc_sbuf_tensor_at(f"WSTGb{l}_{i}", [128, 2304], F32, offset=wbase + i * 16384) for i in range(2)]
            YT = A.alloc("YT", [128, 8, 512], BF16)
            GS = A.alloc("GS", [128, 6, 512], BF16)
            U = A.alloc("U", [128, 2, 528], F32)
            T1 = A.alloc("T1", [128, 528], F32)
            T2 = A.alloc("T2", [128, 528], F32)
            TF = A.alloc("TF", [128, 16], F32)
            PL = A.alloc("PL", [128, 2, 512], BF16)
            DH = A.alloc("DH", [128, 512], F32)
            Z = A.alloc("Z", [128, 2, 514], F32)
            ACC = A.alloc("ACC", [128, 512], F32)
            QC = [A.alloc(f"QC{h}", [65, 512], BF16) for h in range(4)]
            KC = [A.alloc(f"KC{k}", [65, 640], BF16) for k in range(2)]
            VC = A.alloc("VC", [128, 5, 2, 128], BF16)
            PS = [A.alloc(f"PS{i}", [128, 256], BF16) for i in range(4)]
            PJ = pb[0:2]
            SW = pb[2]
            OC = pb[3]
            PO = pb[4:6]

            P.op("pool", lambda e: e.memset(U[:], 0.0), w=[("U", 0), ("U", 1)])
            P.op("pool", lambda e: e.memset(Z[:], 0.0), w=[("Z", 0), ("Z", 1)])
            P.op("pool", lambda e: e.memset(VC[:, :, :, 64:128], 1.0), w=["VCones"])
            P.op("pool", lambda e: e.memset(VC[:, 0, :, 0:64], 0.0), w=["VC"])
            for k in range(2):
                P.op("pool", lambda e, k=k: e.memset(KC[k][64:65, :], 1.0), w=[("KCaug", k)])
                P.op("pool", lambda e, k=k: e.memset(KC[k][0:64, 0:128], 0.0), w=[("KC", k)])
            P.dma_group("const2", [(lambda e, h=h: e.dma_start(out=QC[h][64:65, :], in_=cq_d[h:h + 1, :]), [], [("QCaug", h)]) for h in range(4)])
            for kc in range(8):
                i = kc % 2
                P.dma(lambda e, kc=kc, i=i, l=l: e.dma_start(out=WSTG[i][:], in_=w_in[l, kc * 128:(kc + 1) * 128, 1024:3328]),
                      w=[("X2", i)], key=f"w{i}")
                P.op("dve", lambda e, kc=kc, i=i: e.tensor_scalar(out=W2[:, kc, :], in0=WSTG[i][:], scalar1=SPR[:, kc:kc + 1], scalar2=None, op0=ALU.mult),
                     r=[("X2", i), "SPR"], w=["W2"])
            for kc in range(8):
                i = kc % 2
                P.dma(lambda e, kc=kc, i=i, l=l: e.dma_start(out=WSTG[i][:, 0:1024], in_=w_out[l, kc * 128:(kc + 1) * 128, :]),
                      w=[("X2", i)], key=f"w{i}")
                P.op("pool", lambda e, kc=kc, i=i: e.tensor_copy(out=WO[:, kc, :], in_=WSTG[i][:, 0:1024]), r=[("X2", i)], w=["WO"])
            P.dma(lambda e, l=l: e.dma_start(out=WSTG[0][:, 0:256], in_=pw_d[l].rearrange("p c n -> p (c n)")), w=[("X2", 0)], key="w0")
            P.op("pool", lambda e: e.tensor_copy(out=PWB[:].rearrange("p c n -> p (c n)"), in_=WSTG[0][:, 0:256]), r=[("X2", 0)], w=["PWB"])

            def load2(t):
                s = t % 2
                P.dma(lambda e, t=t, s=s: e.dma_start(out=H2[s][:], in_=hT_d[:, t * TT:(t + 1) * TT].rearrange("(kc p) n -> p kc n", p=128)),
                      r=["hT_d"], w=[("H2", s)], key=f"h2{s}")
                P.dma(lambda e, t=t, s=s: e.dma_start(out=X2[s][:], in_=xsrc[t * TT:(t + 1) * TT, :].rearrange("(s p) d -> p s d", p=128)),
                      w=[("X2", s)], key=f"x2{s}")

            P.cut(7)
            load2(0)
            for t in range(NT):
                s = t % 2
                if t + 1 < NT:
                    load2(t + 1)
                P.dma(lambda e, t=t: e.dma_start(out=YT[:, 0:2, :], in_=ya_d[:, t * TT:(t + 1) * TT].rearrange("(c p) n -> p c n", p=128)),
                      r=["ya_d"], w=[("YT", 0), ("YT", 1)], key="yal")

                def proj2(c0):
                    i = nxt("pj", 2)
                    for kc in range(8):
                        P.op("pe", lambda e, kc=kc, i=i: e.matmul(PJ[i][:], W2[:, kc, c0:c0 + 128], H2[s][:, kc, :], start=(kc == 0), stop=(kc == 7)),
                             r=["W2", ("H2", s)], w=[("PJ", i)], sig=(kc == 7))
                    return i

                for gi, c0 in enumerate((256, 384, 1024, 1152, 2048, 2176)):
                    i = proj2(c0)
                    P.op("act", lambda e, i=i, gi=gi: e.activation(out=GS[:, gi, :], in_=PJ[i][:], func=AF.Silu), r=[("PJ", i)], w=[("GS", gi)])

                P.cut(8)
                for c in range(2):
                    i = proj2(c * 128)
                    P.op("act", lambda e, i=i, c=c: e.activation(out=U[:, c, 16:528], in_=PJ[i][:], func=AF.Copy), r=[("PJ", i)], w=[("U", c)])
                    Uc = U[:, c, :]
                    P.op("pool", lambda e, Uc=Uc: e.tensor_tensor(out=T1[:, 1:528], in0=Uc[:, 1:528], in1=Uc[:, 0:527], op=ALU.add), r=[("U", c)], w=["T1"])
                    lo, hi = slice(0, 64), slice(64, 128)
                    if c == 0:
                        P.op("pool", lambda e: e.tensor_tensor(out=T2[hi, 3:528], in0=T1[hi, 3:528], in1=T1[hi, 1:526], op=ALU.add), r=["T1"], w=["T2"])
                    else:
                        P.op("pool", lambda e: e.tensor_tensor(out=T2[:, 3:528], in0=T1[:, 3:528], in1=T1[:, 1:526], op=ALU.add), r=["T1"], w=["T2"])
                        P.op("pool", lambda e: e.tensor_tensor(out=T1[:, 7:528], in0=T2[:, 7:528], in1=T2[:, 3:524], op=ALU.add), r=["T2"], w=["T1"])
                        P.op("pool", lambda e: e.tensor_tensor(out=T2[hi, 15:528], in0=T1[hi, 15:528], in1=T1[hi, 7:520], op=ALU.add), r=["T1"], w=["T2"])
                    for rows, src, sk in ((lo, T1, "T1"), (hi, T2, "T2")):
                        P.op("dve", lambda e, rows=rows, src=src, c=c, Uc=Uc: e.scalar_tensor_tensor(
                            out=PL[rows, c, :], in0=src[rows, 16:528], scalar=CF[rows, CF_IW + c:CF_IW + c + 1], in1=Uc[rows, 16:528],
                            op0=ALU.mult, op1=ALU.subtract), r=[sk, ("U", c), "CF"], w=[("PL", c)])
                        if t == 0:
                            P.op("dve", lambda e, rows=rows, src=src, c=c: e.tensor_tensor(out=TF[rows, :], in0=src[rows, 16:32],
                                                                                          in1=CF[rows, CF_TB + c * 16:CF_TB + (c + 1) * 16], op=ALU.mult),
                                 r=[sk, "CF"], w=["TF"])
                            P.op("dve", lambda e, rows=rows, c=c, Uc=Uc: e.tensor_tensor(out=PL[rows, c, 0:16], in0=TF[rows, :], in1=Uc[rows, 16:32], op=ALU.subtract),
                                 r=["TF", ("U", c)], w=[("PL", c)])
                    P.op("pool", lambda e, Uc=Uc: e.tensor_copy(out=Uc[:, 0:16], in_=Uc[:, 512:528]), r=["T1", "T2", ("PL", c)], w=[("U", c)])
                    P.op("pe", lambda e, c=c: e.matmul(msb[:], PWB[:, c, :], PL[:, c, :], start=True, stop=True), r=["PWB", ("PL", c)], w=["MS"])
                    P.op("dve", lambda e, c=c: e.scalar_tensor_tensor(out=YT[:, 2 + c, :], in0=msb[:], scalar=SPR[:, 12 + c:13 + c], in1=GS[:, c, :],
                                                                      op0=ALU.mult, op1=ALU.mult),
                         r=["MS", "SPR", ("GS", c)], w=[("YT", 2 + c)])

                P.cut(9)
                for c in range(2):
                    i = proj2(1280 + c * 128)
                    P.op("act", lambda e, i=i: e.activation(out=DH[:], in_=PJ[i][:], func=AF.Copy), r=[("PJ", i)], w=["DH"])
                    i = proj2(1792 + c * 128)
                    Zc = Z[:, c, :]
                    P.op("dve", lambda e, i=i, Zc=Zc: e.tensor_tensor(out=Zc[:, 2:514], in0=PJ[i][:], in1=DH[:], op=ALU.mult), r=[("PJ", i), "DH"], w=[("Z", c)])
                    wc = 14 + c * 3
                    P.op("dve", lambda e, Zc=Zc, wc=wc: e.tensor_scalar(out=ACC[:], in0=Zc[:, 2:514], scalar1=SPR[:, wc + 2:wc + 3], scalar2=None, op0=ALU.mult),
                         r=[("Z", c), "SPR"], w=["ACC"])
                    P.op("dve", lambda e, Zc=Zc, wc=wc: e.scalar_tensor_tensor(out=ACC[:], in0=Zc[:, 1:513], scalar=SPR[:, wc + 1:wc + 2], in1=ACC[:], op0=ALU.mult, op1=ALU.add),
                         r=[("Z", c), "SPR", "ACC"], w=["ACC"])
                    P.op("dve", lambda e, Zc=Zc, wc=wc: e.scalar_tensor_tensor(out=ACC[:], in0=Zc[:, 0:512], scalar=SPR[:, wc:wc + 1], in1=ACC[:], op0=ALU.mult, op1=ALU.add),
                         r=[("Z", c), "SPR", "ACC"], w=["ACC"])
                    P.op("pool", lambda e, Zc=Zc: e.tensor_copy(out=Zc[:, 0:2], in_=Zc[:, 512:514]), r=[("Z", c)], w=[("Z", c)])
                    i = proj2(1536 + c * 128)
                    P.op("dve", lambda e, i=i: e.tensor_tensor(out=ACC[:], in0=PJ[i][:], in1=ACC[:], op=ALU.mult), r=[("PJ", i), "ACC"], w=["ACC"])
                    P.op("pool", lambda e, c=c: e.tensor_tensor(out=YT[:, 6 + c, :], in0=ACC[:], in1=GS[:, 4 + c, :], op=ALU.mult),
                         r=["ACC", ("GS", 4 + c)], w=[("YT", 6 + c)])

                P.cut(10)
                i = proj2(768)
                ri = qk_norm_prep(PJ[i], ("PJ", i))
                for k in range(2):
                    rows = slice(k * 64, (k + 1) * 64)
                    P.op("dve", lambda e, i=i, ri=ri, k=k, rows=rows: e.scalar_tensor_tensor(
                        out=KC[k][0:64, 128:640], in0=PJ[i][rows, :], scalar=SPR[rows, 11:12], in1=RS[ri][rows, :], op0=ALU.mult, op1=ALU.mult),
                         r=[("PJ", i), ("RS", ri), "SPR"], w=[("KC", k)])
                for c in range(2):
                    i = proj2(512 + c * 128)
                    ri = qk_norm_prep(PJ[i], ("PJ", i))
                    for j in range(2):
                        h = 2 * c + j
                        rows = slice(j * 64, (j + 1) * 64)
                        P.op("dve", lambda e, i=i, ri=ri, h=h, rows=rows: e.scalar_tensor_tensor(
                            out=QC[h][0:64, :], in0=PJ[i][rows, :], scalar=SPR[rows, 10:11], in1=RS[ri][rows, :], op0=ALU.mult, op1=ALU.mult),
                             r=[("PJ", i), ("RS", ri), "SPR"], w=[("QC", h)])
                for sub in range(4):
                    i = nxt("pj", 2)
                    for kc in range(8):
                        P.op("pe", lambda e, kc=kc, i=i, sub=sub: e.matmul(PJ[i][:, 0:128], H2[s][:, kc, sub * 128:(sub + 1) * 128], W2[:, kc, 896:1024],
                                                                           start=(kc == 0), stop=(kc == 7)),
                             r=["W2", ("H2", s)], w=[("PJ", i)], sig=(kc == 7))
                    P.op("dve", lambda e, i=i, sub=sub: e.tensor_copy(out=VC[:, 1 + sub, :, 0:64], in_=PJ[i][:, 0:128].rearrange("p (k d) -> p k d", k=2)),
                         r=[("PJ", i)], w=["VC"])
                for h in range(4):
                    k = h // 2
                    for b in range(4):
                        g = 4 * t + b
                        lo = 0 if g > 0 else 128
                        pi = (h * 4 + b) % 4
                        qs = QC[h][0:65, b * 128:(b + 1) * 128]
                        P.op("pe", lambda e, h=h, lo=lo: e.matmul(SW[:, lo:256], ident, CB[:, CB_CMS + h * 256 + lo:CB_CMS + (h + 1) * 256], start=True, stop=False),
                             r=["CB"], w=["SW"], sig=False)
                        if g > 0:
                            P.op("pe", lambda e, k=k, b=b, qs=qs: e.matmul(SW[:, 0:128], KC[k][0:65, b * 128:(b + 1) * 128], qs, start=False, stop=False),
                                 r=[("KC", k), ("KCaug", k), ("QC", h), ("QCaug", h)], w=["SW"], sig=False)
                        P.op("pe", lambda e, k=k, b=b, qs=qs: e.matmul(SW[:, 128:256], KC[k][0:65, 128 + b * 128:128 + (b + 1) * 128], qs, start=False, stop=True),
                             r=[("KC", k), ("KCaug", k), ("QC", h), ("QCaug", h)], w=["SW"])
                        P.op("act", lambda e, pi=pi, lo=lo, h=h: e.activation(out=PS[pi][:, lo:256], in_=SW[:, lo:256], func=AF.Exp,
                                                                              bias=CF[:, CF_SB + h:CF_SB + h + 1], scale=0.125),
                             r=["SW", "CF"], w=[("PS", pi)])
                        oc = OC[:, b * 128:(b + 1) * 128]
                        if g > 0:
                            P.op("pe", lambda e, oc=oc, b=b, k=k, pi=pi: e.matmul(oc, VC[:, b, k, :], PS[pi][:, 0:128], start=True, stop=False),
                                 r=["VC", "VCones", ("PS", pi)], w=["OC"], sig=False)
                        P.op("pe", lambda e, oc=oc, b=b, k=k, pi=pi, g=g: e.matmul(oc, VC[:, 1 + b, k, :], PS[pi][:, 128:256], start=(g == 0), stop=True),
                             r=["VC", "VCones", ("PS", pi)], w=["OC"])
                    hr = slice((h % 2) * 64, (h % 2) * 64 + 64)
                    ci = nxt("rc", 2)
                    P.op("dve", lambda e, ci=ci, h=h, hr=hr: e.tensor_scalar(out=RC[ci][hr, :], in0=OC[64:128, :], scalar1=ESK[64:128, h:h + 1], scalar2=None, op0=ALU.add),
                         r=["OC", "ESK"], w=[("RC", ci)])
                    P.op("dve", lambda e, ci=ci, hr=hr: e.reciprocal(out=RC[ci][hr, :], in_=RC[ci][hr, :]), r=[("RC", ci)], w=[("RC", ci)])
                    P.op("dve", lambda e, ci=ci, hr=hr, h=h: e.tensor_tensor(out=RC[ci][hr, :], in0=RC[ci][hr, :], in1=GS[hr, 2 + h // 2, :], op=ALU.mult),
                         r=[("RC", ci), ("GS", 2 + h // 2)], w=[("RC", ci)])
                    P.op("dve", lambda e, ci=ci, hr=hr, h=h: e.tensor_tensor(out=YT[hr, 4 + h // 2, :], in0=OC[0:64, :], in1=RC[ci][hr, :], op=ALU.mult),
                         r=["OC", ("RC", ci)], w=[("YT", 4 + h // 2)])
                for k in range(2):
                    P.op("pool", lambda e, k=k: e.tensor_copy(out=KC[k][0:64, 0:128], in_=KC[k][0:64, 512:640]), r=[("KC", k)], w=[("KC", k)])
                P.op("pool", lambda e: e.tensor_copy(out=VC[:, 0, :, 0:64], in_=VC[:, 4, :, 0:64]), r=["VC"], w=["VC"])

                if t == 0 and l == 0:
                    dump("H20", H2[s][:, 0, :], [("H2", s)])
                    for c in range(8):
                        dump(f"YT{c}", YT[:, c, :], [("YT", c)])
                P.cut(11)
                for sub in range(4):
                    for hf in range(2):
                        oi = (sub * 2 + hf) % 2
                        for kc in range(8):
                            P.op("pe", lambda e, kc=kc, oi=oi, sub=sub, hf=hf: e.matmul(PO[oi][:], YT[:, kc, sub * 128:(sub + 1) * 128], WO[:, kc, hf * 512:(hf + 1) * 512],
                                                                                       start=(kc == 0), stop=(kc == 7)),
                                 r=["WO"] + [("YT", q) for q in range(8)], w=[("PO", oi)], sig=(kc == 7))
                        P.op("dve", lambda e, oi=oi, sub=sub, hf=hf: e.tensor_tensor(out=X2[s][:, sub, hf * 512:(hf + 1) * 512], in0=PO[oi][:],
                                                                                    in1=X2[s][:, sub, hf * 512:(hf + 1) * 512], op=ALU.add),
                             r=[("PO", oi), ("X2", s)], w=[("X2", s)])
                P.dma(lambda e, t=t, s=s: e.dma_start(out=xdst[t * TT:(t + 1) * TT, :].rearrange("(s p) d -> p s d", p=128), in_=X2[s][:]),
                      r=[("X2", s)], w=["xdst"], key=f"x2{s}")
            P.barrier()

        if DBG:
            P.stopped = False
            P.op("pool", lambda e: e.memset(DBGT[:, dbg_off[0]:12288], 0.0), w=["DBG"]) if dbg_off[0] < 12288 else None
            P.dma(lambda e: e.dma_start(out=dbg_d[:, :], in_=DBGT[:]), r=["DBG"], w=["dbg_d"], key="dbg")
            P.barrier()
        block = es.enter_context(nc.Block())
        P.emit(block)
    return nc


_CACHE = {}


def _host_layout(inputs, NL):
    f = lambda a: np.ascontiguousarray(np.asarray(a, dtype=np.float32))
    spr = np.zeros((NL, 128, NSPR), np.float32)
    pw = np.zeros((NL, 128, 2, 128), np.float32)
    for l in range(NL):
        spr[l, :, 0:8] = f(inputs["norm_g"])[l].reshape(8, 128).T
        spr[l, :, 8] = np.tile(f(inputs["a_q_norm"])[l], 2)
        spr[l, :, 9] = np.tile(f(inputs["a_k_norm"])[l], 2)
        spr[l, :, 10] = np.tile(f(inputs["c_q_norm"])[l], 2)
        spr[l, :, 11] = np.tile(f(inputs["c_k_norm"])[l], 2)
        spr[l, :, 12:14] = f(inputs["pool_scale"])[l].reshape(2, 128).T
        cw = f(inputs["conv_w"])[l]
        for c in range(2):
            for j in range(3):
                spr[l, :, 14 + c * 3 + j] = cw[j, c * 128:(c + 1) * 128]
        spr[l, :, 20:24] = f(inputs["c_sinks"])[l][None, :]
        pwl = f(inputs["pool_w"])[l]
        for c in range(2):
            for half in range(2):
                pw[l, half * 64:(half + 1) * 64, c, half * 64:(half + 1) * 64] = pwl[2 * c + half]
    return spr, pw


def run(inputs, S, NL, n_cores):
    key = (S, NL)
    if key not in _CACHE:
        _CACHE[key] = (build_program(S, NL), make_consts(S))
    nc, consts = _CACHE[key]
    spr, pw = _host_layout(inputs, NL)
    x = np.asarray(inputs["x"], dtype=np.float32)
    w_in = np.ascontiguousarray(np.asarray(inputs["w_in"], dtype=np.float32))
    w_out = np.ascontiguousarray(np.asarray(inputs["w_out"], dtype=np.float32))
    in_maps = []
    for c in range(n_cores):
        m = {"x": np.ascontiguousarray(x[c]), "w_in": w_in, "w_out": w_out, "spr": spr, "pw": pw}
        m.update(consts)
        in_maps.append(m)
    res = run_bass_kernel_spmd(nc, in_maps, core_ids=list(range(n_cores)))
    if "dbg" in res.results[0]:
        run.dbg = np.asarray(res.results[0]["dbg"]).astype(np.float32)
    return np.stack([np.asarray(r["out"], dtype=np.float32) for r in res.results], axis=0)


def kernel(x, norm_g, w_in, w_out, a_q_norm, a_k_norm, pool_w, pool_scale, c_q_norm, c_k_norm, c_sinks, conv_w):
    inputs = dict(x=x, norm_g=norm_g, w_in=w_in, w_out=w_out, a_q_norm=a_q_norm, a_k_norm=a_k_norm, pool_w=pool_w,
                  pool_scale=pool_scale, c_q_norm=c_q_norm, c_k_norm=c_k_norm, c_sinks=c_sinks, conv_w=conv_w)
    x = np.asarray(x)
    return run(inputs, x.shape[1], np.asarray(w_in).shape[0], x.shape[0])
```

```python
import contextlib
import types
import numpy as np
import ml_dtypes
import concourse.bass as bass
import concourse.mybir as mybir
from concourse.bass_utils import run_bass_kernel_spmd

F32 = mybir.dt.float32
BF16 = mybir.dt.bfloat16
AF = mybir.ActivationFunctionType
ALU = mybir.AluOpType

D = 1024
NIN = 3328
TT = 512
BIG = 30000.0
EPS = 1e-6
NSPR = 24
ENGS = ("pe", "act", "dve", "pool", "sp")
BLK = {"pe": "tensor", "act": "scalar", "dve": "vector", "pool": "gpsimd", "sp": "sync"}

CB_ID, CB_OB, CB_CM, CB_CMS, CB_N = 0, 128, 256, 256 + 2048, 256 + 2048 + 1024
CF_AB, CF_SB, CF_IW, CF_TB, CF_N = 0, 256, 260, 262, 262 + 32


def slopes_all():
    return np.exp2(-(8.0 / 8) * np.arange(1, 9, dtype=np.float32)).astype(np.float32)


def make_consts(S):
    sl = slopes_all()
    sl_c, sl_a = sl[:4], sl[4:]
    cb = np.zeros((128, CB_N), np.float32)
    cb[:, CB_ID:CB_ID + 128] = np.eye(128)
    cb[0:64, CB_OB:CB_OB + 64] = 1.0
    cb[64:128, CB_OB + 64:CB_OB + 128] = 1.0
    kl = np.arange(128)[:, None]
    ql = np.arange(512)[None, :]
    for kt in range(4):
        m = ((ql // 256) == (kt // 2)) & (ql < 128 * kt + kl)
        cb[:, CB_CM + kt * 512:CB_CM + (kt + 1) * 512] = np.where(m, -BIG, 0.0)
    q1 = np.arange(128)[None, :]
    for h in range(4):
        prev = np.where(kl > q1, -8.0 * sl_c[h] * 128.0, -BIG)
        own = np.where(kl <= q1, 0.0, -BIG)
        cb[:, CB_CMS + h * 256:CB_CMS + h * 256 + 128] = prev
        cb[:, CB_CMS + h * 256 + 128:CB_CMS + (h + 1) * 256] = own
    cf = np.zeros((128, CF_N), np.float32)
    for h in range(4):
        for i in range(64):
            d = 3 - i
            cf[:, CF_AB + h * 64 + i] = sl_a[h] * (np.arange(128) - 511 + 128 * d)
        cf[:, CF_SB + h] = sl_c[h] * np.arange(128)
    wins = (2, 4, 8, 16)
    for c in range(2):
        for half in range(2):
            w = wins[2 * c + half]
            rows = slice(half * 64, (half + 1) * 64)
            cf[rows, CF_IW + c] = 1.0 / w
            cf[rows, CF_TB + c * 16:CF_TB + (c + 1) * 16] = 1.0 / np.minimum(np.arange(16) + 1, w)
    khot = np.zeros((32, S), np.float32)
    for n in range(S // 256):
        khot[n, n * 256:(n + 1) * 256] = 1.0
    cq = np.zeros((4, 512), np.float32)
    for h in range(4):
        cq[h] = -8.0 * sl_c[h] * (np.arange(512) % 128)
    bf = ml_dtypes.bfloat16
    return {"cb": cb.astype(bf), "cf": cf, "khot": khot.astype(bf), "cqrow": cq.astype(bf)}


def _freeze(fn):
    if fn is None or fn.__closure__ is None:
        return fn
    cells = []
    for c in fn.__closure__:
        try:
            cells.append(types.CellType(c.cell_contents))
        except ValueError:
            cells.append(c)
    g = types.FunctionType(fn.__code__, fn.__globals__, fn.__name__, fn.__defaults__, tuple(cells))
    g.__kwdefaults__ = fn.__kwdefaults__
    return g


class Prog:
    def __init__(self, nc, es):
        self.nc = nc
        self.es = es
        self.ops = {e: [] for e in ENGS}
        self.cnt = {e: 0 for e in ENGS}
        self.semh = {}
        self.dcnt = {}
        self.lastw = {}
        self.readers = {}
        self.waited = {e: {} for e in ENGS}
        for e in ENGS:
            self.semh["E:" + e] = es.enter_context(nc.semaphore("sem_" + e))

    def _collect(self, eng, r, w):
        need = {}

        def add(ev):
            semid, val, src = ev
            if src == eng and eng == "pe":
                return
            if self.waited[eng].get(semid, 0) >= val:
                return
            if need.get(semid, 0) < val:
                need[semid] = val

        for k in r:
            if k in self.lastw:
                add(self.lastw[k])
        for k in w:
            if k in self.lastw:
                add(self.lastw[k])
            for ev in self.readers.get(k, {}).values():
                add(ev)
        for semid, val in need.items():
            self.waited[eng][semid] = val
        return list(need.items())

    def _commit(self, ev, r, w):
        for k in r:
            d = self.readers.setdefault(k, {})
            if ev[0] not in d or d[ev[0]][1] < ev[1]:
                d[ev[0]] = ev
        for k in w:
            self.lastw[k] = ev
            self.readers[k] = {}

    stopped = False

    def cut(self, k):
        import os
        if int(os.environ.get("STAGE", "99")) == k and not self.stopped:
            self.barrier()
            self.stopped = True

    def op(self, eng, fn, r=(), w=(), sig=True):
        if self.stopped:
            return
        fn = _freeze(fn)
        waits = self._collect(eng, r, w)
        semid = "E:" + eng
        if sig:
            self.cnt[eng] += 1
            ev = (semid, self.cnt[eng], eng)
            inc = (semid, 1)
        else:
            ev = (semid, self.cnt[eng] + 1, eng)
            inc = None
        self.ops[eng].append((waits, fn, inc))
        self._commit(ev, r, w)

    def dma(self, fn, r=(), w=(), key=None, q="sp"):
        if self.stopped:
            return
        fn = _freeze(fn)
        waits = self._collect(q, r, w)
        semid = "D:" + key
        if semid not in self.semh:
            self.semh[semid] = self.es.enter_context(self.nc.semaphore("dsem_" + key))
            self.dcnt[semid] = 0
        self.dcnt[semid] += 16
        ev = (semid, self.dcnt[semid], "dma")
        self.ops[q].append((waits, fn, (semid, 16)))
        self._commit(ev, r, w)

    def dma_group(self, key, items, q="sp"):
        if self.stopped:
            return
        semid = "D:" + key
        if semid not in self.semh:
            self.semh[semid] = self.es.enter_context(self.nc.semaphore("dsem_" + key))
            self.dcnt[semid] = 0
        total = self.dcnt[semid] + 16 * len(items)
        ev = (semid, total, "dma")
        for fn, r, w in items:
            waits = self._collect(q, r, w)
            self.ops[q].append((waits, _freeze(fn), (semid, 16)))
        for fn, r, w in items:
            self._commit(ev, r, w)
        self.dcnt[semid] = total

    def barrier(self):
        if self.stopped:
            return
        for e in ENGS:
            waits = []
            for o in ENGS:
                if o == e or self.cnt[o] == 0:
                    continue
                sid = "E:" + o
                if self.waited[e].get(sid, 0) < self.cnt[o]:
                    waits.append((sid, self.cnt[o]))
                    self.waited[e][sid] = self.cnt[o]
            for sid, c in self.dcnt.items():
                if self.waited[e].get(sid, 0) < c:
                    waits.append((sid, c))
                    self.waited[e][sid] = c
            if waits:
                self.ops[e].append((waits, None, None))
        self.lastw = {}
        self.readers = {}

    def emit(self, block):
        for eng in ENGS:
            def body(e, eng=eng):
                for waits, fn, inc in self.ops[eng]:
                    for semid, val in waits:
                        e.wait_ge(self.semh[semid], val)
                    if fn is None:
                        continue
                    ins = fn(e)
                    if inc is not None:
                        ins.then_inc(self.semh[inc[0]], inc[1])
            getattr(block, BLK[eng])(body)


class Arena:
    def __init__(self, nc, base, top):
        self.nc, self.base, self.top, self.cur, self.n = nc, base, top, base, 0

    def alloc(self, name, shape, dtype):
        nbytes = int(np.prod(shape[1:])) * (2 if dtype == BF16 else 4)
        nbytes = (nbytes + 63) // 64 * 64
        off = self.cur
        assert off + nbytes <= self.top, f"SBUF overflow at {name}: {off + nbytes} > {self.top}"
        self.cur += nbytes
        self.n += 1
        return self.nc.alloc_sbuf_tensor_at(f"{name}_{self.n}", list(shape), dtype, offset=off)


def build_program(S, NL):
    NT = S // TT
    NKT = S // 128
    nc = bass.Bass("TRN2", target_bir_lowering=False)
    dt = nc.dram_tensor
    x_in = dt("x", [S, D], F32, kind="ExternalInput").ap()
    w_in = dt("w_in", [NL, D, NIN], F32, kind="ExternalInput").ap()
    w_out = dt("w_out", [NL, D, D], F32, kind="ExternalInput").ap()
    spr_d = dt("spr", [NL, 128, NSPR], F32, kind="ExternalInput").ap()
    pw_d = dt("pw", [NL, 128, 2, 128], F32, kind="ExternalInput").ap()
    cb_d = dt("cb", [128, CB_N], BF16, kind="ExternalInput").ap()
    cf_d = dt("cf", [128, CF_N], F32, kind="ExternalInput").ap()
    khot_d = dt("khot", [32, S], BF16, kind="ExternalInput").ap()
    cq_d = dt("cqrow", [4, 512], BF16, kind="ExternalInput").ap()
    out_d = dt("out", [S, D], F32, kind="ExternalOutput").ap()
    hT_d = dt("hT_scr", [D, S], BF16, kind="Internal").ap()
    ya_d = dt("ya_scr", [256, S], BF16, kind="Internal").ap()
    x1_d = dt("x1_scr", [S, D], F32, kind="Internal").ap() if NL > 1 else None

    with contextlib.ExitStack() as es:
        P = Prog(nc, es)
        a_base = (int(nc.sbuf_base) + 63) // 64 * 64
        a_size = int(nc.sbuf_top) - a_base - 2048
        es.enter_context(nc.sbuf_tensor("arena_slab", [128, a_size], mybir.dt.uint8))
        A = Arena(nc, a_base, a_base + a_size)
        pb = [es.enter_context(nc.psum_tensor(f"pb{i}", [128, 512], F32)) for i in range(6)]
        tpb = es.enter_context(nc.psum_tensor("tpb", [128, 512], F32))
        msb = es.enter_context(nc.psum_tensor("msb", [128, 512], F32))
        CB = A.alloc("CB", [128, CB_N], BF16)
        CF = A.alloc("CF", [128, CF_N], F32)
        SPR = A.alloc("SPR", [128, NSPR], F32)
        ESK = A.alloc("ESK", [128, 4], F32)
        SQ = [A.alloc(f"SQ{i}", [128, 512], BF16) for i in range(2)]
        RS = [A.alloc(f"RS{i}", [128, 512], F32) for i in range(2)]
        RC = [A.alloc(f"RC{i}", [128, 512], F32) for i in range(2)]
        ident = CB[:, CB_ID:CB_ID + 128]
        onesblk = CB[:, CB_OB:CB_OB + 128]
        import os
        DBG = os.environ.get("DBG") == "1"
        dbg_off = [0]
        dbg_names = []
        if DBG:
            dbg_d = dt("dbg", [128, 12288], BF16, kind="ExternalOutput").ap()
            DBGT = A.alloc("DBGT", [128, 12288], BF16)

        def dump(name, ap, r, npart=128):
            if not DBG or P.stopped:
                return
            n = ap.shape[-1] if len(ap.shape) == 2 else int(np.prod(ap.shape[1:]))
            o = dbg_off[0]
            dbg_names.append((name, o, n, npart))
            dbg_off[0] += n
            assert dbg_off[0] <= 12288
            P.op("dve", lambda e: e.tensor_copy(out=DBGT[0:npart, o:o + n], in_=ap), r=r, w=["DBG"])
        build_program.dbg_names = dbg_names
        mark = A.cur

        first_items = [(lambda e: e.dma_start(out=CB[:], in_=cb_d[:, :]), [], ["CB"]),
                       (lambda e: e.dma_start(out=CF[:], in_=cf_d[:, :]), [], ["CF"])]

        rr = {"sq": 0, "pj": 0, "rc": 0}

        def nxt(name, n):
            rr[name] = (rr[name] + 1) % n
            return rr[name]

        def qk_norm_prep(pj, pjk):
            i = nxt("sq", 2)
            P.op("act", lambda e: e.activation(out=SQ[i][:], in_=pj[:], func=AF.Square), r=[pjk], w=[("SQ", i)])
            P.op("pe", lambda e: e.matmul(msb[:], onesblk, SQ[i][:], start=True, stop=True),
                 r=[("SQ", i), "CB"], w=["MS"])
            P.op("act", lambda e: e.activation(out=RS[i][:], in_=msb[:], func=AF.Ln, bias=EPS, scale=1.0 / 64),
                 r=["MS"], w=[("RS", i)])
            P.op("act", lambda e: e.activation(out=RS[i][:], in_=RS[i][:], func=AF.Exp, scale=-0.5),
                 r=[("RS", i)], w=[("RS", i)])
            return i

        for l in range(NL):
            xsrc = x_in if l == 0 else x1_d
            xdst = out_d if l == NL - 1 else x1_d
            items = first_items if l == 0 else []
            items.append((lambda e, l=l: e.dma_start(out=SPR[:], in_=spr_d[l, :, :]), [], ["SPR"]))

            A.cur = mark
            WA = A.alloc("WA", [128, 8, 1024], BF16)
            KA = [A.alloc(f"KA{h}", [96, S], BF16) for h in range(4)]
            VA = A.alloc("VA", [128, NKT, 384], BF16)
            KM = A.alloc("KM", [128, 2, 64], F32)
            wbase = A.cur
            XT = A.alloc("XT", [128, 4, 1024], F32)
            XS = A.alloc("XS", [128, 4, 1024], BF16)
            WSTG = [nc.alloc_sbuf_tensor_at(f"WSTGa{l}_{i}", [128, 2304], F32, offset=wbase + i * 9216) for i in range(2)]
            HT = [A.alloc(f"HT{i}", [128, 8, 512], BF16) for i in range(2)]
            QF = A.alloc("QF", [128, 2, 512], F32)
            QA = [[A.alloc(f"QA{h}_{s}", [96, 512], BF16) for s in range(1)] * 2 for h in range(4)]
            GA = [A.alloc("GA", [128, 2, 512], BF16)] * 2
            PT = [A.alloc(f"PT{i}", [128, 512], BF16) for i in range(4)]
            YA = [A.alloc("YA", [128, 2, 512], BF16)] * 2
            SSX = A.alloc("SSX", [128, 4], F32)
            RSX = A.alloc("RSX", [128, 4], F32)
            BSM = A.alloc("BSM", [128, 4, 32], F32)
            M8 = A.alloc("M8", [128, 4, 8], F32)
            THR = A.alloc("THR", [128, 4], F32)
            MB = A.alloc("MB", [128, 4, 32], BF16)
            PJ = pb[0:1]
            ST = pb[1:4]
            OT = pb[4:6]

            for h in range(4):
                items.append((lambda e, h=h: e.dma_start(out=KA[h][64:96, :], in_=khot_d[:, :]), [], [("KAaug", h)]))
            P.dma_group("const", items)
            P.op("act", lambda e: e.activation(out=ESK[:], in_=SPR[:, 20:24], func=AF.Exp), r=["SPR"], w=["ESK"])
            P.op("pool", lambda e: e.memset(VA[:, :, 64:128], 1.0), w=["VAones"])
            P.op("pool", lambda e: e.memset(VA[:, :, 256:320], 1.0), w=["VAones"])
            P.op("pool", lambda e: e.memset(KM[:], 0.0), w=["KM"])
            for kc in range(8):
                i = kc % 2
                P.dma(lambda e, kc=kc, i=i, l=l: e.dma_start(out=WSTG[i][:, 0:1024], in_=w_in[l, kc * 128:(kc + 1) * 128, 0:1024]),
                      w=[("WSTG", i)], key=f"w{i}")
                P.op("dve", lambda e, kc=kc, i=i: e.tensor_scalar(out=WA[:, kc, :], in0=WSTG[i][:, 0:1024], scalar1=SPR[:, kc:kc + 1],
                                                                  scalar2=None, op0=ALU.mult),
                     r=[("WSTG", i), "SPR"], w=["WA", "XTa", "XS"])

            def load_x(t):
                P.dma(lambda e, t=t: e.dma_start(out=XT[:], in_=xsrc[t * TT:(t + 1) * TT, :].rearrange("(s p) d -> p s d", p=128)),
                      r=["XTa"], w=["XT"], key="xt")

            P.cut(1)
            load_x(0)
            def proj(c0, s):
                i = nxt("pj", len(PJ))
                for kc in range(8):
                    P.op("pe", lambda e, kc=kc, i=i: e.matmul(PJ[i][:], WA[:, kc, c0:c0 + 128], HT[s][:, kc, :], start=(kc == 0), stop=(kc == 7)),
                         r=["WA", ("HT", s)], w=[("PJ", i)], sig=(kc == 7))
                return i

            def stage1(t):
                s = t % 2
                for sub in range(4):
                    P.op("act", lambda e, sub=sub: e.activation(out=XS[:, sub, :], in_=XT[:, sub, :], func=AF.Square,
                                                                accum_out=SSX[:, sub:sub + 1]),
                         r=["XT"], w=["XS", "SSX"])
                P.op("act", lambda e: e.activation(out=RSX[:], in_=SSX[:], func=AF.Ln, bias=EPS, scale=1.0 / D), r=["SSX"], w=["RSX"])
                P.op("act", lambda e: e.activation(out=RSX[:], in_=RSX[:], func=AF.Exp, scale=-0.5), r=["RSX"], w=["RSX"])
                for sub in range(4):
                    P.op("dve", lambda e, sub=sub: e.tensor_scalar(out=XS[:, sub, :], in0=XT[:, sub, :], scalar1=RSX[:, sub:sub + 1],
                                                                   scalar2=None, op0=ALU.mult),
                         r=["XT", "RSX"], w=["XS"])
                if t + 1 < NT:
                    load_x(t + 1)
                for kc in range(8):
                    hf = kc % 2
                    bank, bkey = (tpb, ("TP", 0)) if hf == 0 else (msb, "MS")
                    for sub in range(4):
                        P.op("pe", lambda e, kc=kc, sub=sub, bank=bank: e.matmul(bank[:, sub * 128:(sub + 1) * 128], XS[:, sub, kc * 128:(kc + 1) * 128], ident,
                                                                                 start=True, stop=True),
                             r=["XS", "CB"], w=[bkey], sig=(sub == 3))
                    P.op("dve", lambda e, kc=kc, bank=bank: e.tensor_copy(out=HT[s][:, kc, :], in_=bank[:, 0:512]),
                         r=[bkey], w=[("HT", s)])
                P.dma(lambda e, t=t, s=s: e.dma_start(out=hT_d[:, t * TT:(t + 1) * TT].rearrange("(kc p) n -> p kc n", p=128), in_=HT[s][:]),
                      r=[("HT", s)], w=["hT_d"], key=f"hts{s}")


                for c in range(2):
                    i = proj(256 + c * 128, s)
                    ri = qk_norm_prep(PJ[i], ("PJ", i))
                    for j in range(2):
                        h = 2 * c + j
                        rows = slice(j * 64, (j + 1) * 64)
                        for b in range(2):
                            n = 2 * t + b
                            P.op("dve", lambda e, i=i, ri=ri, h=h, rows=rows, b=b, n=n: e.scalar_tensor_tensor(
                                out=KA[h][0:64, t * TT + b * 256:t * TT + (b + 1) * 256], in0=PJ[i][rows, b * 256:(b + 1) * 256],
                                scalar=SPR[rows, 9:10], in1=RS[ri][rows, b * 256:(b + 1) * 256], op0=ALU.mult, op1=ALU.mult,
                                accum_out=KM[rows, c, j * 32 + n:j * 32 + n + 1]),
                                 r=[("PJ", i), ("RS", ri), "SPR"], w=[("KA", h, t), "KM"])
                for sub in range(4):
                    i = nxt("pj", len(PJ))
                    for kc in range(8):
                        P.op("pe", lambda e, kc=kc, i=i, sub=sub: e.matmul(PJ[i][:, 0:256], HT[s][:, kc, sub * 128:(sub + 1) * 128], WA[:, kc, 512:768],
                                                                           start=(kc == 0), stop=(kc == 7)),
                             r=["WA", ("HT", s)], w=[("PJ", i)], sig=(kc == 7))
                    kt = 4 * t + sub
                    for (d0, s0, wd) in ((0, 0, 64), (128, 64, 128), (320, 192, 64)):
                        P.op("dve", lambda e, i=i, kt=kt, d0=d0, s0=s0, wd=wd: e.tensor_copy(out=VA[:, kt, d0:d0 + wd], in_=PJ[i][:, s0:s0 + wd]),
                             r=[("PJ", i)], w=[("VA", t)])

            def stage2(t):
                s = t % 2
                for c in range(2):
                    i = proj(c * 128, s)
                    ri = qk_norm_prep(PJ[i], ("PJ", i))
                    P.op("dve", lambda e, i=i, ri=ri, c=c: e.scalar_tensor_tensor(
                        out=QF[:, c, :], in0=PJ[i][:], scalar=SPR[:, 8:9], in1=RS[ri][:], op0=ALU.mult, op1=ALU.mult),
                         r=[("PJ", i), ("RS", ri), "SPR"], w=[("QF", c)])
                    P.op("pool", lambda e, c=c: e.tensor_copy(out=QA[2 * c][0][0:64, :], in_=QF[0:64, c, :]),
                         r=[("QF", c)], w=[("QA", 2 * c, 0)])
                    P.op("dve", lambda e, c=c: e.tensor_copy(out=QA[2 * c + 1][0][0:64, :], in_=QF[64:128, c, :]),
                         r=[("QF", c)], w=[("QA", 2 * c + 1, 0)])
                for c in range(2):
                    i = proj(768 + c * 128, s)
                    P.op("act", lambda e, i=i, c=c: e.activation(out=GA[0][:, c, :], in_=PJ[i][:], func=AF.Silu),
                         r=[("PJ", i)], w=[("GA", 0, c)])
                for sub in range(4):
                    own = 2 * t + sub // 2
                    for c in range(2):
                        P.op("pe", lambda e, c=c, sub=sub: e.matmul(msb[:, c * 64:(c + 1) * 64], QF[:, c, sub * 128:(sub + 1) * 128], KM[:, c, :],
                                                                    start=True, stop=True),
                             r=[("QF", c), "KM"], w=["MS"], sig=(c == 1))
                    P.op("pool", lambda e: e.memset(BSM[:], -1e30), w=["BSM"])
                    if own > 0:
                        P.op("dve", lambda e, own=own: e.tensor_copy(out=BSM[:, :, 0:own],
                                                                     in_=msb[:, 0:128].rearrange("p (h n) -> p h n", h=4)[:, :, 0:own]),
                             r=["MS"], w=["BSM"])
                    for h in range(4):
                        P.op("dve", lambda e, h=h: e.max(out=M8[:, h, :], in_=BSM[:, h, :]), r=["BSM"], w=["M8"])
                    P.op("dve", lambda e: e.tensor_scalar(out=THR[:], in0=M8[:, :, 2], scalar1=-1e29, scalar2=None, op0=ALU.max),
                         r=["M8"], w=["THR"])
                    for h in range(4):
                        P.op("dve", lambda e, h=h: e.tensor_scalar(out=MB[:, h, :], in0=BSM[:, h, :], scalar1=THR[:, h:h + 1], scalar2=-BIG,
                                                                   op0=ALU.is_lt, op1=ALU.mult),
                             r=["BSM", "THR"], w=["MB"])
                    P.op("dve", lambda e, own=own: e.memset(MB[:, :, own:own + 1], 0.0), w=["MB"])
                    P.op("pe", lambda e: e.matmul(tpb[:, 0:128], MB[:].rearrange("p h n -> p (h n)"), ident, start=True, stop=True), r=["MB", "CB"], w=[("TP", 0)])
                    for h in range(4):
                        P.op("dve", lambda e, h=h, sub=sub: e.tensor_copy(out=QA[h][0][64:96, sub * 128:(sub + 1) * 128], in_=tpb[h * 32:(h + 1) * 32, 0:128]),
                             r=[("TP", 0)], w=[("QAm", h, 0)])


            def moba(t, hook):
                s = t % 2
                for h in range(4):
                    oslot = h % 2
                    nkt = 4 * t + 4
                    vc0 = (0, 64, 192, 256)[h]
                    pend = []
                    for kt in range(nkt):
                        si = kt % 3
                        diag = kt >= 4 * t
                        P.op("pe", lambda e, h=h, kt=kt, si=si, diag=diag: e.matmul(ST[si][:], KA[h][0:96, kt * 128:(kt + 1) * 128], QA[h][0][0:96, :],
                                                                                    start=True, stop=not diag),
                             r=[("KA", h, kt // 4), ("KAaug", h), ("QA", h, 0), ("QAm", h, 0)], w=[("ST", si)], sig=not diag)
                        if diag:
                            dk = kt - 4 * t
                            P.op("pe", lambda e, si=si, dk=dk: e.matmul(ST[si][:], ident, CB[:, CB_CM + dk * 512:CB_CM + (dk + 1) * 512], start=False, stop=True),
                                 r=["CB"], w=[("ST", si)])
                        pi = kt % 4
                        col = CF_AB + h * 64 + (3 - (kt - 4 * t))
                        P.op("act", lambda e, si=si, pi=pi, col=col: e.activation(out=PT[pi][:], in_=ST[si][:], func=AF.Exp, bias=CF[:, col:col + 1], scale=0.125),
                             r=[("ST", si), "CF"], w=[("PT", pi)])

                        def pv(h=h, kt=kt, pi=pi, oslot=oslot, vc0=vc0, nkt=nkt):
                            P.op("pe", lambda e: e.matmul(OT[oslot][:], VA[:, kt, vc0:vc0 + 128], PT[pi][:], start=(kt == 0), stop=(kt == nkt - 1)),
                                 r=[("VA", kt // 4), "VAones", ("PT", pi)], w=[("OT", oslot)], sig=True)
                        pend.append(pv)
                        if len(pend) > 2:
                            pend.pop(0)()
                    for f_ in pend:
                        f_()
                    nr = slice(0, 64) if h % 2 == 0 else slice(64, 128)
                    dr = slice(64, 128) if h % 2 == 0 else slice(0, 64)
                    hr = slice((h % 2) * 64, (h % 2) * 64 + 64)
                    ci = nxt("rc", 2)
                    P.op("dve", lambda e, oslot=oslot, dr=dr, ci=ci, hr=hr: e.reciprocal(out=RC[ci][hr, :], in_=OT[oslot][dr, :]), r=[("OT", oslot)], w=[("RC", ci)])
                    P.op("dve", lambda e, ci=ci, hr=hr, h=h: e.tensor_tensor(out=RC[ci][hr, :], in0=RC[ci][hr, :], in1=GA[0][hr, h // 2, :], op=ALU.mult),
                         r=[("RC", ci), ("GA", 0, h // 2)], w=[("RC", ci)])
                    P.op("dve", lambda e, oslot=oslot, nr=nr, ci=ci, hr=hr, h=h: e.tensor_tensor(out=YA[0][hr, h // 2, :], in0=OT[oslot][nr, :], in1=RC[ci][hr, :], op=ALU.mult),
                         r=[("OT", oslot), ("RC", ci)], w=[("YA", 0)])
                    hook(h)
                P.dma(lambda e, t=t, s=s: e.dma_start(out=ya_d[:, t * TT:(t + 1) * TT].rearrange("(c p) n -> p c n", p=128), in_=YA[0][:]),
                      r=[("YA", 0)], w=["ya_d"], key="yas")

            stage1(0)
            stage2(0)
            for t in range(NT):
                def hook(h, t=t):
                    if h == 0 and t + 1 < NT:
                        stage1(t + 1)
                moba(t, hook)
                if t + 1 < NT:
                    stage2(t + 1)
            P.barrier()
            P.cut(6)

            A.cur = mark
            W2 = A.alloc("W2", [128, 8, 2304], BF16)
            WO = A.alloc("WO", [128, 8, 1024], BF16)
            PWB = A.alloc("PWB", [128, 2, 128], BF16)
            H2 = [A.alloc(f"H2{i}", [128, 8, 512], BF16) for i in range(2)]
            wbase = A.cur
            X2 = [A.alloc(f"X2{i}", [128, 4, 1024], F32) for i in range(2)]
            WSTG = [nc.alloc_sbuf_tensor_at(f"WSTGb{l}_{i}", [128, 2304], F32, offset=wbase + i * 16384) for i in range(2)]
            YT = A.alloc("YT", [128, 8, 512], BF16)
            GS = A.alloc("GS", [128, 6, 512], BF16)
            U = A.alloc("U", [128, 2, 528], F32)
            T1 = A.alloc("T1", [128, 528], F32)
            T2 = A.alloc("T2", [128, 528], F32)
            TF = A.alloc("TF", [128, 16], F32)
            PL = A.alloc("PL", [128, 2, 512], BF16)
            DH = A.alloc("DH", [128, 512], F32)
            Z = A.alloc("Z", [128, 2, 514], F32)
            ACC = A.alloc("ACC", [128, 512], F32)
            QC = [A.alloc(f"QC{h}", [65, 512], BF16) for h in range(4)]
            KC = [A.alloc(f"KC{k}", [65, 640], BF16) for k in range(2)]
            VC = A.alloc("VC", [128, 5, 2, 128], BF16)
            PS = [A.alloc(f"PS{i}", [128, 256], BF16) for i in range(4)]
            PJ = pb[0:2]
            SW = pb[2]
            OC = pb[3]
            PO = pb[4:6]

            P.op("pool", lambda e: e.memset(U[:], 0.0), w=[("U", 0), ("U", 1)])
            P.op("pool", lambda e: e.memset(Z[:], 0.0), w=[("Z", 0), ("Z", 1)])
            P.op("pool", lambda e: e.memset(VC[:, :, :, 64:128], 1.0), w=["VCones"])
            P.op("pool", lambda e: e.memset(VC[:, 0, :, 0:64], 0.0), w=["VC"])
            for k in range(2):
                P.op("pool", lambda e, k=k: e.memset(KC[k][64:65, :], 1.0), w=[("KCaug", k)])
                P.op("pool", lambda e, k=k: e.memset(KC[k][0:64, 0:128], 0.0), w=[("KC", k)])
            P.dma_group("const2", [(lambda e, h=h: e.dma_start(out=QC[h][64:65, :], in_=cq_d[h:h + 1, :]), [], [("QCaug", h)]) for h in range(4)])
            for kc in range(8):
                i = kc % 2
                P.dma(lambda e, kc=kc, i=i, l=l: e.dma_start(out=WSTG[i][:], in_=w_in[l, kc * 128:(kc + 1) * 128, 1024:3328]),
                      w=[("X2", i)], key=f"w{i}")
                P.op("dve", lambda e, kc=kc, i=i: e.tensor_scalar(out=W2[:, kc, :], in0=WSTG[i][:], scalar1=SPR[:, kc:kc + 1], scalar2=None, op0=ALU.mult),
                     r=[("X2", i), "SPR"], w=["W2"])
            for kc in range(8):
                i = kc % 2
                P.dma(lambda e, kc=kc, i=i, l=l: e.dma_start(out=WSTG[i][:, 0:1024], in_=w_out[l, kc * 128:(kc + 1) * 128, :]),
                      w=[("X2", i)], key=f"w{i}")
                P.op("pool", lambda e, kc=kc, i=i: e.tensor_copy(out=WO[:, kc, :], in_=WSTG[i][:, 0:1024]), r=[("X2", i)], w=["WO"])
            P.dma(lambda e, l=l: e.dma_start(out=WSTG[0][:, 0:256], in_=pw_d[l].rearrange("p c n -> p (c n)")), w=[("X2", 0)], key="w0")
            P.op("pool", lambda e: e.tensor_copy(out=PWB[:].rearrange("p c n -> p (c n)"), in_=WSTG[0][:, 0:256]), r=[("X2", 0)], w=["PWB"])

            def load2(t):
                s = t % 2
                P.dma(lambda e, t=t, s=s: e.dma_start(out=H2[s][:], in_=hT_d[:, t * TT:(t + 1) * TT].rearrange("(kc p) n -> p kc n", p=128)),
                      r=["hT_d"], w=[("H2", s)], key=f"h2{s}")
                P.dma(lambda e, t=t, s=s: e.dma_start(out=X2[s][:], in_=xsrc[t * TT:(t + 1) * TT, :].rearrange("(s p) d -> p s d", p=128)),
                      w=[("X2", s)], key=f"x2{s}")

            P.cut(7)
            load2(0)
            for t in range(NT):
                s = t % 2
                if t + 1 < NT:
                    load2(t + 1)
                P.dma(lambda e, t=t: e.dma_start(out=YT[:, 0:2, :], in_=ya_d[:, t * TT:(t + 1) * TT].rearrange("(c p) n -> p c n", p=128)),
                      r=["ya_d"], w=[("YT", 0), ("YT", 1)], key="yal")

                def proj2(c0):
                    i = nxt("pj", 2)
                    for kc in range(8):
                        P.op("pe", lambda e, kc=kc, i=i: e.matmul(PJ[i][:], W2[:, kc, c0:c0 + 128], H2[s][:, kc, :], start=(kc == 0), stop=(kc == 7)),
                             r=["W2", ("H2", s)], w=[("PJ", i)], sig=(kc == 7))
                    return i

                for gi, c0 in enumerate((256, 384, 1024, 1152, 2048, 2176)):
                    i = proj2(c0)
                    P.op("act", lambda e, i=i, gi=gi: e.activation(out=GS[:, gi, :], in_=PJ[i][:], func=AF.Silu), r=[("PJ", i)], w=[("GS", gi)])

                P.cut(8)
                for c in range(2):
                    i = proj2(c * 128)
                    P.op("act", lambda e, i=i, c=c: e.activation(out=U[:, c, 16:528], in_=PJ[i][:], func=AF.Copy), r=[("PJ", i)], w=[("U", c)])
                    Uc = U[:, c, :]
                    P.op("pool", lambda e, Uc=Uc: e.tensor_tensor(out=T1[:, 1:528], in0=Uc[:, 1:528], in1=Uc[:, 0:527], op=ALU.add), r=[("U", c)], w=["T1"])
                    lo, hi = slice(0, 64), slice(64, 128)
                    if c == 0:
                        P.op("pool", lambda e: e.tensor_tensor(out=T2[hi, 3:528], in0=T1[hi, 3:528], in1=T1[hi, 1:526], op=ALU.add), r=["T1"], w=["T2"])
                    else:
                        P.op("pool", lambda e: e.tensor_tensor(out=T2[:, 3:528], in0=T1[:, 3:528], in1=T1[:, 1:526], op=ALU.add), r=["T1"], w=["T2"])
                        P.op("pool", lambda e: e.tensor_tensor(out=T1[:, 7:528], in0=T2[:, 7:528], in1=T2[:, 3:524], op=ALU.add), r=["T2"], w=["T1"])
                        P.op("pool", lambda e: e.tensor_tensor(out=T2[hi, 15:528], in0=T1[hi, 15:528], in1=T1[hi, 7:520], op=ALU.add), r=["T1"], w=["T2"])
                    for rows, src, sk in ((lo, T1, "T1"), (hi, T2, "T2")):
                        P.op("dve", lambda e, rows=rows, src=src, c=c, Uc=Uc: e.scalar_tensor_tensor(
                            out=PL[rows, c, :], in0=src[rows, 16:528], scalar=CF[rows, CF_IW + c:CF_IW + c + 1], in1=Uc[rows, 16:528],
                            op0=ALU.mult, op1=ALU.subtract), r=[sk, ("U", c), "CF"], w=[("PL", c)])
                        if t == 0:
                            P.op("dve", lambda e, rows=rows, src=src, c=c: e.tensor_tensor(out=TF[rows, :], in0=src[rows, 16:32],
                                                                                          in1=CF[rows, CF_TB + c * 16:CF_TB + (c + 1) * 16], op=ALU.mult),
                                 r=[sk, "CF"], w=["TF"])
                            P.op("dve", lambda e, rows=rows, c=c, Uc=Uc: e.tensor_tensor(out=PL[rows, c, 0:16], in0=TF[rows, :], in1=Uc[rows, 16:32], op=ALU.subtract),
                                 r=["TF", ("U", c)], w=[("PL", c)])
                    P.op("pool", lambda e, Uc=Uc: e.tensor_copy(out=Uc[:, 0:16], in_=Uc[:, 512:528]), r=["T1", "T2", ("PL", c)], w=[("U", c)])
                    P.op("pe", lambda e, c=c: e.matmul(msb[:], PWB[:, c, :], PL[:, c, :], start=True, stop=True), r=["PWB", ("PL", c)], w=["MS"])
                    P.op("dve", lambda e, c=c: e.scalar_tensor_tensor(out=YT[:, 2 + c, :], in0=msb[:], scalar=SPR[:, 12 + c:13 + c], in1=GS[:, c, :],
                                                                      op0=ALU.mult, op1=ALU.mult),
                         r=["MS", "SPR", ("GS", c)], w=[("YT", 2 + c)])

                P.cut(9)
                for c in range(2):
                    i = proj2(1280 + c * 128)
                    P.op("act", lambda e, i=i: e.activation(out=DH[:], in_=PJ[i][:], func=AF.Copy), r=[("PJ", i)], w=["DH"])
                    i = proj2(1792 + c * 128)
                    Zc = Z[:, c, :]
                    P.op("dve", lambda e, i=i, Zc=Zc: e.tensor_tensor(out=Zc[:, 2:514], in0=PJ[i][:], in1=DH[:], op=ALU.mult), r=[("PJ", i), "DH"], w=[("Z", c)])
                    wc = 14 + c * 3
                    P.op("dve", lambda e, Zc=Zc, wc=wc: e.tensor_scalar(out=ACC[:], in0=Zc[:, 2:514], scalar1=SPR[:, wc + 2:wc + 3], scalar2=None, op0=ALU.mult),
                         r=[("Z", c), "SPR"], w=["ACC"])
                    P.op("dve", lambda e, Zc=Zc, wc=wc: e.scalar_tensor_tensor(out=ACC[:], in0=Zc[:, 1:513], scalar=SPR[:, wc + 1:wc + 2], in1=ACC[:], op0=ALU.mult, op1=ALU.add),
                         r=[("Z", c), "SPR", "ACC"], w=["ACC"])
                    P.op("dve", lambda e, Zc=Zc, wc=wc: e.scalar_tensor_tensor(out=ACC[:], in0=Zc[:, 0:512], scalar=SPR[:, wc:wc + 1], in1=ACC[:], op0=ALU.mult, op1=ALU.add),
                         r=[("Z", c), "SPR", "ACC"], w=["ACC"])
                    P.op("pool", lambda e, Zc=Zc: e.tensor_copy(out=Zc[:, 0:2], in_=Zc[:, 512:514]), r=[("Z", c)], w=[("Z", c)])
                    i = proj2(1536 + c * 128)
                    P.op("dve", lambda e, i=i: e.tensor_tensor(out=ACC[:], in0=PJ[i][:], in1=ACC[:], op=ALU.mult), r=[("PJ", i), "ACC"], w=["ACC"])
                    P.op("pool", lambda e, c=c: e.tensor_tensor(out=YT[:, 6 + c, :], in0=ACC[:], in1=GS[:, 4 + c, :], op=ALU.mult),
                         r=["ACC", ("GS", 4 + c)], w=[("YT", 6 + c)])

                P.cut(10)
                i = proj2(768)
                ri = qk_norm_prep(PJ[i], ("PJ", i))
                for k in range(2):
                    rows = slice(k * 64, (k + 1) * 64)
                    P.op("dve", lambda e, i=i, ri=ri, k=k, rows=rows: e.scalar_tensor_tensor(
                        out=KC[k][0:64, 128:640], in0=PJ[i][rows, :], scalar=SPR[rows, 11:12], in1=RS[ri][rows, :], op0=ALU.mult, op1=ALU.mult),
                         r=[("PJ", i), ("RS", ri), "SPR"], w=[("KC", k)])
                for c in range(2):
                    i = proj2(512 + c * 128)
                    ri = qk_norm_prep(PJ[i], ("PJ", i))
                    for j in range(2):
                        h = 2 * c + j
                        rows = slice(j * 64, (j + 1) * 64)
                        P.op("dve", lambda e, i=i, ri=ri, h=h, rows=rows: e.scalar_tensor_tensor(
                            out=QC[h][0:64, :], in0=PJ[i][rows, :], scalar=SPR[rows, 10:11], in1=RS[ri][rows, :], op0=ALU.mult, op1=ALU.mult),
                             r=[("PJ", i), ("RS", ri), "SPR"], w=[("QC", h)])
                for sub in range(4):
                    i = nxt("pj", 2)
                    for kc in range(8):
                        P.op("pe", lambda e, kc=kc, i=i, sub=sub: e.matmul(PJ[i][:, 0:128], H2[s][:, kc, sub * 128:(sub + 1) * 128], W2[:, kc, 896:1024],
                                                                           start=(kc == 0), stop=(kc == 7)),
                             r=["W2", ("H2", s)], w=[("PJ", i)], sig=(kc == 7))
                    P.op("dve", lambda e, i=i, sub=sub: e.tensor_copy(out=VC[:, 1 + sub, :, 0:64], in_=PJ[i][:, 0:128].rearrange("p (k d) -> p k d", k=2)),
                         r=[("PJ", i)], w=["VC"])
                for h in range(4):
                    k = h // 2
                    for b in range(4):
                        g = 4 * t + b
                        lo = 0 if g > 0 else 128
                        pi = (h * 4 + b) % 4
                        qs = QC[h][0:65, b * 128:(b + 1) * 128]
                        P.op("pe", lambda e, h=h, lo=lo: e.matmul(SW[:, lo:256], ident, CB[:, CB_CMS + h * 256 + lo:CB_CMS + (h + 1) * 256], start=True, stop=False),
                             r=["CB"], w=["SW"], sig=False)
                        if g > 0:
                            P.op("pe", lambda e, k=k, b=b, qs=qs: e.matmul(SW[:, 0:128], KC[k][0:65, b * 128:(b + 1) * 128], qs, start=False, stop=False),
                                 r=[("KC", k), ("KCaug", k), ("QC", h), ("QCaug", h)], w=["SW"], sig=False)
                        P.op("pe", lambda e, k=k, b=b, qs=qs: e.matmul(SW[:, 128:256], KC[k][0:65, 128 + b * 128:128 + (b + 1) * 128], qs, start=False, stop=True),
                             r=[("KC", k), ("KCaug", k), ("QC", h), ("QCaug", h)], w=["SW"])
                        P.op("act", lambda e, pi=pi, lo=lo, h=h: e.activation(out=PS[pi][:, lo:256], in_=SW[:, lo:256], func=AF.Exp,
                                                                              bias=CF[:, CF_SB + h:CF_SB + h + 1], scale=0.125),
                             r=["SW", "CF"], w=[("PS", pi)])
                        oc = OC[:, b * 128:(b + 1) * 128]
                        if g > 0:
                            P.op("pe", lambda e, oc=oc, b=b, k=k, pi=pi: e.matmul(oc, VC[:, b, k, :], PS[pi][:, 0:128], start=True, stop=False),
                                 r=["VC", "VCones", ("PS", pi)], w=["OC"], sig=False)
                        P.op("pe", lambda e, oc=oc, b=b, k=k, pi=pi, g=g: e.matmul(oc, VC[:, 1 + b, k, :], PS[pi][:, 128:256], start=(g == 0), stop=True),
                             r=["VC", "VCones", ("PS", pi)], w=["OC"])
                    hr = slice((h % 2) * 64, (h % 2) * 64 + 64)
                    ci = nxt("rc", 2)
                    P.op("dve", lambda e, ci=ci, h=h, hr=hr: e.tensor_scalar(out=RC[ci][hr, :], in0=OC[64:128, :], scalar1=ESK[64:128, h:h + 1], scalar2=None, op0=ALU.add),
                         r=["OC", "ESK"], w=[("RC", ci)])
                    P.op("dve", lambda e, ci=ci, hr=hr: e.reciprocal(out=RC[ci][hr, :], in_=RC[ci][hr, :]), r=[("RC", ci)], w=[("RC", ci)])
                    P.op("dve", lambda e, ci=ci, hr=hr, h=h: e.tensor_tensor(out=RC[ci][hr, :], in0=RC[ci][hr, :], in1=GS[hr, 2 + h // 2, :], op=ALU.mult),
                         r=[("RC", ci), ("GS", 2 + h // 2)], w=[("RC", ci)])
                    P.op("dve", lambda e, ci=ci, hr=hr, h=h: e.tensor_tensor(out=YT[hr, 4 + h // 2, :], in0=OC[0:64, :], in1=RC[ci][hr, :], op=ALU.mult),
                         r=["OC", ("RC", ci)], w=[("YT", 4 + h // 2)])
                for k in range(2):
                    P.op("pool", lambda e, k=k: e.tensor_copy(out=KC[k][0:64, 0:128], in_=KC[k][0:64, 512:640]), r=[("KC", k)], w=[("KC", k)])
                P.op("pool", lambda e: e.tensor_copy(out=VC[:, 0, :, 0:64], in_=VC[:, 4, :, 0:64]), r=["VC"], w=["VC"])

                if t == 0 and l == 0:
                    dump("H20", H2[s][:, 0, :], [("H2", s)])
                    for c in range(8):
                        dump(f"YT{c}", YT[:, c, :], [("YT", c)])
                P.cut(11)
                for sub in range(4):
                    for hf in range(2):
                        oi = (sub * 2 + hf) % 2
                        for kc in range(8):
                            P.op("pe", lambda e, kc=kc, oi=oi, sub=sub, hf=hf: e.matmul(PO[oi][:], YT[:, kc, sub * 128:(sub + 1) * 128], WO[:, kc, hf * 512:(hf + 1) * 512],
                                                                                       start=(kc == 0), stop=(kc == 7)),
                                 r=["WO"] + [("YT", q) for q in range(8)], w=[("PO", oi)], sig=(kc == 7))
                        P.op("dve", lambda e, oi=oi, sub=sub, hf=hf: e.tensor_tensor(out=X2[s][:, sub, hf * 512:(hf + 1) * 512], in0=PO[oi][:],
                                                                                    in1=X2[s][:, sub, hf * 512:(hf + 1) * 512], op=ALU.add),
                             r=[("PO", oi), ("X2", s)], w=[("X2", s)])
                P.dma(lambda e, t=t, s=s: e.dma_start(out=xdst[t * TT:(t + 1) * TT, :].rearrange("(s p) d -> p s d", p=128), in_=X2[s][:]),
                      r=[("X2", s)], w=["xdst"], key=f"x2{s}")
            P.barrier()

        if DBG:
            P.stopped = False
            P.op("pool", lambda e: e.memset(DBGT[:, dbg_off[0]:12288], 0.0), w=["DBG"]) if dbg_off[0] < 12288 else None
            P.dma(lambda e: e.dma_start(out=dbg_d[:, :], in_=DBGT[:]), r=["DBG"], w=["dbg_d"], key="dbg")
            P.barrier()
        block = es.enter_context(nc.Block())
        P.emit(block)
    return nc


_CACHE = {}


def _host_layout(inputs, NL):
    f = lambda a: np.ascontiguousarray(np.asarray(a, dtype=np.float32))
    spr = np.zeros((NL, 128, NSPR), np.float32)
    pw = np.zeros((NL, 128, 2, 128), np.float32)
    for l in range(NL):
        spr[l, :, 0:8] = f(inputs["norm_g"])[l].reshape(8, 128).T
        spr[l, :, 8] = np.tile(f(inputs["a_q_norm"])[l], 2)
        spr[l, :, 9] = np.tile(f(inputs["a_k_norm"])[l], 2)
        spr[l, :, 10] = np.tile(f(inputs["c_q_norm"])[l], 2)
        spr[l, :, 11] = np.tile(f(inputs["c_k_norm"])[l], 2)
        spr[l, :, 12:14] = f(inputs["pool_scale"])[l].reshape(2, 128).T
        cw = f(inputs["conv_w"])[l]
        for c in range(2):
            for j in range(3):
                spr[l, :, 14 + c * 3 + j] = cw[j, c * 128:(c + 1) * 128]
        spr[l, :, 20:24] = f(inputs["c_sinks"])[l][None, :]
        pwl = f(inputs["pool_w"])[l]
        for c in range(2):
            for half in range(2):
                pw[l, half * 64:(half + 1) * 64, c, half * 64:(half + 1) * 64] = pwl[2 * c + half]
    return spr, pw


def run(inputs, S, NL, n_cores):
    key = (S, NL)
    if key not in _CACHE:
        _CACHE[key] = (build_program(S, NL), make_consts(S))
    nc, consts = _CACHE[key]
    spr, pw = _host_layout(inputs, NL)
    x = np.asarray(inputs["x"], dtype=np.float32)
    w_in = np.ascontiguousarray(np.asarray(inputs["w_in"], dtype=np.float32))
    w_out = np.ascontiguousarray(np.asarray(inputs["w_out"], dtype=np.float32))
    in_maps = []
    for c in range(n_cores):
        m = {"x": np.ascontiguousarray(x[c]), "w_in": w_in, "w_out": w_out, "spr": spr, "pw": pw}
        m.update(consts)
        in_maps.append(m)
    res = run_bass_kernel_spmd(nc, in_maps, core_ids=list(range(n_cores)))
    if "dbg" in res.results[0]:
        run.dbg = np.asarray(res.results[0]["dbg"]).astype(np.float32)
    return np.stack([np.asarray(r["out"], dtype=np.float32) for r in res.results], axis=0)


def kernel(x, norm_g, w_in, w_out, a_q_norm, a_k_norm, pool_w, pool_scale, c_q_norm, c_k_norm, c_sinks, conv_w):
    inputs = dict(x=x, norm_g=norm_g, w_in=w_in, w_out=w_out, a_q_norm=a_q_norm, a_k_norm=a_k_norm, pool_w=pool_w,
                  pool_scale=pool_scale, c_q_norm=c_q_norm, c_k_norm=c_k_norm, c_sinks=c_sinks, conv_w=conv_w)
    x = np.asarray(x)
    return run(inputs, x.shape[1], np.asarray(w_in).shape[0], x.shape[0])
```

```python
import contextlib
import types
import numpy as np
import ml_dtypes
import concourse.bass as bass
import concourse.mybir as mybir
from concourse.bass_utils import run_bass_kernel_spmd

F32 = mybir.dt.float32
BF16 = mybir.dt.bfloat16
AF = mybir.ActivationFunctionType
ALU = mybir.AluOpType

D = 1024
NIN = 3328
TT = 512
BIG = 30000.0
EPS = 1e-6
NSPR = 24
ENGS = ("pe", "act", "dve", "pool", "sp")
BLK = {"pe": "tensor", "act": "scalar", "dve": "vector", "pool": "gpsimd", "sp": "sync"}

CB_ID, CB_OB, CB_CM, CB_CMS, CB_N = 0, 128, 256, 256 + 2048, 256 + 2048 + 1024
CF_AB, CF_SB, CF_IW, CF_TB, CF_N = 0, 256, 260, 262, 262 + 32


def slopes_all():
    return np.exp2(-(8.0 / 8) * np.arange(1, 9, dtype=np.float32)).astype(np.float32)


def make_consts(S):
    sl = slopes_all()
    sl_c, sl_a = sl[:4], sl[4:]
    cb = np.zeros((128, CB_N), np.float32)
    cb[:, CB_ID:CB_ID + 128] = np.eye(128)
    cb[0:64, CB_OB:CB_OB + 64] = 1.0
    cb[64:128, CB_OB + 64:CB_OB + 128] = 1.0
    kl = np.arange(128)[:, None]
    ql = np.arange(512)[None, :]
    for kt in range(4):
        m = ((ql // 256) == (kt // 2)) & (ql < 128 * kt + kl)
        cb[:, CB_CM + kt * 512:CB_CM + (kt + 1) * 512] = np.where(m, -BIG, 0.0)
    q1 = np.arange(128)[None, :]
    for h in range(4):
        prev = np.where(kl > q1, -8.0 * sl_c[h] * 128.0, -BIG)
        own = np.where(kl <= q1, 0.0, -BIG)
        cb[:, CB_CMS + h * 256:CB_CMS + h * 256 + 128] = prev
        cb[:, CB_CMS + h * 256 + 128:CB_CMS + (h + 1) * 256] = own
    cf = np.zeros((128, CF_N), np.float32)
    for h in range(4):
        for i in range(64):
            d = 3 - i
            cf[:, CF_AB + h * 64 + i] = sl_a[h] * (np.arange(128) - 511 + 128 * d)
        cf[:, CF_SB + h] = sl_c[h] * np.arange(128)
    wins = (2, 4, 8, 16)
    for c in range(2):
        for half in range(2):
            w = wins[2 * c + half]
            rows = slice(half * 64, (half + 1) * 64)
            cf[rows, CF_IW + c] = 1.0 / w
            cf[rows, CF_TB + c * 16:CF_TB + (c + 1) * 16] = 1.0 / np.minimum(np.arange(16) + 1, w)
    khot = np.zeros((32, S), np.float32)
    for n in range(S // 256):
        khot[n, n * 256:(n + 1) * 256] = 1.0
    cq = np.zeros((4, 512), np.float32)
    for h in range(4):
        cq[h] = -8.0 * sl_c[h] * (np.arange(512) % 128)
    bf = ml_dtypes.bfloat16
    return {"cb": cb.astype(bf), "cf": cf, "khot": khot.astype(bf), "cqrow": cq.astype(bf)}


def _freeze(fn):
    if fn is None or fn.__closure__ is None:
        return fn
    cells = []
    for c in fn.__closure__:
        try:
            cells.append(types.CellType(c.cell_contents))
        except ValueError:
            cells.append(c)
    g = types.FunctionType(fn.__code__, fn.__globals__, fn.__name__, fn.__defaults__, tuple(cells))
    g.__kwdefaults__ = fn.__kwdefaults__
    return g


class Prog:
    def __init__(self, nc, es):
        self.nc = nc
        self.es = es
        self.ops = {e: [] for e in ENGS}
        self.cnt = {e: 0 for e in ENGS}
        self.semh = {}
        self.dcnt = {}
        self.lastw = {}
        self.readers = {}
        self.waited = {e: {} for e in ENGS}
        for e in ENGS:
            self.semh["E:" + e] = es.enter_context(nc.semaphore("sem_" + e))

    def _collect(self, eng, r, w):
        need = {}

        def add(ev):
            semid, val, src = ev
            if src == eng and eng == "pe":
                return
            if self.waited[eng].get(semid, 0) >= val:
                return
            if need.get(semid, 0) < val:
                need[semid] = val

        for k in r:
            if k in self.lastw:
                add(self.lastw[k])
        for k in w:
            if k in self.lastw:
                add(self.lastw[k])
            for ev in self.readers.get(k, {}).values():
                add(ev)
        for semid, val in need.items():
            self.waited[eng][semid] = val
        return list(need.items())

    def _commit(self, ev, r, w):
        for k in r:
            d = self.readers.setdefault(k, {})
            if ev[0] not in d or d[ev[0]][1] < ev[1]:
                d[ev[0]] = ev
        for k in w:
            self.lastw[k] = ev
            self.readers[k] = {}

    stopped = False

    def cut(self, k):
        import os
        if int(os.environ.get("STAGE", "99")) == k and not self.stopped:
            self.barrier()
            self.stopped = True

    def op(self, eng, fn, r=(), w=(), sig=True):
        if self.stopped:
            return
        fn = _freeze(fn)
        waits = self._collect(eng, r, w)
        semid = "E:" + eng
        if sig:
            self.cnt[eng] += 1
            ev = (semid, self.cnt[eng], eng)
            inc = (semid, 1)
        else:
            ev = (semid, self.cnt[eng] + 1, eng)
            inc = None
        self.ops[eng].append((waits, fn, inc))
        self._commit(ev, r, w)

    def dma(self, fn, r=(), w=(), key=None, q="sp"):
        if self.stopped:
            return
        fn = _freeze(fn)
        waits = self._collect(q, r, w)
        semid = "D:" + key
        if semid not in self.semh:
            self.semh[semid] = self.es.enter_context(self.nc.semaphore("dsem_" + key))
            self.dcnt[semid] = 0
        self.dcnt[semid] += 16
        ev = (semid, self.dcnt[semid], "dma")
        self.ops[q].append((waits, fn, (semid, 16)))
        self._commit(ev, r, w)

    def dma_group(self, key, items, q="sp"):
        if self.stopped:
            return
        semid = "D:" + key
        if semid not in self.semh:
            self.semh[semid] = self.es.enter_context(self.nc.semaphore("dsem_" + key))
            self.dcnt[semid] = 0
        total = self.dcnt[semid] + 16 * len(items)
        ev = (semid, total, "dma")
        for fn, r, w in items:
            waits = self._collect(q, r, w)
            self.ops[q].append((waits, _freeze(fn), (semid, 16)))
        for fn, r, w in items:
            self._commit(ev, r, w)
        self.dcnt[semid] = total

    def barrier(self):
        if self.stopped:
            return
        for e in ENGS:
            waits = []
            for o in ENGS:
                if o == e or self.cnt[o] == 0:
                    continue
                sid = "E:" + o
                if self.waited[e].get(sid, 0) < self.cnt[o]:
                    waits.append((sid, self.cnt[o]))
                    self.waited[e][sid] = self.cnt[o]
            for sid, c in self.dcnt.items():
                if self.waited[e].get(sid, 0) < c:
                    waits.append((sid, c))
                    self.waited[e][sid] = c
            if waits:
                self.ops[e].append((waits, None, None))
        self.lastw = {}
        self.readers = {}

    def emit(self, block):
        for eng in ENGS:
            def body(e, eng=eng):
                for waits, fn, inc in self.ops[eng]:
                    for semid, val in waits:
                        e.wait_ge(self.semh[semid], val)
                    if fn is None:
                        continue
                    ins = fn(e)
                    if inc is not None:
                        ins.then_inc(self.semh[inc[0]], inc[1])
            getattr(block, BLK[eng])(body)


class Arena:
    def __init__(self, nc, base, top):
        self.nc, self.base, self.top, self.cur, self.n = nc, base, top, base, 0

    def alloc(self, name, shape, dtype):
        nbytes = int(np.prod(shape[1:])) * (2 if dtype == BF16 else 4)
        nbytes = (nbytes + 63) // 64 * 64
        off = self.cur
        assert off + nbytes <= self.top, f"SBUF overflow at {name}: {off + nbytes} > {self.top}"
        self.cur += nbytes
        self.n += 1
        return self.nc.alloc_sbuf_tensor_at(f"{name}_{self.n}", list(shape), dtype, offset=off)


def build_program(S, NL):
    NT = S // TT
    NKT = S // 128
    nc = bass.Bass("TRN2", target_bir_lowering=False)
    dt = nc.dram_tensor
    x_in = dt("x", [S, D], F32, kind="ExternalInput").ap()
    w_in = dt("w_in", [NL, D, NIN], F32, kind="ExternalInput").ap()
    w_out = dt("w_out", [NL, D, D], F32, kind="ExternalInput").ap()
    spr_d = dt("spr", [NL, 128, NSPR], F32, kind="ExternalInput").ap()
    pw_d = dt("pw", [NL, 128, 2, 128], F32, kind="ExternalInput").ap()
    cb_d = dt("cb", [128, CB_N], BF16, kind="ExternalInput").ap()
    cf_d = dt("cf", [128, CF_N], F32, kind="ExternalInput").ap()
    khot_d = dt("khot", [32, S], BF16, kind="ExternalInput").ap()
    cq_d = dt("cqrow", [4, 512], BF16, kind="ExternalInput").ap()
    out_d = dt("out", [S, D], F32, kind="ExternalOutput").ap()
    hT_d = dt("hT_scr", [D, S], BF16, kind="Internal").ap()
    ya_d = dt("ya_scr", [256, S], BF16, kind="Internal").ap()
    x1_d = dt("x1_scr", [S, D], F32, kind="Internal").ap() if NL > 1 else None

    with contextlib.ExitStack() as es:
        P = Prog(nc, es)
        a_base = (int(nc.sbuf_base) + 63) // 64 * 64
        a_size = int(nc.sbuf_top) - a_base - 2048
        es.enter_context(nc.sbuf_tensor("arena_slab", [128, a_size], mybir.dt.uint8))
        A = Arena(nc, a_base, a_base + a_size)
        pb = [es.enter_context(nc.psum_tensor(f"pb{i}", [128, 512], F32)) for i in range(6)]
        tpb = es.enter_context(nc.psum_tensor("tpb", [128, 512], F32))
        msb = es.enter_context(nc.psum_tensor("msb", [128, 512], F32))
        CB = A.alloc("CB", [128, CB_N], BF16)
        CF = A.alloc("CF", [128, CF_N], F32)
        SPR = A.alloc("SPR", [128, NSPR], F32)
        ESK = A.alloc("ESK", [128, 4], F32)
        SQ = [A.alloc(f"SQ{i}", [128, 512], BF16) for i in range(2)]
        RS = [A.alloc(f"RS{i}", [128, 512], F32) for i in range(2)]
        RC = [A.alloc(f"RC{i}", [128, 512], F32) for i in range(2)]
        ident = CB[:, CB_ID:CB_ID + 128]
        onesblk = CB[:, CB_OB:CB_OB + 128]
        import os
        DBG = os.environ.get("DBG") == "1"
        dbg_off = [0]
        dbg_names = []
        if DBG:
            dbg_d = dt("dbg", [128, 12288], BF16, kind="ExternalOutput").ap()
            DBGT = A.alloc("DBGT", [128, 12288], BF16)

        def dump(name, ap, r, npart=128):
            if not DBG or P.stopped:
                return
            n = ap.shape[-1] if len(ap.shape) == 2 else int(np.prod(ap.shape[1:]))
            o = dbg_off[0]
            dbg_names.append((name, o, n, npart))
            dbg_off[0] += n
            assert dbg_off[0] <= 12288
            P.op("dve", lambda e: e.tensor_copy(out=DBGT[0:npart, o:o + n], in_=ap), r=r, w=["DBG"])
        build_program.dbg_names = dbg_names
        mark = A.cur

        first_items = [(lambda e: e.dma_start(out=CB[:], in_=cb_d[:, :]), [], ["CB"]),
                       (lambda e: e.dma_start(out=CF[:], in_=cf_d[:, :]), [], ["CF"])]

        rr = {"sq": 0, "pj": 0, "rc": 0}

        def nxt(name, n):
            rr[name] = (rr[name] + 1) % n
            return rr[name]

        def qk_norm_prep(pj, pjk):
            i = nxt("sq", 2)
            P.op("act", lambda e: e.activation(out=SQ[i][:], in_=pj[:], func=AF.Square), r=[pjk], w=[("SQ", i)])
            P.op("pe", lambda e: e.matmul(msb[:], onesblk, SQ[i][:], start=True, stop=True),
                 r=[("SQ", i), "CB"], w=["MS"])
            P.op("act", lambda e: e.activation(out=RS[i][:], in_=msb[:], func=AF.Ln, bias=EPS, scale=1.0 / 64),
                 r=["MS"], w=[("RS", i)])
            P.op("act", lambda e: e.activation(out=RS[i][:], in_=RS[i][:], func=AF.Exp, scale=-0.5),
                 r=[("RS", i)], w=[("RS", i)])
            return i

        def qk_norm_prep_gen(pj, pjk):
            i = nxt("sq", 2)
            P.op("act", lambda e: e.activation(out=SQ[i][:], in_=pj[:], func=AF.Square), r=[pjk], w=[("SQ", i)])
            yield
            P.op("pe", lambda e: e.matmul(msb[:], onesblk, SQ[i][:], start=True, stop=True),
                 r=[("SQ", i), "CB"], w=["MS"])
            yield
            P.op("act", lambda e: e.activation(out=RS[i][:], in_=msb[:], func=AF.Ln, bias=EPS, scale=1.0 / 64),
                 r=["MS"], w=[("RS", i)])
            P.op("act", lambda e: e.activation(out=RS[i][:], in_=RS[i][:], func=AF.Exp, scale=-0.5),
                 r=[("RS", i)], w=[("RS", i)])
            yield
            return i

        for l in range(NL):
            xsrc = x_in if l == 0 else x1_d
            xdst = out_d if l == NL - 1 else x1_d
            items = first_items if l == 0 else []
            items.append((lambda e, l=l: e.dma_start(out=SPR[:], in_=spr_d[l, :, :]), [], ["SPR"]))

            A.cur = mark
            WA = A.alloc("WA", [128, 8, 1024], BF16)
            KA = [A.alloc(f"KA{h}", [96, S], BF16) for h in range(4)]
            VA = A.alloc("VA", [128, NKT, 384], BF16)
            KM = A.alloc("KM", [128, 2, 64], F32)
            wbase = A.cur
            XT = A.alloc("XT", [128, 4, 1024], F32)
            XS = A.alloc("XS", [128, 4, 1024], BF16)
            WSTG = [nc.alloc_sbuf_tensor_at(f"WSTGa{l}_{i}", [128, 2304], F32, offset=wbase + i * 9216) for i in range(2)]
            HT = [A.alloc(f"HT{i}", [128, 8, 512], BF16) for i in range(2)]
            QF = A.alloc("QF", [128, 2, 512], F32)
            QA = [[A.alloc(f"QA{h}_{s}", [96, 512], BF16) for s in range(1)] * 2 for h in range(4)]
            GA = [A.alloc("GA", [128, 2, 512], BF16)] * 2
            PT = [A.alloc(f"PT{i}", [128, 512], BF16) for i in range(4)]
            YA = [A.alloc("YA", [128, 2, 512], BF16)] * 2
            SSX = A.alloc("SSX", [128, 4], F32)
            RSX = A.alloc("RSX", [128, 4], F32)
            BSM = A.alloc("BSM", [128, 4, 32], F32)
            M8 = A.alloc("M8", [128, 4, 8], F32)
            THR = A.alloc("THR", [128, 4], F32)
            MB = A.alloc("MB", [128, 4, 32], BF16)
            PJ = pb[0:1]
            ST = pb[1:4]
            OT = pb[4:6]

            for h in range(4):
                items.append((lambda e, h=h: e.dma_start(out=KA[h][64:96, :], in_=khot_d[:, :]), [], [("KAaug", h)]))
            P.dma_group("const", items)
            P.op("act", lambda e: e.activation(out=ESK[:], in_=SPR[:, 20:24], func=AF.Exp), r=["SPR"], w=["ESK"])
            P.op("pool", lambda e: e.memset(VA[:, :, 64:128], 1.0), w=["VAones"])
            P.op("pool", lambda e: e.memset(VA[:, :, 256:320], 1.0), w=["VAones"])
            P.op("pool", lambda e: e.memset(KM[:], 0.0), w=["KM"])
            for kc in range(8):
                i = kc % 2
                P.dma(lambda e, kc=kc, i=i, l=l: e.dma_start(out=WSTG[i][:, 0:1024], in_=w_in[l, kc * 128:(kc + 1) * 128, 0:1024]),
                      w=[("WSTG", i)], key=f"w{i}")
                P.op("dve", lambda e, kc=kc, i=i: e.tensor_scalar(out=WA[:, kc, :], in0=WSTG[i][:, 0:1024], scalar1=SPR[:, kc:kc + 1],
                                                                  scalar2=None, op0=ALU.mult),
                     r=[("WSTG", i), "SPR"], w=["WA", "XTa", "XS"])

            def load_x(t):
                P.dma(lambda e, t=t: e.dma_start(out=XT[:], in_=xsrc[t * TT:(t + 1) * TT, :].rearrange("(s p) d -> p s d", p=128)),
                      r=["XTa"], w=["XT"], key="xt")

            P.cut(1)
            load_x(0)
            def proj(c0, s):
                i = nxt("pj", len(PJ))
                for kc in range(8):
                    P.op("pe", lambda e, kc=kc, i=i: e.matmul(PJ[i][:], WA[:, kc, c0:c0 + 128], HT[s][:, kc, :], start=(kc == 0), stop=(kc == 7)),
                         r=["WA", ("HT", s)], w=[("PJ", i)], sig=(kc == 7))
                return i

            def stage1(t):
                s = t % 2
                for sub in range(4):
                    P.op("act", lambda e, sub=sub: e.activation(out=XS[:, sub, :], in_=XT[:, sub, :], func=AF.Square,
                                                                accum_out=SSX[:, sub:sub + 1]),
                         r=["XT"], w=["XS", "SSX"])
                    yield
                P.op("act", lambda e: e.activation(out=RSX[:], in_=SSX[:], func=AF.Ln, bias=EPS, scale=1.0 / D), r=["SSX"], w=["RSX"])
                P.op("act", lambda e: e.activation(out=RSX[:], in_=RSX[:], func=AF.Exp, scale=-0.5), r=["RSX"], w=["RSX"])
                yield
                for sub in range(4):
                    P.op("dve", lambda e, sub=sub: e.tensor_scalar(out=XS[:, sub, :], in0=XT[:, sub, :], scalar1=RSX[:, sub:sub + 1],
                                                                   scalar2=None, op0=ALU.mult),
                         r=["XT", "RSX"], w=["XS"])
                    yield
                if t + 1 < NT:
                    load_x(t + 1)
                for kc in range(8):
                    hf = kc % 2
                    bank, bkey = (tpb, ("TP", 0)) if hf == 0 else (msb, "MS")
                    for sub in range(4):
                        P.op("pe", lambda e, kc=kc, sub=sub, bank=bank: e.matmul(bank[:, sub * 128:(sub + 1) * 128], XS[:, sub, kc * 128:(kc + 1) * 128], ident,
                                                                                 start=True, stop=True),
                             r=["XS", "CB"], w=[bkey], sig=(sub == 3))
                    P.op("dve", lambda e, kc=kc, bank=bank: e.tensor_copy(out=HT[s][:, kc, :], in_=bank[:, 0:512]),
                         r=[bkey], w=[("HT", s)])
                    yield
                P.dma(lambda e, t=t, s=s: e.dma_start(out=hT_d[:, t * TT:(t + 1) * TT].rearrange("(kc p) n -> p kc n", p=128), in_=HT[s][:]),
                      r=[("HT", s)], w=["hT_d"], key=f"hts{s}")


                for c in range(2):
                    i = proj(256 + c * 128, s)
                    yield
                    ri = yield from qk_norm_prep_gen(PJ[i], ("PJ", i))
                    for j in range(2):
                        h = 2 * c + j
                        rows = slice(j * 64, (j + 1) * 64)
                        for b in range(2):
                            n = 2 * t + b
                            P.op("dve", lambda e, i=i, ri=ri, h=h, rows=rows, b=b, n=n: e.scalar_tensor_tensor(
                                out=KA[h][0:64, t * TT + b * 256:t * TT + (b + 1) * 256], in0=PJ[i][rows, b * 256:(b + 1) * 256],
                                scalar=SPR[rows, 9:10], in1=RS[ri][rows, b * 256:(b + 1) * 256], op0=ALU.mult, op1=ALU.mult,
                                accum_out=KM[rows, c, j * 32 + n:j * 32 + n + 1]),
                                 r=[("PJ", i), ("RS", ri), "SPR"], w=[("KA", h, t), "KM"])
                        yield
                for sub in range(4):
                    i = nxt("pj", len(PJ))
                    for kc in range(8):
                        P.op("pe", lambda e, kc=kc, i=i, sub=sub: e.matmul(PJ[i][:, 0:256], HT[s][:, kc, sub * 128:(sub + 1) * 128], WA[:, kc, 512:768],
                                                                           start=(kc == 0), stop=(kc == 7)),
                             r=["WA", ("HT", s)], w=[("PJ", i)], sig=(kc == 7))
                    yield
                    kt = 4 * t + sub
                    for (d0, s0, wd) in ((0, 0, 64), (128, 64, 128), (320, 192, 64)):
                        P.op("dve", lambda e, i=i, kt=kt, d0=d0, s0=s0, wd=wd: e.tensor_copy(out=VA[:, kt, d0:d0 + wd], in_=PJ[i][:, s0:s0 + wd]),
                             r=[("PJ", i)], w=[("VA", t)])
                    yield

            def stage2(t):
                s = t % 2
                for c in range(2):
                    i = proj(c * 128, s)
                    ri = qk_norm_prep(PJ[i], ("PJ", i))
                    P.op("dve", lambda e, i=i, ri=ri, c=c: e.scalar_tensor_tensor(
                        out=QF[:, c, :], in0=PJ[i][:], scalar=SPR[:, 8:9], in1=RS[ri][:], op0=ALU.mult, op1=ALU.mult),
                         r=[("PJ", i), ("RS", ri), "SPR"], w=[("QF", c)])
                    P.op("pool", lambda e, c=c: e.tensor_copy(out=QA[2 * c][0][0:64, :], in_=QF[0:64, c, :]),
                         r=[("QF", c)], w=[("QA", 2 * c, 0)])
                    P.op("dve", lambda e, c=c: e.tensor_copy(out=QA[2 * c + 1][0][0:64, :], in_=QF[64:128, c, :]),
                         r=[("QF", c)], w=[("QA", 2 * c + 1, 0)])
                for c in range(2):
                    i = proj(768 + c * 128, s)
                    P.op("act", lambda e, i=i, c=c: e.activation(out=GA[0][:, c, :], in_=PJ[i][:], func=AF.Silu),
                         r=[("PJ", i)], w=[("GA", 0, c)])
                for sub in range(4):
                    own = 2 * t + sub // 2
                    for c in range(2):
                        P.op("pe", lambda e, c=c, sub=sub: e.matmul(msb[:, c * 64:(c + 1) * 64], QF[:, c, sub * 128:(sub + 1) * 128], KM[:, c, :],
                                                                    start=True, stop=True),
                             r=[("QF", c), "KM"], w=["MS"], sig=(c == 1))
                    P.op("pool", lambda e: e.memset(BSM[:], -1e30), w=["BSM"])
                    if own > 0:
                        P.op("dve", lambda e, own=own: e.tensor_copy(out=BSM[:, :, 0:own],
                                                                     in_=msb[:, 0:128].rearrange("p (h n) -> p h n", h=4)[:, :, 0:own]),
                             r=["MS"], w=["BSM"])
                    for h in range(4):
                        P.op("dve", lambda e, h=h: e.max(out=M8[:, h, :], in_=BSM[:, h, :]), r=["BSM"], w=["M8"])
                    P.op("dve", lambda e: e.tensor_scalar(out=THR[:], in0=M8[:, :, 2], scalar1=-1e29, scalar2=None, op0=ALU.max),
                         r=["M8"], w=["THR"])
                    for h in range(4):
                        P.op("dve", lambda e, h=h: e.tensor_scalar(out=MB[:, h, :], in0=BSM[:, h, :], scalar1=THR[:, h:h + 1], scalar2=-BIG,
                                                                   op0=ALU.is_lt, op1=ALU.mult),
                             r=["BSM", "THR"], w=["MB"])
                    P.op("dve", lambda e, own=own: e.memset(MB[:, :, own:own + 1], 0.0), w=["MB"])
                    P.op("pe", lambda e: e.matmul(tpb[:, 0:128], MB[:].rearrange("p h n -> p (h n)"), ident, start=True, stop=True), r=["MB", "CB"], w=[("TP", 0)])
                    for h in range(4):
                        P.op("dve", lambda e, h=h, sub=sub: e.tensor_copy(out=QA[h][0][64:96, sub * 128:(sub + 1) * 128], in_=tpb[h * 32:(h + 1) * 32, 0:128]),
                             r=[("TP", 0)], w=[("QAm", h, 0)])


            def moba(t, hook):
                s = t % 2
                for h in range(4):
                    oslot = h % 2
                    nkt = 4 * t + 4
                    vc0 = (0, 64, 192, 256)[h]
                    pend = []
                    for kt in range(nkt):
                        si = kt % 3
                        diag = kt >= 4 * t
                        P.op("pe", lambda e, h=h, kt=kt, si=si, diag=diag: e.matmul(ST[si][:], KA[h][0:96, kt * 128:(kt + 1) * 128], QA[h][0][0:96, :],
                                                                                    start=True, stop=not diag),
                             r=[("KA", h, kt // 4), ("KAaug", h), ("QA", h, 0), ("QAm", h, 0)], w=[("ST", si)], sig=not diag)
                        if diag:
                            dk = kt - 4 * t
                            P.op("pe", lambda e, si=si, dk=dk: e.matmul(ST[si][:], ident, CB[:, CB_CM + dk * 512:CB_CM + (dk + 1) * 512], start=False, stop=True),
                                 r=["CB"], w=[("ST", si)])
                        pi = kt % 4
                        col = CF_AB + h * 64 + (3 - (kt - 4 * t))
                        P.op("act", lambda e, si=si, pi=pi, col=col: e.activation(out=PT[pi][:], in_=ST[si][:], func=AF.Exp, bias=CF[:, col:col + 1], scale=0.125),
                             r=[("ST", si), "CF"], w=[("PT", pi)])
                        hook()

                        def pv(h=h, kt=kt, pi=pi, oslot=oslot, vc0=vc0, nkt=nkt):
                            P.op("pe", lambda e: e.matmul(OT[oslot][:], VA[:, kt, vc0:vc0 + 128], PT[pi][:], start=(kt == 0), stop=(kt == nkt - 1)),
                                 r=[("VA", kt // 4), "VAones", ("PT", pi)], w=[("OT", oslot)], sig=True)
                        pend.append(pv)
                        if len(pend) > 2:
                            pend.pop(0)()
                    for f_ in pend:
                        f_()
                    nr = slice(0, 64) if h % 2 == 0 else slice(64, 128)
                    dr = slice(64, 128) if h % 2 == 0 else slice(0, 64)
                    hr = slice((h % 2) * 64, (h % 2) * 64 + 64)
                    ci = nxt("rc", 2)
                    P.op("dve", lambda e, oslot=oslot, dr=dr, ci=ci, hr=hr: e.reciprocal(out=RC[ci][hr, :], in_=OT[oslot][dr, :]), r=[("OT", oslot)], w=[("RC", ci)])
                    P.op("dve", lambda e, ci=ci, hr=hr, h=h: e.tensor_tensor(out=RC[ci][hr, :], in0=RC[ci][hr, :], in1=GA[0][hr, h // 2, :], op=ALU.mult),
                         r=[("RC", ci), ("GA", 0, h // 2)], w=[("RC", ci)])
                    P.op("dve", lambda e, oslot=oslot, nr=nr, ci=ci, hr=hr, h=h: e.tensor_tensor(out=YA[0][hr, h // 2, :], in0=OT[oslot][nr, :], in1=RC[ci][hr, :], op=ALU.mult),
                         r=[("OT", oslot), ("RC", ci)], w=[("YA", 0)])
                P.dma(lambda e, t=t, s=s: e.dma_start(out=ya_d[:, t * TT:(t + 1) * TT].rearrange("(c p) n -> p c n", p=128), in_=YA[0][:]),
                      r=[("YA", 0)], w=["ya_d"], key="yas")

            for _ in stage1(0):
                pass
            stage2(0)
            for t in range(NT):
                gen = stage1(t + 1) if t + 1 < NT else iter(())

                def hook(gen=gen):
                    next(gen, None)
                moba(t, hook)
                for _ in gen:
                    pass
                if t + 1 < NT:
                    stage2(t + 1)
            P.barrier()
            P.cut(6)

            A.cur = mark
            W2 = A.alloc("W2", [128, 8, 2304], BF16)
            WO = A.alloc("WO", [128, 8, 1024], BF16)
            PWB = A.alloc("PWB", [128, 2, 128], BF16)
            H2 = [A.alloc(f"H2{i}", [128, 8, 512], BF16) for i in range(2)]
            wbase = A.cur
            X2 = [A.alloc(f"X2{i}", [128, 4, 1024], F32) for i in range(2)]
            WSTG = [nc.alloc_sbuf_tensor_at(f"WSTGb{l}_{i}", [128, 2304], F32, offset=wbase + i * 16384) for i in range(2)]
            YT = A.alloc("YT", [128, 8, 512], BF16)
            GS = A.alloc("GS", [128, 6, 512], BF16)
            U = A.alloc("U", [128, 2, 528], F32)
            T1 = A.alloc("T1", [128, 528], F32)
            T2 = A.alloc("T2", [128, 528], F32)
            TF = A.alloc("TF", [128, 16], F32)
            PL = A.alloc("PL", [128, 2, 512], BF16)
            DH = A.alloc("DH", [128, 512], F32)
            Z = A.alloc("Z", [128, 2, 514], F32)
            ACC = A.alloc("ACC", [128, 512], F32)
            QC = [A.alloc(f"QC{h}", [65, 512], BF16) for h in range(4)]
            KC = [A.alloc(f"KC{k}", [65, 640], BF16) for k in range(2)]
            VC = A.alloc("VC", [128, 5, 2, 128], BF16)
            PS = [A.alloc(f"PS{i}", [128, 256], BF16) for i in range(4)]
            PJ = pb[0:2]
            SW = pb[2]
            OC = pb[3]
            PO = pb[4:6]

            P.op("pool", lambda e: e.memset(U[:], 0.0), w=[("U", 0), ("U", 1)])
            P.op("pool", lambda e: e.memset(Z[:], 0.0), w=[("Z", 0), ("Z", 1)])
            P.op("pool", lambda e: e.memset(VC[:, :, :, 64:128], 1.0), w=["VCones"])
            P.op("pool", lambda e: e.memset(VC[:, 0, :, 0:64], 0.0), w=["VC"])
            for k in range(2):
                P.op("pool", lambda e, k=k: e.memset(KC[k][64:65, :], 1.0), w=[("KCaug", k)])
                P.op("pool", lambda e, k=k: e.memset(KC[k][0:64, 0:128], 0.0), w=[("KC", k)])
            P.dma_group("const2", [(lambda e, h=h: e.dma_start(out=QC[h][64:65, :], in_=cq_d[h:h + 1, :]), [], [("QCaug", h)]) for h in range(4)])
            for kc in range(8):
                i = kc % 2
                P.dma(lambda e, kc=kc, i=i, l=l: e.dma_start(out=WSTG[i][:], in_=w_in[l, kc * 128:(kc + 1) * 128, 1024:3328]),
                      w=[("X2", i)], key=f"w{i}")
                P.op("dve", lambda e, kc=kc, i=i: e.tensor_scalar(out=W2[:, kc, :], in0=WSTG[i][:], scalar1=SPR[:, kc:kc + 1], scalar2=None, op0=ALU.mult),
                     r=[("X2", i), "SPR"], w=["W2"])
            for kc in range(8):
                i = kc % 2
                P.dma(lambda e, kc=kc, i=i, l=l: e.dma_start(out=WSTG[i][:, 0:1024], in_=w_out[l, kc * 128:(kc + 1) * 128, :]),
                      w=[("X2", i)], key=f"w{i}")
                P.op("pool", lambda e, kc=kc, i=i: e.tensor_copy(out=WO[:, kc, :], in_=WSTG[i][:, 0:1024]), r=[("X2", i)], w=["WO"])
            P.dma(lambda e, l=l: e.dma_start(out=WSTG[0][:, 0:256], in_=pw_d[l].rearrange("p c n -> p (c n)")), w=[("X2", 0)], key="w0")
            P.op("pool", lambda e: e.tensor_copy(out=PWB[:].rearrange("p c n -> p (c n)"), in_=WSTG[0][:, 0:256]), r=[("X2", 0)], w=["PWB"])

            def load2(t):
                s = t % 2
                P.dma(lambda e, t=t, s=s: e.dma_start(out=H2[s][:], in_=hT_d[:, t * TT:(t + 1) * TT].rearrange("(kc p) n -> p kc n", p=128)),
                      r=["hT_d"], w=[("H2", s)], key=f"h2{s}")
                P.dma(lambda e, t=t, s=s: e.dma_start(out=X2[s][:], in_=xsrc[t * TT:(t + 1) * TT, :].rearrange("(s p) d -> p s d", p=128)),
                      w=[("X2", s)], key=f"x2{s}")

            P.cut(7)
            load2(0)
            for t in range(NT):
                s = t % 2
                if t + 1 < NT:
                    load2(t + 1)
                P.dma(lambda e, t=t: e.dma_start(out=YT[:, 0:2, :], in_=ya_d[:, t * TT:(t + 1) * TT].rearrange("(c p) n -> p c n", p=128)),
                      r=["ya_d"], w=[("YT", 0), ("YT", 1)], key="yal")

                def proj2(c0):
                    i = nxt("pj", 2)
                    for kc in range(8):
                        P.op("pe", lambda e, kc=kc, i=i: e.matmul(PJ[i][:], W2[:, kc, c0:c0 + 128], H2[s][:, kc, :], start=(kc == 0), stop=(kc == 7)),
                             r=["W2", ("H2", s)], w=[("PJ", i)], sig=(kc == 7))
                    return i

                for gi, c0 in enumerate((256, 384, 1024, 1152, 2048, 2176)):
                    i = proj2(c0)
                    P.op("act", lambda e, i=i, gi=gi: e.activation(out=GS[:, gi, :], in_=PJ[i][:], func=AF.Silu), r=[("PJ", i)], w=[("GS", gi)])

                P.cut(8)
                for c in range(2):
                    i = proj2(c * 128)
                    P.op("act", lambda e, i=i, c=c: e.activation(out=U[:, c, 16:528], in_=PJ[i][:], func=AF.Copy), r=[("PJ", i)], w=[("U", c)])
                    Uc = U[:, c, :]
                    P.op("pool", lambda e, Uc=Uc: e.tensor_tensor(out=T1[:, 1:528], in0=Uc[:, 1:528], in1=Uc[:, 0:527], op=ALU.add), r=[("U", c)], w=["T1"])
                    lo, hi = slice(0, 64), slice(64, 128)
                    if c == 0:
                        P.op("pool", lambda e: e.tensor_tensor(out=T2[hi, 3:528], in0=T1[hi, 3:528], in1=T1[hi, 1:526], op=ALU.add), r=["T1"], w=["T2"])
                    else:
                        P.op("pool", lambda e: e.tensor_tensor(out=T2[:, 3:528], in0=T1[:, 3:528], in1=T1[:, 1:526], op=ALU.add), r=["T1"], w=["T2"])
                        P.op("pool", lambda e: e.tensor_tensor(out=T1[:, 7:528], in0=T2[:, 7:528], in1=T2[:, 3:524], op=ALU.add), r=["T2"], w=["T1"])
                        P.op("pool", lambda e: e.tensor_tensor(out=T2[hi, 15:528], in0=T1[hi, 15:528], in1=T1[hi, 7:520], op=ALU.add), r=["T1"], w=["T2"])
                    for rows, src, sk in ((lo, T1, "T1"), (hi, T2, "T2")):
                        P.op("dve", lambda e, rows=rows, src=src, c=c, Uc=Uc: e.scalar_tensor_tensor(
                            out=PL[rows, c, :], in0=src[rows, 16:528], scalar=CF[rows, CF_IW + c:CF_IW + c + 1], in1=Uc[rows, 16:528],
                            op0=ALU.mult, op1=ALU.subtract), r=[sk, ("U", c), "CF"], w=[("PL", c)])
                        if t == 0:
                            P.op("dve", lambda e, rows=rows, src=src, c=c: e.tensor_tensor(out=TF[rows, :], in0=src[rows, 16:32],
                                                                                          in1=CF[rows, CF_TB + c * 16:CF_TB + (c + 1) * 16], op=ALU.mult),
                                 r=[sk, "CF"], w=["TF"])
                            P.op("dve", lambda e, rows=rows, c=c, Uc=Uc: e.tensor_tensor(out=PL[rows, c, 0:16], in0=TF[rows, :], in1=Uc[rows, 16:32], op=ALU.subtract),
                                 r=["TF", ("U", c)], w=[("PL", c)])
                    P.op("pool", lambda e, Uc=Uc: e.tensor_copy(out=Uc[:, 0:16], in_=Uc[:, 512:528]), r=["T1", "T2", ("PL", c)], w=[("U", c)])
                    P.op("pe", lambda e, c=c: e.matmul(msb[:], PWB[:, c, :], PL[:, c, :], start=True, stop=True), r=["PWB", ("PL", c)], w=["MS"])
                    P.op("dve", lambda e, c=c: e.scalar_tensor_tensor(out=YT[:, 2 + c, :], in0=msb[:], scalar=SPR[:, 12 + c:13 + c], in1=GS[:, c, :],
                                                                      op0=ALU.mult, op1=ALU.mult),
                         r=["MS", "SPR", ("GS", c)], w=[("YT", 2 + c)])

                P.cut(9)
                for c in range(2):
                    i = proj2(1280 + c * 128)
                    P.op("act", lambda e, i=i: e.activation(out=DH[:], in_=PJ[i][:], func=AF.Copy), r=[("PJ", i)], w=["DH"])
                    i = proj2(1792 + c * 128)
                    Zc = Z[:, c, :]
                    P.op("dve", lambda e, i=i, Zc=Zc: e.tensor_tensor(out=Zc[:, 2:514], in0=PJ[i][:], in1=DH[:], op=ALU.mult), r=[("PJ", i), "DH"], w=[("Z", c)])
                    wc = 14 + c * 3
                    P.op("dve", lambda e, Zc=Zc, wc=wc: e.tensor_scalar(out=ACC[:], in0=Zc[:, 2:514], scalar1=SPR[:, wc + 2:wc + 3], scalar2=None, op0=ALU.mult),
                         r=[("Z", c), "SPR"], w=["ACC"])
                    P.op("dve", lambda e, Zc=Zc, wc=wc: e.scalar_tensor_tensor(out=ACC[:], in0=Zc[:, 1:513], scalar=SPR[:, wc + 1:wc + 2], in1=ACC[:], op0=ALU.mult, op1=ALU.add),
                         r=[("Z", c), "SPR", "ACC"], w=["ACC"])
                    P.op("dve", lambda e, Zc=Zc, wc=wc: e.scalar_tensor_tensor(out=ACC[:], in0=Zc[:, 0:512], scalar=SPR[:, wc:wc + 1], in1=ACC[:], op0=ALU.mult, op1=ALU.add),
                         r=[("Z", c), "SPR", "ACC"], w=["ACC"])
                    P.op("pool", lambda e, Zc=Zc: e.tensor_copy(out=Zc[:, 0:2], in_=Zc[:, 512:514]), r=[("Z", c)], w=[("Z", c)])
                    i = proj2(1536 + c * 128)
                    P.op("dve", lambda e, i=i: e.tensor_tensor(out=ACC[:], in0=PJ[i][:], in1=ACC[:], op=ALU.mult), r=[("PJ", i), "ACC"], w=["ACC"])
                    P.op("pool", lambda e, c=c: e.tensor_tensor(out=YT[:, 6 + c, :], in0=ACC[:], in1=GS[:, 4 + c, :], op=ALU.mult),
                         r=["ACC", ("GS", 4 + c)], w=[("YT", 6 + c)])

                P.cut(10)
                i = proj2(768)
                ri = qk_norm_prep(PJ[i], ("PJ", i))
                for k in range(2):
                    rows = slice(k * 64, (k + 1) * 64)
                    P.op("dve", lambda e, i=i, ri=ri, k=k, rows=rows: e.scalar_tensor_tensor(
                        out=KC[k][0:64, 128:640], in0=PJ[i][rows, :], scalar=SPR[rows, 11:12], in1=RS[ri][rows, :], op0=ALU.mult, op1=ALU.mult),
                         r=[("PJ", i), ("RS", ri), "SPR"], w=[("KC", k)])
                for c in range(2):
                    i = proj2(512 + c * 128)
                    ri = qk_norm_prep(PJ[i], ("PJ", i))
                    for j in range(2):
                        h = 2 * c + j
                        rows = slice(j * 64, (j + 1) * 64)
                        P.op("dve", lambda e, i=i, ri=ri, h=h, rows=rows: e.scalar_tensor_tensor(
                            out=QC[h][0:64, :], in0=PJ[i][rows, :], scalar=SPR[rows, 10:11], in1=RS[ri][rows, :], op0=ALU.mult, op1=ALU.mult),
                             r=[("PJ", i), ("RS", ri), "SPR"], w=[("QC", h)])
                for sub in range(4):
                    i = nxt("pj", 2)
                    for kc in range(8):
                        P.op("pe", lambda e, kc=kc, i=i, sub=sub: e.matmul(PJ[i][:, 0:128], H2[s][:, kc, sub * 128:(sub + 1) * 128], W2[:, kc, 896:1024],
                                                                           start=(kc == 0), stop=(kc == 7)),
                             r=["W2", ("H2", s)], w=[("PJ", i)], sig=(kc == 7))
                    P.op("dve", lambda e, i=i, sub=sub: e.tensor_copy(out=VC[:, 1 + sub, :, 0:64], in_=PJ[i][:, 0:128].rearrange("p (k d) -> p k d", k=2)),
                         r=[("PJ", i)], w=["VC"])
                for h in range(4):
                    k = h // 2
                    for b in range(4):
                        g = 4 * t + b
                        lo = 0 if g > 0 else 128
                        pi = (h * 4 + b) % 4
                        qs = QC[h][0:65, b * 128:(b + 1) * 128]
                        P.op("pe", lambda e, h=h, lo=lo: e.matmul(SW[:, lo:256], ident, CB[:, CB_CMS + h * 256 + lo:CB_CMS + (h + 1) * 256], start=True, stop=False),
                             r=["CB"], w=["SW"], sig=False)
                        if g > 0:
                            P.op("pe", lambda e, k=k, b=b, qs=qs: e.matmul(SW[:, 0:128], KC[k][0:65, b * 128:(b + 1) * 128], qs, start=False, stop=False),
                                 r=[("KC", k), ("KCaug", k), ("QC", h), ("QCaug", h)], w=["SW"], sig=False)
                        P.op("pe", lambda e, k=k, b=b, qs=qs: e.matmul(SW[:, 128:256], KC[k][0:65, 128 + b * 128:128 + (b + 1) * 128], qs, start=False, stop=True),
                             r=[("KC", k), ("KCaug", k), ("QC", h), ("QCaug", h)], w=["SW"])
                        P.op("act", lambda e, pi=pi, lo=lo, h=h: e.activation(out=PS[pi][:, lo:256], in_=SW[:, lo:256], func=AF.Exp,
                                                                              bias=CF[:, CF_SB + h:CF_SB + h + 1], scale=0.125),
                             r=["SW", "CF"], w=[("PS", pi)])
                        oc = OC[:, b * 128:(b + 1) * 128]
                        if g > 0:
                            P.op("pe", lambda e, oc=oc, b=b, k=k, pi=pi: e.matmul(oc, VC[:, b, k, :], PS[pi][:, 0:128], start=True, stop=False),
                                 r=["VC", "VCones", ("PS", pi)], w=["OC"], sig=False)
                        P.op("pe", lambda e, oc=oc, b=b, k=k, pi=pi, g=g: e.matmul(oc, VC[:, 1 + b, k, :], PS[pi][:, 128:256], start=(g == 0), stop=True),
                             r=["VC", "VCones", ("PS", pi)], w=["OC"])
                    hr = slice((h % 2) * 64, (h % 2) * 64 + 64)
                    ci = nxt("rc", 2)
                    P.op("dve", lambda e, ci=ci, h=h, hr=hr: e.tensor_scalar(out=RC[ci][hr, :], in0=OC[64:128, :], scalar1=ESK[64:128, h:h + 1], scalar2=None, op0=ALU.add),
                         r=["OC", "ESK"], w=[("RC", ci)])
                    P.op("dve", lambda e, ci=ci, hr=hr: e.reciprocal(out=RC[ci][hr, :], in_=RC[ci][hr, :]), r=[("RC", ci)], w=[("RC", ci)])
                    P.op("dve", lambda e, ci=ci, hr=hr, h=h: e.tensor_tensor(out=RC[ci][hr, :], in0=RC[ci][hr, :], in1=GS[hr, 2 + h // 2, :], op=ALU.mult),
                         r=[("RC", ci), ("GS", 2 + h // 2)], w=[("RC", ci)])
                    P.op("dve", lambda e, ci=ci, hr=hr, h=h: e.tensor_tensor(out=YT[hr, 4 + h // 2, :], in0=OC[0:64, :], in1=RC[ci][hr, :], op=ALU.mult),
                         r=["OC", ("RC", ci)], w=[("YT", 4 + h // 2)])
                for k in range(2):
                    P.op("pool", lambda e, k=k: e.tensor_copy(out=KC[k][0:64, 0:128], in_=KC[k][0:64, 512:640]), r=[("KC", k)], w=[("KC", k)])
                P.op("pool", lambda e: e.tensor_copy(out=VC[:, 0, :, 0:64], in_=VC[:, 4, :, 0:64]), r=["VC"], w=["VC"])

                if t == 0 and l == 0:
                    dump("H20", H2[s][:, 0, :], [("H2", s)])
                    for c in range(8):
                        dump(f"YT{c}", YT[:, c, :], [("YT", c)])
                P.cut(11)
                for sub in range(4):
                    for hf in range(2):
                        oi = (sub * 2 + hf) % 2
                        for kc in range(8):
                            P.op("pe", lambda e, kc=kc, oi=oi, sub=sub, hf=hf: e.matmul(PO[oi][:], YT[:, kc, sub * 128:(sub + 1) * 128], WO[:, kc, hf * 512:(hf + 1) * 512],
                                                                                       start=(kc == 0), stop=(kc == 7)),
                                 r=["WO"] + [("YT", q) for q in range(8)], w=[("PO", oi)], sig=(kc == 7))
                        P.op("dve", lambda e, oi=oi, sub=sub, hf=hf: e.tensor_tensor(out=X2[s][:, sub, hf * 512:(hf + 1) * 512], in0=PO[oi][:],
                                                                                    in1=X2[s][:, sub, hf * 512:(hf + 1) * 512], op=ALU.add),
                             r=[("PO", oi), ("X2", s)], w=[("X2", s)])
                P.dma(lambda e, t=t, s=s: e.dma_start(out=xdst[t * TT:(t + 1) * TT, :].rearrange("(s p) d -> p s d", p=128), in_=X2[s][:]),
                      r=[("X2", s)], w=["xdst"], key=f"x2{s}")
            P.barrier()

        if DBG:
            P.stopped = False
            P.op("pool", lambda e: e.memset(DBGT[:, dbg_off[0]:12288], 0.0), w=["DBG"]) if dbg_off[0] < 12288 else None
            P.dma(lambda e: e.dma_start(out=dbg_d[:, :], in_=DBGT[:]), r=["DBG"], w=["dbg_d"], key="dbg")
            P.barrier()
        block = es.enter_context(nc.Block())
        P.emit(block)
    return nc


_CACHE = {}


def _host_layout(inputs, NL):
    f = lambda a: np.ascontiguousarray(np.asarray(a, dtype=np.float32))
    spr = np.zeros((NL, 128, NSPR), np.float32)
    pw = np.zeros((NL, 128, 2, 128), np.float32)
    for l in range(NL):
        spr[l, :, 0:8] = f(inputs["norm_g"])[l].reshape(8, 128).T
        spr[l, :, 8] = np.tile(f(inputs["a_q_norm"])[l], 2)
        spr[l, :, 9] = np.tile(f(inputs["a_k_norm"])[l], 2)
        spr[l, :, 10] = np.tile(f(inputs["c_q_norm"])[l], 2)
        spr[l, :, 11] = np.tile(f(inputs["c_k_norm"])[l], 2)
        spr[l, :, 12:14] = f(inputs["pool_scale"])[l].reshape(2, 128).T
        cw = f(inputs["conv_w"])[l]
        for c in range(2):
            for j in range(3):
                spr[l, :, 14 + c * 3 + j] = cw[j, c * 128:(c + 1) * 128]
        spr[l, :, 20:24] = f(inputs["c_sinks"])[l][None, :]
        pwl = f(inputs["pool_w"])[l]
        for c in range(2):
            for half in range(2):
                pw[l, half * 64:(half + 1) * 64, c, half * 64:(half + 1) * 64] = pwl[2 * c + half]
    return spr, pw


def run(inputs, S, NL, n_cores):
    key = (S, NL)
    if key not in _CACHE:
        _CACHE[key] = (build_program(S, NL), make_consts(S))
    nc, consts = _CACHE[key]
    spr, pw = _host_layout(inputs, NL)
    x = np.asarray(inputs["x"], dtype=np.float32)
    w_in = np.ascontiguousarray(np.asarray(inputs["w_in"], dtype=np.float32))
    w_out = np.ascontiguousarray(np.asarray(inputs["w_out"], dtype=np.float32))
    in_maps = []
    for c in range(n_cores):
        m = {"x": np.ascontiguousarray(x[c]), "w_in": w_in, "w_out": w_out, "spr": spr, "pw": pw}
        m.update(consts)
        in_maps.append(m)
    res = run_bass_kernel_spmd(nc, in_maps, core_ids=list(range(n_cores)))
    if "dbg" in res.results[0]:
        run.dbg = np.asarray(res.results[0]["dbg"]).astype(np.float32)
    return np.stack([np.asarray(r["out"], dtype=np.float32) for r in res.results], axis=0)


def kernel(x, norm_g, w_in, w_out, a_q_norm, a_k_norm, pool_w, pool_scale, c_q_norm, c_k_norm, c_sinks, conv_w):
    inputs = dict(x=x, norm_g=norm_g, w_in=w_in, w_out=w_out, a_q_norm=a_q_norm, a_k_norm=a_k_norm, pool_w=pool_w,
                  pool_scale=pool_scale, c_q_norm=c_q_norm, c_k_norm=c_k_norm, c_sinks=c_sinks, conv_w=conv_w)
    x = np.asarray(x)
    return run(inputs, x.shape[1], np.asarray(w_in).shape[0], x.shape[0])
```

```python
import contextlib
import types
import numpy as np
import ml_dtypes
import concourse.bass as bass
import concourse.mybir as mybir
from concourse.bass_utils import run_bass_kernel_spmd

F32 = mybir.dt.float32
BF16 = mybir.dt.bfloat16
AF = mybir.ActivationFunctionType
ALU = mybir.AluOpType

D = 1024
NIN = 3328
TT = 512
BIG = 30000.0
EPS = 1e-6
NSPR = 24
ENGS = ("pe", "act", "dve", "pool", "sp")
BLK = {"pe": "tensor", "act": "scalar", "dve": "vector", "pool": "gpsimd", "sp": "sync"}

CB_ID, CB_OB, CB_CM, CB_CMS, CB_N = 0, 128, 256, 256 + 2048, 256 + 2048 + 1024
CF_AB, CF_SB, CF_IW, CF_TB, CF_N = 0, 256, 260, 262, 262 + 32


def slopes_all():
    return np.exp2(-(8.0 / 8) * np.arange(1, 9, dtype=np.float32)).astype(np.float32)


def make_consts(S):
    sl = slopes_all()
    sl_c, sl_a = sl[:4], sl[4:]
    cb = np.zeros((128, CB_N), np.float32)
    cb[:, CB_ID:CB_ID + 128] = np.eye(128)
    cb[0:64, CB_OB:CB_OB + 64] = 1.0
    cb[64:128, CB_OB + 64:CB_OB + 128] = 1.0
    kl = np.arange(128)[:, None]
    ql = np.arange(512)[None, :]
    for kt in range(4):
        m = ((ql // 256) == (kt // 2)) & (ql < 128 * kt + kl)
        cb[:, CB_CM + kt * 512:CB_CM + (kt + 1) * 512] = np.where(m, -BIG, 0.0)
    q1 = np.arange(128)[None, :]
    for h in range(4):
        prev = np.where(kl > q1, -8.0 * sl_c[h] * 128.0, -BIG)
        own = np.where(kl <= q1, 0.0, -BIG)
        cb[:, CB_CMS + h * 256:CB_CMS + h * 256 + 128] = prev
        cb[:, CB_CMS + h * 256 + 128:CB_CMS + (h + 1) * 256] = own
    cf = np.zeros((128, CF_N), np.float32)
    for h in range(4):
        for i in range(64):
            d = 3 - i
            cf[:, CF_AB + h * 64 + i] = sl_a[h] * (np.arange(128) - 511 + 128 * d)
        cf[:, CF_SB + h] = sl_c[h] * np.arange(128)
    wins = (2, 4, 8, 16)
    for c in range(2):
        for half in range(2):
            w = wins[2 * c + half]
            rows = slice(half * 64, (half + 1) * 64)
            cf[rows, CF_IW + c] = 1.0 / w
            cf[rows, CF_TB + c * 16:CF_TB + (c + 1) * 16] = 1.0 / np.minimum(np.arange(16) + 1, w)
    khot = np.zeros((32, S), np.float32)
    for n in range(S // 256):
        khot[n, n * 256:(n + 1) * 256] = 1.0
    cq = np.zeros((4, 512), np.float32)
    for h in range(4):
        cq[h] = -8.0 * sl_c[h] * (np.arange(512) % 128)
    bf = ml_dtypes.bfloat16
    return {"cb": cb.astype(bf), "cf": cf, "khot": khot.astype(bf), "cqrow": cq.astype(bf)}


def _freeze(fn):
    if fn is None or fn.__closure__ is None:
        return fn
    cells = []
    for c in fn.__closure__:
        try:
            cells.append(types.CellType(c.cell_contents))
        except ValueError:
            cells.append(c)
    g = types.FunctionType(fn.__code__, fn.__globals__, fn.__name__, fn.__defaults__, tuple(cells))
    g.__kwdefaults__ = fn.__kwdefaults__
    return g


class Prog:
    def __init__(self, nc, es):
        self.nc = nc
        self.es = es
        self.ops = {e: [] for e in ENGS}
        self.cnt = {e: 0 for e in ENGS}
        self.semh = {}
        self.dcnt = {}
        self.lastw = {}
        self.readers = {}
        self.waited = {e: {} for e in ENGS}
        for e in ENGS:
            self.semh["E:" + e] = es.enter_context(nc.semaphore("sem_" + e))

    def _collect(self, eng, r, w):
        need = {}

        def add(ev):
            semid, val, src = ev
            if src == eng and eng == "pe":
                return
            if self.waited[eng].get(semid, 0) >= val:
                return
            if need.get(semid, 0) < val:
                need[semid] = val

        for k in r:
            if k in self.lastw:
                add(self.lastw[k])
        for k in w:
            if k in self.lastw:
                add(self.lastw[k])
            for ev in self.readers.get(k, {}).values():
                add(ev)
        for semid, val in need.items():
            self.waited[eng][semid] = val
        return list(need.items())

    def _commit(self, ev, r, w):
        for k in r:
            d = self.readers.setdefault(k, {})
            if ev[0] not in d or d[ev[0]][1] < ev[1]:
                d[ev[0]] = ev
        for k in w:
            self.lastw[k] = ev
            self.readers[k] = {}

    stopped = False

    def cut(self, k):
        import os
        if int(os.environ.get("STAGE", "99")) == k and not self.stopped:
            self.barrier()
            self.stopped = True

    def op(self, eng, fn, r=(), w=(), sig=True):
        if self.stopped:
            return
        fn = _freeze(fn)
        waits = self._collect(eng, r, w)
        semid = "E:" + eng
        if sig:
            self.cnt[eng] += 1
            ev = (semid, self.cnt[eng], eng)
            inc = (semid, 1)
        else:
            ev = (semid, self.cnt[eng] + 1, eng)
            inc = None
        self.ops[eng].append((waits, fn, inc))
        self._commit(ev, r, w)

    def dma(self, fn, r=(), w=(), key=None, q="sp"):
        if self.stopped:
            return
        fn = _freeze(fn)
        waits = self._collect(q, r, w)
        semid = "D:" + key
        if semid not in self.semh:
            self.semh[semid] = self.es.enter_context(self.nc.semaphore("dsem_" + key))
            self.dcnt[semid] = 0
        self.dcnt[semid] += 16
        ev = (semid, self.dcnt[semid], "dma")
        self.ops[q].append((waits, fn, (semid, 16)))
        self._commit(ev, r, w)

    def dma_group(self, key, items, q="sp"):
        if self.stopped:
            return
        semid = "D:" + key
        if semid not in self.semh:
            self.semh[semid] = self.es.enter_context(self.nc.semaphore("dsem_" + key))
            self.dcnt[semid] = 0
        total = self.dcnt[semid] + 16 * len(items)
        ev = (semid, total, "dma")
        for fn, r, w in items:
            waits = self._collect(q, r, w)
            self.ops[q].append((waits, _freeze(fn), (semid, 16)))
        for fn, r, w in items:
            self._commit(ev, r, w)
        self.dcnt[semid] = total

    def barrier(self):
        if self.stopped:
            return
        for e in ENGS:
            waits = []
            for o in ENGS:
                if o == e or self.cnt[o] == 0:
                    continue
                sid = "E:" + o
                if self.waited[e].get(sid, 0) < self.cnt[o]:
                    waits.append((sid, self.cnt[o]))
                    self.waited[e][sid] = self.cnt[o]
            for sid, c in self.dcnt.items():
                if self.waited[e].get(sid, 0) < c:
                    waits.append((sid, c))
                    self.waited[e][sid] = c
            if waits:
                self.ops[e].append((waits, None, None))
        self.lastw = {}
        self.readers = {}

    def emit(self, block):
        for eng in ENGS:
            def body(e, eng=eng):
                for waits, fn, inc in self.ops[eng]:
                    for semid, val in waits:
                        e.wait_ge(self.semh[semid], val)
                    if fn is None:
                        continue
                    ins = fn(e)
                    if inc is not None:
                        ins.then_inc(self.semh[inc[0]], inc[1])
            getattr(block, BLK[eng])(body)


class Arena:
    def __init__(self, nc, base, top):
        self.nc, self.base, self.top, self.cur, self.n = nc, base, top, base, 0

    def alloc(self, name, shape, dtype):
        nbytes = int(np.prod(shape[1:])) * (2 if dtype == BF16 else 4)
        nbytes = (nbytes + 63) // 64 * 64
        off = self.cur
        assert off + nbytes <= self.top, f"SBUF overflow at {name}: {off + nbytes} > {self.top}"
        self.cur += nbytes
        self.n += 1
        return self.nc.alloc_sbuf_tensor_at(f"{name}_{self.n}", list(shape), dtype, offset=off)


def build_program(S, NL):
    NT = S // TT
    NKT = S // 128
    nc = bass.Bass("TRN2", target_bir_lowering=False)
    dt = nc.dram_tensor
    x_in = dt("x", [S, D], F32, kind="ExternalInput").ap()
    w_in = dt("w_in", [NL, D, NIN], F32, kind="ExternalInput").ap()
    w_out = dt("w_out", [NL, D, D], F32, kind="ExternalInput").ap()
    spr_d = dt("spr", [NL, 128, NSPR], F32, kind="ExternalInput").ap()
    pw_d = dt("pw", [NL, 128, 2, 128], F32, kind="ExternalInput").ap()
    cb_d = dt("cb", [128, CB_N], BF16, kind="ExternalInput").ap()
    cf_d = dt("cf", [128, CF_N], F32, kind="ExternalInput").ap()
    khot_d = dt("khot", [32, S], BF16, kind="ExternalInput").ap()
    cq_d = dt("cqrow", [4, 512], BF16, kind="ExternalInput").ap()
    out_d = dt("out", [S, D], F32, kind="ExternalOutput").ap()
    hT_d = dt("hT_scr", [D, S], BF16, kind="Internal").ap()
    ya_d = dt("ya_scr", [256, S], BF16, kind="Internal").ap()
    x1_d = dt("x1_scr", [S, D], F32, kind="Internal").ap() if NL > 1 else None

    with contextlib.ExitStack() as es:
        P = Prog(nc, es)
        a_base = (int(nc.sbuf_base) + 63) // 64 * 64
        a_size = int(nc.sbuf_top) - a_base - 2048
        es.enter_context(nc.sbuf_tensor("arena_slab", [128, a_size], mybir.dt.uint8))
        A = Arena(nc, a_base, a_base + a_size)
        pb = [es.enter_context(nc.psum_tensor(f"pb{i}", [128, 512], F32)) for i in range(6)]
        tpb = es.enter_context(nc.psum_tensor("tpb", [128, 512], F32))
        msb = es.enter_context(nc.psum_tensor("msb", [128, 512], F32))
        CB = A.alloc("CB", [128, CB_N], BF16)
        CF = A.alloc("CF", [128, CF_N], F32)
        SPR = A.alloc("SPR", [128, NSPR], F32)
        ESK = A.alloc("ESK", [128, 4], F32)
        SQ = [A.alloc(f"SQ{i}", [128, 512], BF16) for i in range(2)]
        RS = [A.alloc(f"RS{i}", [128, 512], F32) for i in range(2)]
        RC = [A.alloc(f"RC{i}", [128, 512], F32) for i in range(2)]
        ident = CB[:, CB_ID:CB_ID + 128]
        onesblk = CB[:, CB_OB:CB_OB + 128]
        import os
        DBG = os.environ.get("DBG") == "1"
        dbg_off = [0]
        dbg_names = []
        if DBG:
            dbg_d = dt("dbg", [128, 12288], BF16, kind="ExternalOutput").ap()
            DBGT = A.alloc("DBGT", [128, 12288], BF16)

        def dump(name, ap, r, npart=128):
            if not DBG or P.stopped:
                return
            n = ap.shape[-1] if len(ap.shape) == 2 else int(np.prod(ap.shape[1:]))
            o = dbg_off[0]
            dbg_names.append((name, o, n, npart))
            dbg_off[0] += n
            assert dbg_off[0] <= 12288
            P.op("dve", lambda e: e.tensor_copy(out=DBGT[0:npart, o:o + n], in_=ap), r=r, w=["DBG"])
        build_program.dbg_names = dbg_names
        mark = A.cur

        first_items = [(lambda e: e.dma_start(out=CB[:], in_=cb_d[:, :]), [], ["CB"]),
                       (lambda e: e.dma_start(out=CF[:], in_=cf_d[:, :]), [], ["CF"])]

        rr = {"sq": 0, "pj": 0, "rc": 0}

        def nxt(name, n):
            rr[name] = (rr[name] + 1) % n
            return rr[name]

        def qk_norm_prep(pj, pjk):
            i = nxt("sq", 2)
            P.op("act", lambda e: e.activation(out=SQ[i][:], in_=pj[:], func=AF.Square), r=[pjk], w=[("SQ", i)])
            P.op("pe", lambda e: e.matmul(msb[:], onesblk, SQ[i][:], start=True, stop=True),
                 r=[("SQ", i), "CB"], w=["MS"])
            P.op("act", lambda e: e.activation(out=RS[i][:], in_=msb[:], func=AF.Ln, bias=EPS, scale=1.0 / 64),
                 r=["MS"], w=[("RS", i)])
            P.op("act", lambda e: e.activation(out=RS[i][:], in_=RS[i][:], func=AF.Exp, scale=-0.5),
                 r=[("RS", i)], w=[("RS", i)])
            return i

        def qk_norm_prep_gen(pj, pjk):
            i = nxt("sq", 2)
            P.op("act", lambda e: e.activation(out=SQ[i][:], in_=pj[:], func=AF.Square), r=[pjk], w=[("SQ", i)])
            yield
            P.op("pe", lambda e: e.matmul(msb[:], onesblk, SQ[i][:], start=True, stop=True),
                 r=[("SQ", i), "CB"], w=["MS"])
            yield
            P.op("act", lambda e: e.activation(out=RS[i][:], in_=msb[:], func=AF.Ln, bias=EPS, scale=1.0 / 64),
                 r=["MS"], w=[("RS", i)])
            P.op("act", lambda e: e.activation(out=RS[i][:], in_=RS[i][:], func=AF.Exp, scale=-0.5),
                 r=[("RS", i)], w=[("RS", i)])
            yield
            return i

        for l in range(NL):
            xsrc = x_in if l == 0 else x1_d
            xdst = out_d if l == NL - 1 else x1_d
            items = first_items if l == 0 else []
            items.append((lambda e, l=l: e.dma_start(out=SPR[:], in_=spr_d[l, :, :]), [], ["SPR"]))

            A.cur = mark
            WA = A.alloc("WA", [128, 8, 1024], BF16)
            KA = [A.alloc(f"KA{h}", [96, S], BF16) for h in range(4)]
            VA = A.alloc("VA", [128, NKT, 384], BF16)
            KM = A.alloc("KM", [128, 2, 64], F32)
            wbase = A.cur
            XT = A.alloc("XT", [128, 4, 1024], F32)
            XS = A.alloc("XS", [128, 4, 1024], BF16)
            WSTG = [nc.alloc_sbuf_tensor_at(f"WSTGa{l}_{i}", [128, 2304], F32, offset=wbase + i * 9216) for i in range(2)]
            HT = [A.alloc(f"HT{i}", [128, 8, 512], BF16) for i in range(2)]
            QF = A.alloc("QF", [128, 2, 512], F32)
            QA = [[A.alloc(f"QA{h}_{s}", [96, 512], BF16) for s in range(1)] * 2 for h in range(4)]
            GA = [A.alloc("GA", [128, 2, 512], BF16)] * 2
            PT = [A.alloc(f"PT{i}", [128, 512], BF16) for i in range(4)]
            YA = [A.alloc("YA", [128, 2, 512], BF16)] * 2
            SSX = A.alloc("SSX", [128, 4], F32)
            RSX = A.alloc("RSX", [128, 4], F32)
            BSM = A.alloc("BSM", [128, 4, 32], F32)
            M8 = A.alloc("M8", [128, 4, 8], F32)
            THR = A.alloc("THR", [128, 4], F32)
            MB = A.alloc("MB", [128, 4, 32], BF16)
            PJ = pb[0:1]
            ST = pb[1:4]
            OT = pb[4:6]

            for h in range(4):
                items.append((lambda e, h=h: e.dma_start(out=KA[h][64:96, :], in_=khot_d[:, :]), [], [("KAaug", h)]))
            P.dma_group("const", items)
            P.op("act", lambda e: e.activation(out=ESK[:], in_=SPR[:, 20:24], func=AF.Exp), r=["SPR"], w=["ESK"])
            P.op("pool", lambda e: e.memset(VA[:, :, 64:128], 1.0), w=["VAones"])
            P.op("pool", lambda e: e.memset(VA[:, :, 256:320], 1.0), w=["VAones"])
            P.op("pool", lambda e: e.memset(KM[:], 0.0), w=["KM"])
            for kc in range(8):
                i = kc % 2
                P.dma(lambda e, kc=kc, i=i, l=l: e.dma_start(out=WSTG[i][:, 0:1024], in_=w_in[l, kc * 128:(kc + 1) * 128, 0:1024]),
                      w=[("WSTG", i)], key=f"w{i}")
                P.op("dve", lambda e, kc=kc, i=i: e.tensor_scalar(out=WA[:, kc, :], in0=WSTG[i][:, 0:1024], scalar1=SPR[:, kc:kc + 1],
                                                                  scalar2=None, op0=ALU.mult),
                     r=[("WSTG", i), "SPR"], w=["WA", "XTa", "XS"])

            def load_x(t):
                P.dma(lambda e, t=t: e.dma_start(out=XT[:], in_=xsrc[t * TT:(t + 1) * TT, :].rearrange("(s p) d -> p s d", p=128)),
                      r=["XTa"], w=["XT"], key="xt")

            P.cut(1)
            load_x(0)
            def proj(c0, s):
                i = nxt("pj", len(PJ))
                for kc in range(8):
                    P.op("pe", lambda e, kc=kc, i=i: e.matmul(PJ[i][:], WA[:, kc, c0:c0 + 128], HT[s][:, kc, :], start=(kc == 0), stop=(kc == 7)),
                         r=["WA", ("HT", s)], w=[("PJ", i)], sig=(kc == 7))
                return i

            def stage1(t):
                s = t % 2
                for sub in range(4):
                    P.op("act", lambda e, sub=sub: e.activation(out=XS[:, sub, :], in_=XT[:, sub, :], func=AF.Square,
                                                                accum_out=SSX[:, sub:sub + 1]),
                         r=["XT"], w=["XS", "SSX"])
                    yield
                P.op("act", lambda e: e.activation(out=RSX[:], in_=SSX[:], func=AF.Ln, bias=EPS, scale=1.0 / D), r=["SSX"], w=["RSX"])
                P.op("act", lambda e: e.activation(out=RSX[:], in_=RSX[:], func=AF.Exp, scale=-0.5), r=["RSX"], w=["RSX"])
                yield
                for sub in range(4):
                    P.op("dve", lambda e, sub=sub: e.tensor_scalar(out=XS[:, sub, :], in0=XT[:, sub, :], scalar1=RSX[:, sub:sub + 1],
                                                                   scalar2=None, op0=ALU.mult),
                         r=["XT", "RSX"], w=["XS"])
                    yield
                if t + 1 < NT:
                    load_x(t + 1)
                for kc in range(8):
                    hf = kc % 2
                    bank, bkey = (tpb, ("TP", 0)) if hf == 0 else (msb, "MS")
                    for sub in range(4):
                        P.op("pe", lambda e, kc=kc, sub=sub, bank=bank: e.matmul(bank[:, sub * 128:(sub + 1) * 128], XS[:, sub, kc * 128:(kc + 1) * 128], ident,
                                                                                 start=True, stop=True),
                             r=["XS", "CB"], w=[bkey], sig=(sub == 3))
                    P.op("dve", lambda e, kc=kc, bank=bank: e.tensor_copy(out=HT[s][:, kc, :], in_=bank[:, 0:512]),
                         r=[bkey], w=[("HT", s)])
                    yield
                P.dma(lambda e, t=t, s=s: e.dma_start(out=hT_d[:, t * TT:(t + 1) * TT].rearrange("(kc p) n -> p kc n", p=128), in_=HT[s][:]),
                      r=[("HT", s)], w=["hT_d"], key=f"hts{s}")


                for c in range(2):
                    i = proj(256 + c * 128, s)
                    yield
                    ri = yield from qk_norm_prep_gen(PJ[i], ("PJ", i))
                    for j in range(2):
                        h = 2 * c + j
                        rows = slice(j * 64, (j + 1) * 64)
                        for b in range(2):
                            n = 2 * t + b
                            P.op("dve", lambda e, i=i, ri=ri, h=h, rows=rows, b=b, n=n: e.scalar_tensor_tensor(
                                out=KA[h][0:64, t * TT + b * 256:t * TT + (b + 1) * 256], in0=PJ[i][rows, b * 256:(b + 1) * 256],
                                scalar=SPR[rows, 9:10], in1=RS[ri][rows, b * 256:(b + 1) * 256], op0=ALU.mult, op1=ALU.mult,
                                accum_out=KM[rows, c, j * 32 + n:j * 32 + n + 1]),
                                 r=[("PJ", i), ("RS", ri), "SPR"], w=[("KA", h, t), "KM"])
                        yield
                for sub in range(4):
                    i = nxt("pj", len(PJ))
                    for kc in range(8):
                        P.op("pe", lambda e, kc=kc, i=i, sub=sub: e.matmul(PJ[i][:, 0:256], HT[s][:, kc, sub * 128:(sub + 1) * 128], WA[:, kc, 512:768],
                                                                           start=(kc == 0), stop=(kc == 7)),
                             r=["WA", ("HT", s)], w=[("PJ", i)], sig=(kc == 7))
                    yield
                    kt = 4 * t + sub
                    for (d0, s0, wd) in ((0, 0, 64), (128, 64, 128), (320, 192, 64)):
                        P.op("dve", lambda e, i=i, kt=kt, d0=d0, s0=s0, wd=wd: e.tensor_copy(out=VA[:, kt, d0:d0 + wd], in_=PJ[i][:, s0:s0 + wd]),
                             r=[("PJ", i)], w=[("VA", t)])
                    yield

            def stage2(t):
                s = t % 2
                for c in range(2):
                    i = proj(c * 128, s)
                    ri = qk_norm_prep(PJ[i], ("PJ", i))
                    P.op("dve", lambda e, i=i, ri=ri, c=c: e.scalar_tensor_tensor(
                        out=QF[:, c, :], in0=PJ[i][:], scalar=SPR[:, 8:9], in1=RS[ri][:], op0=ALU.mult, op1=ALU.mult),
                         r=[("PJ", i), ("RS", ri), "SPR"], w=[("QF", c)])
                    P.op("pool", lambda e, c=c: e.tensor_copy(out=QA[2 * c][0][0:64, :], in_=QF[0:64, c, :]),
                         r=[("QF", c)], w=[("QA", 2 * c, 0)])
                    P.op("dve", lambda e, c=c: e.tensor_copy(out=QA[2 * c + 1][0][0:64, :], in_=QF[64:128, c, :]),
                         r=[("QF", c)], w=[("QA", 2 * c + 1, 0)])
                for c in range(2):
                    i = proj(768 + c * 128, s)
                    P.op("act", lambda e, i=i, c=c: e.activation(out=GA[0][:, c, :], in_=PJ[i][:], func=AF.Silu),
                         r=[("PJ", i)], w=[("GA", 0, c)])
                for sub in range(4):
                    own = 2 * t + sub // 2
                    for c in range(2):
                        P.op("pe", lambda e, c=c, sub=sub: e.matmul(msb[:, c * 64:(c + 1) * 64], QF[:, c, sub * 128:(sub + 1) * 128], KM[:, c, :],
                                                                    start=True, stop=True),
                             r=[("QF", c), "KM"], w=["MS"], sig=(c == 1))
                    P.op("pool", lambda e: e.memset(BSM[:], -1e30), w=["BSM"])
                    if own > 0:
                        P.op("dve", lambda e, own=own: e.tensor_copy(out=BSM[:, :, 0:own],
                                                                     in_=msb[:, 0:128].rearrange("p (h n) -> p h n", h=4)[:, :, 0:own]),
                             r=["MS"], w=["BSM"])
                    for h in range(4):
                        P.op("dve", lambda e, h=h: e.max(out=M8[:, h, :], in_=BSM[:, h, :]), r=["BSM"], w=["M8"])
                    P.op("dve", lambda e: e.tensor_scalar(out=THR[:], in0=M8[:, :, 2], scalar1=-1e29, scalar2=None, op0=ALU.max),
                         r=["M8"], w=["THR"])
                    for h in range(4):
                        P.op("dve", lambda e, h=h: e.tensor_scalar(out=MB[:, h, :], in0=BSM[:, h, :], scalar1=THR[:, h:h + 1], scalar2=-BIG,
                                                                   op0=ALU.is_lt, op1=ALU.mult),
                             r=["BSM", "THR"], w=["MB"])
                    P.op("dve", lambda e, own=own: e.memset(MB[:, :, own:own + 1], 0.0), w=["MB"])
                    P.op("pe", lambda e: e.matmul(tpb[:, 0:128], MB[:].rearrange("p h n -> p (h n)"), ident, start=True, stop=True), r=["MB", "CB"], w=[("TP", 0)])
                    for h in range(4):
                        P.op("dve", lambda e, h=h, sub=sub: e.tensor_copy(out=QA[h][0][64:96, sub * 128:(sub + 1) * 128], in_=tpb[h * 32:(h + 1) * 32, 0:128]),
                             r=[("TP", 0)], w=[("QAm", h, 0)])


            def moba(t, hook):
                s = t % 2
                for h in range(4):
                    oslot = h % 2
                    nkt = 4 * t + 4
                    vc0 = (0, 64, 192, 256)[h]
                    pend = []
                    for kt in range(nkt):
                        si = kt % 3
                        diag = kt >= 4 * t
                        P.op("pe", lambda e, h=h, kt=kt, si=si, diag=diag: e.matmul(ST[si][:], KA[h][0:96, kt * 128:(kt + 1) * 128], QA[h][0][0:96, :],
                                                                                    start=True, stop=not diag),
                             r=[("KA", h, kt // 4), ("KAaug", h), ("QA", h, 0), ("QAm", h, 0)], w=[("ST", si)], sig=not diag)
                        if diag:
                            dk = kt - 4 * t
                            P.op("pe", lambda e, si=si, dk=dk: e.matmul(ST[si][:], ident, CB[:, CB_CM + dk * 512:CB_CM + (dk + 1) * 512], start=False, stop=True),
                                 r=["CB"], w=[("ST", si)])
                        pi = kt % 4
                        col = CF_AB + h * 64 + (3 - (kt - 4 * t))
                        P.op("act", lambda e, si=si, pi=pi, col=col: e.activation(out=PT[pi][:], in_=ST[si][:], func=AF.Exp, bias=CF[:, col:col + 1], scale=0.125),
                             r=[("ST", si), "CF"], w=[("PT", pi)])
                        hook()

                        def pv(h=h, kt=kt, pi=pi, oslot=oslot, vc0=vc0, nkt=nkt):
                            P.op("pe", lambda e: e.matmul(OT[oslot][:], VA[:, kt, vc0:vc0 + 128], PT[pi][:], start=(kt == 0), stop=(kt == nkt - 1)),
                                 r=[("VA", kt // 4), "VAones", ("PT", pi)], w=[("OT", oslot)], sig=True)
                        pend.append(pv)
                        if len(pend) > 2:
                            pend.pop(0)()
                    for f_ in pend:
                        f_()
                    nr = slice(0, 64) if h % 2 == 0 else slice(64, 128)
                    dr = slice(64, 128) if h % 2 == 0 else slice(0, 64)
                    hr = slice((h % 2) * 64, (h % 2) * 64 + 64)
                    ci = nxt("rc", 2)
                    P.op("dve", lambda e, oslot=oslot, dr=dr, ci=ci, hr=hr: e.reciprocal(out=RC[ci][hr, :], in_=OT[oslot][dr, :]), r=[("OT", oslot)], w=[("RC", ci)])
                    P.op("dve", lambda e, ci=ci, hr=hr, h=h: e.tensor_tensor(out=RC[ci][hr, :], in0=RC[ci][hr, :], in1=GA[0][hr, h // 2, :], op=ALU.mult),
                         r=[("RC", ci), ("GA", 0, h // 2)], w=[("RC", ci)])
                    P.op("dve", lambda e, oslot=oslot, nr=nr, ci=ci, hr=hr, h=h: e.tensor_tensor(out=YA[0][hr, h // 2, :], in0=OT[oslot][nr, :], in1=RC[ci][hr, :], op=ALU.mult),
                         r=[("OT", oslot), ("RC", ci)], w=[("YA", 0)])
                P.dma(lambda e, t=t, s=s: e.dma_start(out=ya_d[:, t * TT:(t + 1) * TT].rearrange("(c p) n -> p c n", p=128), in_=YA[0][:]),
                      r=[("YA", 0)], w=["ya_d"], key="yas")

            for _ in stage1(0):
                pass
            stage2(0)
            for t in range(NT):
                gen = stage1(t + 1) if t + 1 < NT else iter(())

                def hook(gen=gen):
                    next(gen, None)
                moba(t, hook)
                for _ in gen:
                    pass
                if t + 1 < NT:
                    stage2(t + 1)
            P.barrier()
            P.cut(6)

            A.cur = mark
            W2 = A.alloc("W2", [128, 8, 2304], BF16)
            WO = A.alloc("WO", [128, 8, 1024], BF16)
            PWB = A.alloc("PWB", [128, 2, 128], BF16)
            H2 = [A.alloc(f"H2{i}", [128, 8, 512], BF16) for i in range(2)]
            wbase = A.cur
            X2 = [A.alloc(f"X2{i}", [128, 4, 1024], F32) for i in range(2)]
            WSTG = [nc.alloc_sbuf_tensor_at(f"WSTGb{l}_{i}", [128, 2304], F32, offset=wbase + i * 16384) for i in range(2)]
            YT = A.alloc("YT", [128, 8, 512], BF16)
            GS = A.alloc("GS", [128, 6, 512], BF16)
            U = A.alloc("U", [128, 2, 528], F32)
            T1 = A.alloc("T1", [128, 528], F32)
            T2 = A.alloc("T2", [128, 528], F32)
            TF = A.alloc("TF", [128, 16], F32)
            PL = A.alloc("PL", [128, 2, 512], BF16)
            DH = A.alloc("DH", [128, 512], F32)
            Z = A.alloc("Z", [128, 2, 514], F32)
            ACC = A.alloc("ACC", [128, 512], F32)
            QC = [A.alloc(f"QC{h}", [65, 512], BF16) for h in range(4)]
            KC = [A.alloc(f"KC{k}", [65, 640], BF16) for k in range(2)]
            VC = A.alloc("VC", [128, 5, 2, 128], BF16)
            PS = [A.alloc(f"PS{i}", [128, 256], BF16) for i in range(4)]
            PJ = pb[0:2]
            SW = pb[2]
            OC = pb[3]
            PO = pb[4:6]

            P.op("pool", lambda e: e.memset(U[:], 0.0), w=[("U", 0), ("U", 1)])
            P.op("pool", lambda e: e.memset(Z[:], 0.0), w=[("Z", 0), ("Z", 1)])
            P.op("pool", lambda e: e.memset(VC[:, :, :, 64:128], 1.0), w=["VCones"])
            P.op("pool", lambda e: e.memset(VC[:, 0, :, 0:64], 0.0), w=["VC"])
            for k in range(2):
                P.op("pool", lambda e, k=k: e.memset(KC[k][64:65, :], 1.0), w=[("KCaug", k)])
                P.op("pool", lambda e, k=k: e.memset(KC[k][0:64, 0:128], 0.0), w=[("KC", k)])
            P.dma_group("const2", [(lambda e, h=h: e.dma_start(out=QC[h][64:65, :], in_=cq_d[h:h + 1, :]), [], [("QCaug", h)]) for h in range(4)])
            for kc in range(8):
                i = kc % 2
                P.dma(lambda e, kc=kc, i=i, l=l: e.dma_start(out=WSTG[i][:], in_=w_in[l, kc * 128:(kc + 1) * 128, 1024:3328]),
                      w=[("X2", i)], key=f"w{i}")
                P.op("dve", lambda e, kc=kc, i=i: e.tensor_scalar(out=W2[:, kc, :], in0=WSTG[i][:], scalar1=SPR[:, kc:kc + 1], scalar2=None, op0=ALU.mult),
                     r=[("X2", i), "SPR"], w=["W2"])
            for kc in range(8):
                i = kc % 2
                P.dma(lambda e, kc=kc, i=i, l=l: e.dma_start(out=WSTG[i][:, 0:1024], in_=w_out[l, kc * 128:(kc + 1) * 128, :]),
                      w=[("X2", i)], key=f"w{i}")
                P.op("pool", lambda e, kc=kc, i=i: e.tensor_copy(out=WO[:, kc, :], in_=WSTG[i][:, 0:1024]), r=[("X2", i)], w=["WO"])
            P.dma(lambda e, l=l: e.dma_start(out=WSTG[0][:, 0:256], in_=pw_d[l].rearrange("p c n -> p (c n)")), w=[("X2", 0)], key="w0")
            P.op("pool", lambda e: e.tensor_copy(out=PWB[:].rearrange("p c n -> p (c n)"), in_=WSTG[0][:, 0:256]), r=[("X2", 0)], w=["PWB"])

            def load2(t):
                s = t % 2
                P.dma(lambda e, t=t, s=s: e.dma_start(out=H2[s][:], in_=hT_d[:, t * TT:(t + 1) * TT].rearrange("(kc p) n -> p kc n", p=128)),
                      r=["hT_d"], w=[("H2", s)], key=f"h2{s}")
                P.dma(lambda e, t=t, s=s: e.dma_start(out=X2[s][:], in_=xsrc[t * TT:(t + 1) * TT, :].rearrange("(s p) d -> p s d", p=128)),
                      w=[("X2", s)], key=f"x2{s}")

            P.cut(7)
            load2(0)
            for t in range(NT):
                s = t % 2
                if t + 1 < NT:
                    load2(t + 1)
                P.dma(lambda e, t=t: e.dma_start(out=YT[:, 0:2, :], in_=ya_d[:, t * TT:(t + 1) * TT].rearrange("(c p) n -> p c n", p=128)),
                      r=["ya_d"], w=[("YT", 0), ("YT", 1)], key="yal")

                def proj2(c0):
                    i = nxt("pj", 2)
                    for kc in range(8):
                        P.op("pe", lambda e, kc=kc, i=i: e.matmul(PJ[i][:], W2[:, kc, c0:c0 + 128], H2[s][:, kc, :], start=(kc == 0), stop=(kc == 7)),
                             r=["W2", ("H2", s)], w=[("PJ", i)], sig=(kc == 7))
                    return i

                for gi, c0 in enumerate((256, 384, 1024, 1152, 2048, 2176)):
                    i = proj2(c0)
                    P.op("act", lambda e, i=i, gi=gi: e.activation(out=GS[:, gi, :], in_=PJ[i][:], func=AF.Silu), r=[("PJ", i)], w=[("GS", gi)])

                P.cut(8)
                for c in range(2):
                    i = proj2(c * 128)
                    P.op("act", lambda e, i=i, c=c: e.activation(out=U[:, c, 16:528], in_=PJ[i][:], func=AF.Copy), r=[("PJ", i)], w=[("U", c)])
                    Uc = U[:, c, :]
                    P.op("pool", lambda e, Uc=Uc: e.tensor_tensor(out=T1[:, 1:528], in0=Uc[:, 1:528], in1=Uc[:, 0:527], op=ALU.add), r=[("U", c)], w=["T1"])
                    lo, hi = slice(0, 64), slice(64, 128)
                    if c == 0:
                        P.op("pool", lambda e: e.tensor_tensor(out=T2[hi, 3:528], in0=T1[hi, 3:528], in1=T1[hi, 1:526], op=ALU.add), r=["T1"], w=["T2"])
                    else:
                        P.op("pool", lambda e: e.tensor_tensor(out=T2[:, 3:528], in0=T1[:, 3:528], in1=T1[:, 1:526], op=ALU.add), r=["T1"], w=["T2"])
                        P.op("pool", lambda e: e.tensor_tensor(out=T1[:, 7:528], in0=T2[:, 7:528], in1=T2[:, 3:524], op=ALU.add), r=["T2"], w=["T1"])
                        P.op("pool", lambda e: e.tensor_tensor(out=T2[hi, 15:528], in0=T1[hi, 15:528], in1=T1[hi, 7:520], op=ALU.add), r=["T1"], w=["T2"])
                    for rows, src, sk in ((lo, T1, "T1"), (hi, T2, "T2")):
                        P.op("dve", lambda e, rows=rows, src=src, c=c, Uc=Uc: e.scalar_tensor_tensor(
                            out=PL[rows, c, :], in0=src[rows, 16:528], scalar=CF[rows, CF_IW + c:CF_IW + c + 1], in1=Uc[rows, 16:528],
                            op0=ALU.mult, op1=ALU.subtract), r=[sk, ("U", c), "CF"], w=[("PL", c)])
                        if t == 0:
                            P.op("dve", lambda e, rows=rows, src=src, c=c: e.tensor_tensor(out=TF[rows, :], in0=src[rows, 16:32],
                                                                                          in1=CF[rows, CF_TB + c * 16:CF_TB + (c + 1) * 16], op=ALU.mult),
                                 r=[sk, "CF"], w=["TF"])
                            P.op("dve", lambda e, rows=rows, c=c, Uc=Uc: e.tensor_tensor(out=PL[rows, c, 0:16], in0=TF[rows, :], in1=Uc[rows, 16:32], op=ALU.subtract),
                                 r=["TF", ("U", c)], w=[("PL", c)])
                    P.op("pool", lambda e, Uc=Uc: e.tensor_copy(out=Uc[:, 0:16], in_=Uc[:, 512:528]), r=["T1", "T2", ("PL", c)], w=[("U", c)])
                    P.op("pe", lambda e, c=c: e.matmul(msb[:], PWB[:, c, :], PL[:, c, :], start=True, stop=True), r=["PWB", ("PL", c)], w=["MS"])
                    P.op("dve", lambda e, c=c: e.scalar_tensor_tensor(out=YT[:, 2 + c, :], in0=msb[:], scalar=SPR[:, 12 + c:13 + c], in1=GS[:, c, :],
                                                                      op0=ALU.mult, op1=ALU.mult),
                         r=["MS", "SPR", ("GS", c)], w=[("YT", 2 + c)])

                P.cut(9)
                for c in range(2):
                    i = proj2(1280 + c * 128)
                    P.op("act", lambda e, i=i: e.activation(out=DH[:], in_=PJ[i][:], func=AF.Copy), r=[("PJ", i)], w=["DH"])
                    i = proj2(1792 + c * 128)
                    Zc = Z[:, c, :]
                    P.op("dve", lambda e, i=i, Zc=Zc: e.tensor_tensor(out=Zc[:, 2:514], in0=PJ[i][:], in1=DH[:], op=ALU.mult), r=[("PJ", i), "DH"], w=[("Z", c)])
                    wc = 14 + c * 3
                    P.op("dve", lambda e, Zc=Zc, wc=wc: e.tensor_scalar(out=ACC[:], in0=Zc[:, 2:514], scalar1=SPR[:, wc + 2:wc + 3], scalar2=None, op0=ALU.mult),
                         r=[("Z", c), "SPR"], w=["ACC"])
                    P.op("dve", lambda e, Zc=Zc, wc=wc: e.scalar_tensor_tensor(out=ACC[:], in0=Zc[:, 1:513], scalar=SPR[:, wc + 1:wc + 2], in1=ACC[:], op0=ALU.mult, op1=ALU.add),
                         r=[("Z", c), "SPR", "ACC"], w=["ACC"])
                    P.op("dve", lambda e, Zc=Zc, wc=wc: e.scalar_tensor_tensor(out=ACC[:], in0=Zc[:, 0:512], scalar=SPR[:, wc:wc + 1], in1=ACC[:], op0=ALU.mult, op1=ALU.add),
                         r=[("Z", c), "SPR", "ACC"], w=["ACC"])
                    P.op("pool", lambda e, Zc=Zc: e.tensor_copy(out=Zc[:, 0:2], in_=Zc[:, 512:514]), r=[("Z", c)], w=[("Z", c)])
                    i = proj2(1536 + c * 128)
                    P.op("dve", lambda e, i=i: e.tensor_tensor(out=ACC[:], in0=PJ[i][:], in1=ACC[:], op=ALU.mult), r=[("PJ", i), "ACC"], w=["ACC"])
                    P.op("pool", lambda e, c=c: e.tensor_tensor(out=YT[:, 6 + c, :], in0=ACC[:], in1=GS[:, 4 + c, :], op=ALU.mult),
                         r=["ACC", ("GS", 4 + c)], w=[("YT", 6 + c)])

                P.cut(10)
                i = proj2(768)
                ri = qk_norm_prep(PJ[i], ("PJ", i))
                for k in range(2):
                    rows = slice(k * 64, (k + 1) * 64)
                    P.op("dve", lambda e, i=i, ri=ri, k=k, rows=rows: e.scalar_tensor_tensor(
                        out=KC[k][0:64, 128:640], in0=PJ[i][rows, :], scalar=SPR[rows, 11:12], in1=RS[ri][rows, :], op0=ALU.mult, op1=ALU.mult),
                         r=[("PJ", i), ("RS", ri), "SPR"], w=[("KC", k)])
                for c in range(2):
                    i = proj2(512 + c * 128)
                    ri = qk_norm_prep(PJ[i], ("PJ", i))
                    for j in range(2):
                        h = 2 * c + j
                        rows = slice(j * 64, (j + 1) * 64)
                        P.op("dve", lambda e, i=i, ri=ri, h=h, rows=rows: e.scalar_tensor_tensor(
                            out=QC[h][0:64, :], in0=PJ[i][rows, :], scalar=SPR[rows, 10:11], in1=RS[ri][rows, :], op0=ALU.mult, op1=ALU.mult),
                             r=[("PJ", i), ("RS", ri), "SPR"], w=[("QC", h)])
                for sub in range(4):
                    i = nxt("pj", 2)
                    for kc in range(8):
                        P.op("pe", lambda e, kc=kc, i=i, sub=sub: e.matmul(PJ[i][:, 0:128], H2[s][:, kc, sub * 128:(sub + 1) * 128], W2[:, kc, 896:1024],
                                                                           start=(kc == 0), stop=(kc == 7)),
                             r=["W2", ("H2", s)], w=[("PJ", i)], sig=(kc == 7))
                    P.op("dve", lambda e, i=i, sub=sub: e.tensor_copy(out=VC[:, 1 + sub, :, 0:64], in_=PJ[i][:, 0:128].rearrange("p (k d) -> p k d", k=2)),
                         r=[("PJ", i)], w=["VC"])
                SWs = [(pb[2], "SW"), (tpb, ("TP", 0))]
                OCs = [(pb[3], "OC"), (pb[3], "OC")]
                for h in range(4):
                    k = h // 2
                    OCb, ock = OCs[h % 2]
                    pend = []
                    for b in range(4):
                        g = 4 * t + b
                        lo = 0 if g > 0 else 128
                        pi = (h * 4 + b) % 4
                        SWb, swk = SWs[(h * 4 + b) % 2]
                        qs = QC[h][0:65, b * 128:(b + 1) * 128]
                        P.op("pe", lambda e, h=h, lo=lo, SWb=SWb: e.matmul(SWb[:, lo:256], ident, CB[:, CB_CMS + h * 256 + lo:CB_CMS + (h + 1) * 256], start=True, stop=False),
                             r=["CB"], w=[swk], sig=False)
                        if g > 0:
                            P.op("pe", lambda e, k=k, b=b, qs=qs, SWb=SWb: e.matmul(SWb[:, 0:128], KC[k][0:65, b * 128:(b + 1) * 128], qs, start=False, stop=False),
                                 r=[("KC", k), ("KCaug", k), ("QC", h), ("QCaug", h)], w=[swk], sig=False)
                        P.op("pe", lambda e, k=k, b=b, qs=qs, SWb=SWb: e.matmul(SWb[:, 128:256], KC[k][0:65, 128 + b * 128:128 + (b + 1) * 128], qs, start=False, stop=True),
                             r=[("KC", k), ("KCaug", k), ("QC", h), ("QCaug", h)], w=[swk])
                        P.op("act", lambda e, pi=pi, lo=lo, h=h, SWb=SWb: e.activation(out=PS[pi][:, lo:256], in_=SWb[:, lo:256], func=AF.Exp,
                                                                                       bias=CF[:, CF_SB + h:CF_SB + h + 1], scale=0.125),
                             r=[swk, "CF"], w=[("PS", pi)])

                        def pv(b=b, k=k, pi=pi, g=g, OCb=OCb, ock=ock):
                            oc = OCb[:, b * 128:(b + 1) * 128]
                            if g > 0:
                                P.op("pe", lambda e: e.matmul(oc, VC[:, b, k, :], PS[pi][:, 0:128], start=True, stop=False),
                                     r=["VC", "VCones", ("PS", pi)], w=[ock], sig=False)
                            P.op("pe", lambda e: e.matmul(oc, VC[:, 1 + b, k, :], PS[pi][:, 128:256], start=(g == 0), stop=True),
                                 r=["VC", "VCones", ("PS", pi)], w=[ock])
                        pend.append(pv)
                        if len(pend) > 1:
                            pend.pop(0)()
                    for f_ in pend:
                        f_()
                    hr = slice((h % 2) * 64, (h % 2) * 64 + 64)
                    ci = nxt("rc", 2)
                    P.op("dve", lambda e, ci=ci, h=h, hr=hr, OCb=OCb: e.tensor_scalar(out=RC[ci][hr, :], in0=OCb[64:128, :], scalar1=ESK[64:128, h:h + 1], scalar2=None, op0=ALU.add),
                         r=[ock, "ESK"], w=[("RC", ci)])
                    P.op("dve", lambda e, ci=ci, hr=hr: e.reciprocal(out=RC[ci][hr, :], in_=RC[ci][hr, :]), r=[("RC", ci)], w=[("RC", ci)])
                    P.op("dve", lambda e, ci=ci, hr=hr, h=h: e.tensor_tensor(out=RC[ci][hr, :], in0=RC[ci][hr, :], in1=GS[hr, 2 + h // 2, :], op=ALU.mult),
                         r=[("RC", ci), ("GS", 2 + h // 2)], w=[("RC", ci)])
                    P.op("dve", lambda e, ci=ci, hr=hr, h=h, OCb=OCb: e.tensor_tensor(out=YT[hr, 4 + h // 2, :], in0=OCb[0:64, :], in1=RC[ci][hr, :], op=ALU.mult),
                         r=[ock, ("RC", ci)], w=[("YT", 4 + h // 2)])
                for k in range(2):
                    P.op("pool", lambda e, k=k: e.tensor_copy(out=KC[k][0:64, 0:128], in_=KC[k][0:64, 512:640]), r=[("KC", k)], w=[("KC", k)])
                P.op("pool", lambda e: e.tensor_copy(out=VC[:, 0, :, 0:64], in_=VC[:, 4, :, 0:64]), r=["VC"], w=["VC"])

                if t == 0 and l == 0:
                    dump("H20", H2[s][:, 0, :], [("H2", s)])
                    for c in range(8):
                        dump(f"YT{c}", YT[:, c, :], [("YT", c)])
                P.cut(11)
                for sub in range(4):
                    for hf in range(2):
                        oi = (sub * 2 + hf) % 2
                        for kc in range(8):
                            P.op("pe", lambda e, kc=kc, oi=oi, sub=sub, hf=hf: e.matmul(PO[oi][:], YT[:, kc, sub * 128:(sub + 1) * 128], WO[:, kc, hf * 512:(hf + 1) * 512],
                                                                                       start=(kc == 0), stop=(kc == 7)),
                                 r=["WO"] + [("YT", q) for q in range(8)], w=[("PO", oi)], sig=(kc == 7))
                        P.op("dve", lambda e, oi=oi, sub=sub, hf=hf: e.tensor_tensor(out=X2[s][:, sub, hf * 512:(hf + 1) * 512], in0=PO[oi][:],
                                                                                    in1=X2[s][:, sub, hf * 512:(hf + 1) * 512], op=ALU.add),
                             r=[("PO", oi), ("X2", s)], w=[("X2", s)])
                P.dma(lambda e, t=t, s=s: e.dma_start(out=xdst[t * TT:(t + 1) * TT, :].rearrange("(s p) d -> p s d", p=128), in_=X2[s][:]),
                      r=[("X2", s)], w=["xdst"], key=f"x2{s}")
            P.barrier()

        if DBG:
            P.stopped = False
            P.op("pool", lambda e: e.memset(DBGT[:, dbg_off[0]:12288], 0.0), w=["DBG"]) if dbg_off[0] < 12288 else None
            P.dma(lambda e: e.dma_start(out=dbg_d[:, :], in_=DBGT[:]), r=["DBG"], w=["dbg_d"], key="dbg")
            P.barrier()
        block = es.enter_context(nc.Block())
        P.emit(block)
    return nc


_CACHE = {}


def _host_layout(inputs, NL):
    f = lambda a: np.ascontiguousarray(np.asarray(a, dtype=np.float32))
    spr = np.zeros((NL, 128, NSPR), np.float32)
    pw = np.zeros((NL, 128, 2, 128), np.float32)
    for l in range(NL):
        spr[l, :, 0:8] = f(inputs["norm_g"])[l].reshape(8, 128).T
        spr[l, :, 8] = np.tile(f(inputs["a_q_norm"])[l], 2)
        spr[l, :, 9] = np.tile(f(inputs["a_k_norm"])[l], 2)
        spr[l, :, 10] = np.tile(f(inputs["c_q_norm"])[l], 2)
        spr[l, :, 11] = np.tile(f(inputs["c_k_norm"])[l], 2)
        spr[l, :, 12:14] = f(inputs["pool_scale"])[l].reshape(2, 128).T
        cw = f(inputs["conv_w"])[l]
        for c in range(2):
            for j in range(3):
                spr[l, :, 14 + c * 3 + j] = cw[j, c * 128:(c + 1) * 128]
        spr[l, :, 20:24] = f(inputs["c_sinks"])[l][None, :]
        pwl = f(inputs["pool_w"])[l]
        for c in range(2):
            for half in range(2):
                pw[l, half * 64:(half + 1) * 64, c, half * 64:(half + 1) * 64] = pwl[2 * c + half]
    return spr, pw


def run(inputs, S, NL, n_cores):
    key = (S, NL)
    if key not in _CACHE:
        _CACHE[key] = (build_program(S, NL), make_consts(S))
    nc, consts = _CACHE[key]
    spr, pw = _host_layout(inputs, NL)
    x = np.asarray(inputs["x"], dtype=np.float32)
    w_in = np.ascontiguousarray(np.asarray(inputs["w_in"], dtype=np.float32))
    w_out = np.ascontiguousarray(np.asarray(inputs["w_out"], dtype=np.float32))
    in_maps = []
    for c in range(n_cores):
        m = {"x": np.ascontiguousarray(x[c]), "w_in": w_in, "w_out": w_out, "spr": spr, "pw": pw}
        m.update(consts)
        in_maps.append(m)
    res = run_bass_kernel_spmd(nc, in_maps, core_ids=list(range(n_cores)))
    if "dbg" in res.results[0]:
        run.dbg = np.asarray(res.results[0]["dbg"]).astype(np.float32)
    return np.stack([np.asarray(r["out"], dtype=np.float32) for r in res.results], axis=0)


def kernel(x, norm_g, w_in, w_out, a_q_norm, a_k_norm, pool_w, pool_scale, c_q_norm, c_k_norm, c_sinks, conv_w):
    inputs = dict(x=x, norm_g=norm_g, w_in=w_in, w_out=w_out, a_q_norm=a_q_norm, a_k_norm=a_k_norm, pool_w=pool_w,
                  pool_scale=pool_scale, c_q_norm=c_q_norm, c_k_norm=c_k_norm, c_sinks=c_sinks, conv_w=conv_w)
    x = np.asarray(x)
    return run(inputs, x.shape[1], np.asarray(w_in).shape[0], x.shape[0])
```
